# Optimizing a Trainium2 kernel written in Bass

```python
import math
import jax, jax.numpy as jnp
from jax import lax
import numpy as np

D_MODEL = 1024
BATCH = 16
SEQ = 2048
DEPTH = 2

HEAD_DIM = 64
N_MEM_HEADS = 4
MEM_WIDTH = N_MEM_HEADS * HEAD_DIM
SEQ_WIDTH = D_MODEL - MEM_WIDTH
N_MEM = 256
ROPE_THETA = 10000.0
DSA_HEADS = SEQ_WIDTH // HEAD_DIM
IDX_HEADS = 8
IDX_DIM = 64
DSA_MAX_TOPK = 256
SPARSE_Q_BLOCK = 32
GLA_HEADS = 4
GLA_DV = SEQ_WIDTH // GLA_HEADS
GLA_DK = GLA_DV // 2
GLA_GATE_RANK = 16
GLA_TAU = 16.0
GLA_CHUNK = 64
N_GROUPS = 4
EXPERTS_PER_GROUP = 8
EXPERT_TOPK = 2
EXPERT_FF = 256
ALPHA = (2 * DEPTH) ** 0.25
BETA = (8 * DEPTH) ** -0.25
LN_EPS = 1e-5
RMS_EPS = 1e-6
N_DSA_LAYERS = (DEPTH + 1) // 2
N_GLA_LAYERS = DEPTH // 2

DSA_SPLITS = [SEQ_WIDTH, SEQ_WIDTH, SEQ_WIDTH, IDX_HEADS * IDX_DIM, IDX_DIM, IDX_HEADS, MEM_WIDTH]
GLA_SPLITS = [GLA_HEADS * GLA_DK, GLA_HEADS * GLA_DK, SEQ_WIDTH, SEQ_WIDTH, GLA_GATE_RANK, MEM_WIDTH]
DSA_IN_WIDTH = sum(DSA_SPLITS)
GLA_IN_WIDTH = sum(GLA_SPLITS)

kernel_name = "hybrid_dsa_gla_memory_hmoe_deepnorm"


def _split(a, sizes):
    return jnp.split(a, list(np.cumsum(sizes)[:-1]), axis=-1)


def layer_norm(x, g, b):
    xf = x.astype(jnp.float32)
    mu = jnp.mean(xf, -1, keepdims=True)
    var = jnp.mean(jnp.square(xf - mu), -1, keepdims=True)
    return ((xf - mu) * lax.rsqrt(var + LN_EPS) * g + b).astype(x.dtype)


def rope_tables(positions, dim):
    inv = ROPE_THETA ** (-jnp.arange(0, dim, 2, dtype=jnp.float32) / dim)
    ang = positions.astype(jnp.float32)[..., None] * inv
    return jnp.cos(ang), jnp.sin(ang)


def apply_rope(x, cos, sin):
    xf = x.astype(jnp.float32)
    x1, x2 = jnp.split(xf, 2, axis=-1)
    return jnp.concatenate([x1 * cos - x2 * sin, x2 * cos + x1 * sin], -1).astype(x.dtype)


def memory_attention(qm, mem, w_kv):
    B, M, _ = mem.shape
    km, vm = jnp.split(mem @ w_kv, 2, axis=-1)
    km = km.reshape(B, M, N_MEM_HEADS, HEAD_DIM)
    vm = vm.reshape(B, M, N_MEM_HEADS, HEAD_DIM)
    logits = jnp.einsum('bshd,bmhd->bhsm', qm, km).astype(jnp.float32) * HEAD_DIM ** -0.5
    p = jax.nn.softmax(logits, axis=-1).astype(vm.dtype)
    return jnp.einsum('bhsm,bmhd->bshd', p, vm)


def sparse_attention(q, k, v, qi, ki, wi, topk):
    B, S, H, dh = q.shape
    nb = S // SPARSE_Q_BLOCK
    spos = jnp.arange(S)
    gather = jax.vmap(lambda a, i: a[i])

    def to_blocks(a):
        return a.reshape((B, nb, SPARSE_Q_BLOCK) + a.shape[2:]).swapaxes(0, 1)

    def block(args):
        start, qb, qib, wib = args
        tpos = start + jnp.arange(SPARSE_Q_BLOCK)
        rel = jax.nn.relu(jnp.einsum('bqhd,bsd->bqhs', qib, ki))
        score = jnp.einsum('bqhs,bqh->bqs', rel, wib).astype(jnp.float32)
        causal = spos[None, :] <= tpos[:, None]
        score = jnp.where(causal[None], score, -jnp.inf)
        _, sel = lax.top_k(score, topk)
        valid = sel <= tpos[None, :, None]
        kg = gather(k, sel)
        vg = gather(v, sel)
        logits = jnp.einsum('bqhd,bqkhd->bqhk', qb, kg).astype(jnp.float32) * dh ** -0.5
        logits = jnp.where(valid[:, :, None, :], logits, -jnp.inf)
        p = jax.nn.softmax(logits, axis=-1).astype(vg.dtype)
        return jnp.einsum('bqhk,bqkhd->bqhd', p, vg)

    starts = jnp.arange(nb, dtype=jnp.int32) * SPARSE_Q_BLOCK
    out = lax.map(block, (starts, to_blocks(q), to_blocks(qi), to_blocks(wi)))
    return out.swapaxes(0, 1).reshape(B, S, H, dh)


def dsa_mixer(x, cos, sin, w_in, idx_k_g, idx_k_b):
    B, S, _ = x.shape
    q, k, v, qi, ki, wi, qm = _split(x @ w_in, DSA_SPLITS)
    c4, s4 = cos[:, :, None, :], sin[:, :, None, :]
    q = apply_rope(q.reshape(B, S, DSA_HEADS, HEAD_DIM), c4, s4)
    k = apply_rope(k.reshape(B, S, DSA_HEADS, HEAD_DIM), c4, s4)
    v = v.reshape(B, S, DSA_HEADS, HEAD_DIM)
    qi = apply_rope(qi.reshape(B, S, IDX_HEADS, IDX_DIM), c4, s4)
    ki = apply_rope(layer_norm(ki, idx_k_g, idx_k_b), cos, sin)
    wi = wi * (IDX_HEADS ** -0.5 * IDX_DIM ** -0.5)
    topk = min(DSA_MAX_TOPK, S // 4)
    out = sparse_attention(q, k, v, qi, ki, wi, topk)
    return out.reshape(B, S, SEQ_WIDTH), qm


def gla_chunked(q, k, v, g):
    B, S, H, dk = q.shape
    dv = v.shape[-1]
    C = GLA_CHUNK
    nc = S // C

    def chunks(a):
        return a.astype(jnp.float32).reshape(B, nc, C, H, a.shape[-1]).transpose(1, 0, 3, 2, 4)

    tri = jnp.tril(jnp.ones((C, C), bool))[:, :, None]

    def step(state, inp):
        qc, kc, vc, gc = inp
        b = jnp.cumsum(gc, axis=2)
        o_inter = jnp.einsum('bhcd,bhde->bhce', qc * jnp.exp(b), state)
        diff = b[:, :, :, None, :] - b[:, :, None, :, :]
        decay = jnp.where(tri, jnp.exp(jnp.where(tri, diff, 0.0)), 0.0)
        attn = jnp.einsum('bhid,bhjd,bhijd->bhij', qc, kc, decay)
        o_intra = jnp.einsum('bhij,bhje->bhie', attn, vc)
        b_last = b[:, :, -1, :]
        state = state * jnp.exp(b_last)[..., None] + jnp.einsum(
            'bhjd,bhje->bhde', kc * jnp.exp(b_last[:, :, None, :] - b), vc)
        return state, o_inter + o_intra

    state0 = jnp.zeros((B, H, dk, dv), jnp.float32)
    _, o = lax.scan(step, state0, (chunks(q), chunks(k), chunks(v), chunks(g)))
    return o.transpose(1, 0, 3, 2, 4).reshape(B, S, H, dv)


def gla_mixer(x, w_in, w_gate, b_gate, norm_g):
    B, S, _ = x.shape
    q, k, v, r, a1, qm = _split(x @ w_in, GLA_SPLITS)
    q = q.reshape(B, S, GLA_HEADS, GLA_DK) * GLA_DK ** -0.5
    k = k.reshape(B, S, GLA_HEADS, GLA_DK)
    v = v.reshape(B, S, GLA_HEADS, GLA_DV)
    g = jax.nn.log_sigmoid((a1 @ w_gate + b_gate).astype(jnp.float32)) / GLA_TAU
    g = g.reshape(B, S, GLA_HEADS, GLA_DK)
    o = gla_chunked(q, k, v, g)
    o = o * lax.rsqrt(jnp.mean(jnp.square(o), -1, keepdims=True) + RMS_EPS) * norm_g
    o = o.astype(x.dtype) * jax.nn.silu(r.reshape(B, S, GLA_HEADS, GLA_DV))
    return o.reshape(B, S, SEQ_WIDTH), qm


def hier_moe(x, w_group, b_group, w_router, b_router, w13, w2):
    B, S, D = x.shape
    t = x.reshape(-1, D)
    glog = (t @ w_group + b_group).astype(jnp.float32)
    gprob = jax.nn.softmax(glog, axis=-1)
    gsel = jnp.argmax(glog, axis=-1)
    pg = jnp.take_along_axis(gprob, gsel[:, None], axis=1)[:, 0]
    elog = (jnp.einsum('td,gde->tge', t, w_router) + b_router).astype(jnp.float32)
    elog = jnp.take_along_axis(elog, gsel[:, None, None], axis=1)[:, 0]
    top_v, top_i = lax.top_k(elog, EXPERT_TOPK)
    wts = jax.nn.softmax(top_v, axis=-1) * pg[:, None]
    emask = jnp.sum(jax.nn.one_hot(top_i, EXPERTS_PER_GROUP, dtype=jnp.float32) * wts[..., None], axis=1)
    comb = jax.nn.one_hot(gsel, N_GROUPS, dtype=jnp.float32)[:, :, None] * emask[:, None, :]
    y = jnp.zeros(t.shape, jnp.float32)
    for gi in range(N_GROUPS):
        h = jnp.einsum('td,edf->tef', t, w13[gi])
        a, u = jnp.split(h, 2, axis=-1)
        act = jax.nn.silu(a) * u * comb[:, gi, :, None].astype(h.dtype)
        y = y + jnp.einsum('tef,efd->td', act, w2[gi]).astype(jnp.float32)
    return y.astype(x.dtype).reshape(B, S, D)


def setup_inputs(seed: int = 0) -> dict:
    key = jax.random.key(seed)
    ks = jax.random.split(key, 24)
    f32 = jnp.float32

    def nrm(k, shape, fan_in, scale=1.0):
        return jax.random.normal(k, shape, f32) * (scale * fan_in ** -0.5)

    def small(k, shape, s):
        return jax.random.normal(k, shape, f32) * s

    G, E, F = N_GROUPS, EXPERTS_PER_GROUP, EXPERT_FF
    offsets = jax.random.randint(ks[2], (BATCH, 1), 0, 4096, dtype=jnp.int32)
    positions = (offsets + jnp.arange(SEQ, dtype=jnp.int32)[None, :]).astype(jnp.int32)
    return {
        "x": jax.random.normal(ks[0], (BATCH, SEQ, D_MODEL), f32),
        "mem": jax.random.normal(ks[1], (BATCH, N_MEM, D_MODEL), f32),
        "positions": positions,
        "dsa_w_in": nrm(ks[3], (N_DSA_LAYERS, D_MODEL, DSA_IN_WIDTH), D_MODEL),
        "dsa_idx_k_g": 1.0 + small(ks[4], (N_DSA_LAYERS, IDX_DIM), 0.02),
        "dsa_idx_k_b": small(ks[5], (N_DSA_LAYERS, IDX_DIM), 0.02),
        "gla_w_in": nrm(ks[6], (N_GLA_LAYERS, D_MODEL, GLA_IN_WIDTH), D_MODEL),
        "gla_w_gate": nrm(ks[7], (N_GLA_LAYERS, GLA_GATE_RANK, GLA_HEADS * GLA_DK), GLA_GATE_RANK),
        "gla_b_gate": small(ks[8], (N_GLA_LAYERS, GLA_HEADS * GLA_DK), 0.02),
        "gla_norm_g": 1.0 + small(ks[9], (N_GLA_LAYERS, GLA_DV), 0.02),
        "w_mem_kv": nrm(ks[10], (DEPTH, D_MODEL, 2 * MEM_WIDTH), D_MODEL),
        "w_out": nrm(ks[11], (DEPTH, D_MODEL, D_MODEL), D_MODEL, BETA),
        "ln1_g": 1.0 + small(ks[12], (DEPTH, D_MODEL), 0.02),
        "ln1_b": small(ks[13], (DEPTH, D_MODEL), 0.02),
        "ln2_g": 1.0 + small(ks[14], (DEPTH, D_MODEL), 0.02),
        "ln2_b": small(ks[15], (DEPTH, D_MODEL), 0.02),
        "moe_w_group": nrm(ks[16], (DEPTH, D_MODEL, G), D_MODEL),
        "moe_b_group": small(ks[17], (DEPTH, G), 0.01),
        "moe_w_router": nrm(ks[18], (DEPTH, G, D_MODEL, E), D_MODEL),
        "moe_b_router": small(ks[19], (DEPTH, G, E), 0.01),
        "moe_w13": nrm(ks[20], (DEPTH, G, E, D_MODEL, 2 * F), D_MODEL),
        "moe_w2": nrm(ks[21], (DEPTH, G, E, F, D_MODEL), F, BETA),
    }


def reference(x, mem, positions, dsa_w_in, dsa_idx_k_g, dsa_idx_k_b, gla_w_in, gla_w_gate,
              gla_b_gate, gla_norm_g, w_mem_kv, w_out, ln1_g, ln1_b, ln2_g, ln2_b,
              moe_w_group, moe_b_group, moe_w_router, moe_b_router, moe_w13, moe_w2):
    B, S, _ = x.shape
    cos, sin = rope_tables(positions, HEAD_DIM)
    ia = 0
    ib = 0
    for i in range(DEPTH):
        if i % 2 == 0:
            seq_out, qm = dsa_mixer(x, cos, sin, dsa_w_in[ia], dsa_idx_k_g[ia], dsa_idx_k_b[ia])
            ia += 1
        else:
            seq_out, qm = gla_mixer(x, gla_w_in[ib], gla_w_gate[ib], gla_b_gate[ib], gla_norm_g[ib])
            ib += 1
        mem_out = memory_attention(qm.reshape(B, S, N_MEM_HEADS, HEAD_DIM), mem, w_mem_kv[i])
        mixed = jnp.concatenate([seq_out, mem_out.reshape(B, S, MEM_WIDTH)], axis=-1) @ w_out[i]
        x = layer_norm(ALPHA * x + mixed, ln1_g[i], ln1_b[i])
        ffn = hier_moe(x, moe_w_group[i], moe_b_group[i], moe_w_router[i], moe_b_router[i],
                       moe_w13[i], moe_w2[i])
        x = layer_norm(ALPHA * x + ffn, ln2_g[i], ln2_b[i])
    return x
```

```python
import contextlib
import math
import types
import numpy as np
import concourse.bass as bass
import concourse.mybir as mybir
from concourse.bass_utils import run_bass_kernel_spmd

F32 = mybir.dt.float32
BF16 = mybir.dt.bfloat16
I32 = mybir.dt.int32
AF = mybir.ActivationFunctionType
ALU = mybir.AluOpType
AX = mybir.AxisListType

ENGS = ("pe", "act", "dve", "pool", "sp")

D = 1024
S = 2048
NT = S // 128
NB_PER_CORE = 2
DEPTH = 2
ALPHA = (2 * DEPTH) ** 0.25
LN_EPS = 1e-5
RMS_EPS = 1e-6
DSA_IN = 3144
GLA_IN = 2576
NEG = -1.0e30
MOE_MUL_ENG = "pool"
SCHED = True
SCHED_WINDOW = 48


class Buf:
    __slots__ = ("name", "writer", "readers")

    def __init__(self, name=""):
        self.name = name
        self.writer = None
        self.readers = []


class Op:
    __slots__ = ("eng", "fn", "deps", "is_dma", "signal", "sigval", "lane", "laneval",
                 "lane_prev", "barriered", "gid", "cost", "t0", "t1")

    def __init__(self, eng, fn, is_dma):
        self.eng = eng
        self.fn = fn
        self.deps = []
        self.is_dma = is_dma
        self.signal = False
        self.sigval = 0
        self.lane = None
        self.laneval = 0
        self.lane_prev = 0
        self.barriered = False
        self.gid = 0
        self.cost = None
        self.t0 = None
        self.t1 = None


def _freeze(fn):
    if fn is None or fn.__closure__ is None:
        return fn
    cells = []
    for c in fn.__closure__:
        try:
            cells.append(types.CellType(c.cell_contents))
        except ValueError:
            cells.append(c)
    return types.FunctionType(fn.__code__, fn.__globals__, fn.__name__, fn.__defaults__, tuple(cells))


class Prog:
    def __init__(self, nc):
        self.nc = nc
        self.ops = {e: [] for e in ENGS}
        self.stacks = [contextlib.ExitStack()]
        self.n_lanes = {"sp": 16, "pool": 12, "act": 8}
        self._uid = 0
        self.seq = []

    def sbuf(self, name, shape, dtype):
        self._uid += 1
        return self.stacks[-1].enter_context(
            self.nc.sbuf_tensor(f"{name}_{self._uid}", list(shape), dtype))

    def psum(self, name, shape, dtype=F32):
        self._uid += 1
        return self.stacks[-1].enter_context(
            self.nc.psum_tensor(f"{name}_{self._uid}", list(shape), dtype))

    @contextlib.contextmanager
    def scope(self):
        self.stacks.append(contextlib.ExitStack())
        try:
            yield
        finally:
            self.barrier()
            self.stacks.pop().close()

    def op(self, eng, fn, reads=(), writes=(), dma=False, cost=None):
        o = Op(eng, _freeze(fn), dma)
        o.cost = cost
        o.gid = len(self.seq)
        seen = set()
        for b in reads:
            if b.writer is not None and id(b.writer) not in seen:
                o.deps.append((b.writer, "raw"))
                seen.add(id(b.writer))
        for b in writes:
            if b.writer is not None and id(b.writer) not in seen:
                o.deps.append((b.writer, "waw"))
                seen.add(id(b.writer))
            for r in b.readers:
                if id(r) not in seen:
                    o.deps.append((r, "war"))
                    seen.add(id(r))
        for b in reads:
            b.readers.append(o)
        for b in writes:
            b.writer = o
            b.readers = []
        self.seq.append(o)
        return o

    def pe(self, fn, reads=(), writes=(), cost=None):
        return self.op("pe", fn, reads, writes, cost=cost)

    def act(self, fn, reads=(), writes=(), cost=None):
        return self.op("act", fn, reads, writes, cost=cost)

    def dve(self, fn, reads=(), writes=(), cost=None):
        return self.op("dve", fn, reads, writes, cost=cost)

    def pool(self, fn, reads=(), writes=(), cost=None):
        return self.op("pool", fn, reads, writes, cost=cost)

    def dma(self, eng, out, in_, reads=(), writes=(), **kw):
        return self.op(eng, lambda e: e.dma_start(out=out, in_=in_, **kw), reads, writes, dma=True)

    def barrier(self):
        self.seq.append(None)

    def _materialise_fence(self):
        lasts = []
        for e in ENGS:
            for o in reversed(self.ops[e]):
                if o.fn is not None and not o.is_dma:
                    lasts.append(o)
                    break
        dmas = [o for e in ENGS for o in self.ops[e] if o.is_dma and not o.barriered]
        for e in ENGS:
            o = Op(e, None, False)
            for l in lasts:
                if l.eng != e:
                    o.deps.append((l, "raw"))
            for d in dmas:
                o.deps.append((d, "raw"))
            self.ops[e].append(o)
        for d in dmas:
            d.barriered = True

    DEFCOST = {"pe": 0.3, "act": 0.5, "dve": 0.6, "pool": 0.9, "sp": 0.05}

    def _schedule_segment(self, seg):
        if not SCHED or len(seg) < 3:
            for o in seg:
                self.ops[o.eng].append(o)
            return
        inseg = set(id(o) for o in seg)
        queues = {e: [o for o in seg if o.eng == e] for e in ENGS}
        heads = {e: 0 for e in ENGS}
        done = set()
        free = {e: 0.0 for e in ENGS}
        remaining = len(seg)
        SYNC = 0.25
        while remaining:
            best = None
            for e in ENGS:
                q = queues[e]
                h = heads[e]
                while h < len(q) and id(q[h]) in done:
                    h += 1
                heads[e] = h
                if h >= len(q):
                    continue
                lim = min(len(q), h + SCHED_WINDOW)
                for k in range(h, lim):
                    o = q[k]
                    if id(o) in done:
                        continue
                    rt = 0.0
                    ok = True
                    for d, kind in o.deps:
                        if id(d) not in inseg:
                            continue
                        if id(d) not in done:
                            ok = False
                            break
                        if d.eng == o.eng and not d.is_dma and (o.eng == "pe" or kind != "raw"):
                            t = d.t0
                        else:
                            t = d.t1 + SYNC
                        if t > rt:
                            rt = t
                    if not ok:
                        continue
                    st = rt if rt > free[e] else free[e]
                    key = (st, o.gid)
                    if best is None or key < best[0]:
                        best = (key, e, o)
                    if st <= free[e]:
                        break
            assert best is not None, "scheduler deadlock"
            (st, _), e, o = best
            c = o.cost if o.cost is not None else (3.0 if o.is_dma else self.DEFCOST[e])
            o.t0 = st
            if o.is_dma:
                o.t1 = st + c
                free[e] = st + 0.1
            else:
                o.t1 = st + c
                free[e] = o.t1
            done.add(id(o))
            self.ops[e].append(o)
            remaining -= 1

    def _schedule(self):
        seg = []
        for it in self.seq:
            if it is None:
                self._schedule_segment(seg)
                seg = []
                self._materialise_fence()
            else:
                seg.append(it)
        self._schedule_segment(seg)

    def emit(self):
        nc = self.nc

        def needs_wait(o, d, kind):
            return not ((not d.is_dma) and d.eng == o.eng and (o.eng == "pe" or kind != "raw"))

        for e in ENGS:
            for o in self.ops[e]:
                for d, kind in o.deps:
                    if needs_wait(o, d, kind) and not d.is_dma:
                        d.signal = True
        for e in ENGS:
            cnt = 0
            lane_vals = [0] * self.n_lanes.get(e, 1)
            li = 0
            for o in self.ops[e]:
                if o.is_dma:
                    o.lane = li
                    o.lane_prev = lane_vals[li]
                    lane_vals[li] += 16
                    o.laneval = lane_vals[li]
                    li = (li + 1) % len(lane_vals)
                elif o.signal:
                    cnt += 1
                    o.sigval = cnt
        with contextlib.ExitStack() as st:
            esem = {e: st.enter_context(nc.semaphore(f"s_{e}")) for e in ("pe", "act", "dve", "pool")}
            lsem = {e: [st.enter_context(nc.semaphore(f"l_{e}{i}")) for i in range(n)]
                    for e, n in self.n_lanes.items()}
            block = st.enter_context(nc.Block())

            def run(ename, eng):
                seen = {}

                def wait(sem, key, val):
                    if seen.get(key, 0) >= val:
                        return
                    seen[key] = val
                    eng.wait_ge(sem, val)

                for o in self.ops[ename]:
                    for d, kind in o.deps:
                        if not needs_wait(o, d, kind):
                            continue
                        if d.is_dma:
                            wait(lsem[d.eng][d.lane], ("l", d.eng, d.lane), d.laneval)
                        else:
                            wait(esem[d.eng], ("e", d.eng), d.sigval)
                    if o.fn is None:
                        continue
                    if o.is_dma:
                        if o.lane_prev > 0:
                            wait(lsem[ename][o.lane], ("l", ename, o.lane), o.lane_prev)
                        o.fn(eng).then_inc(lsem[ename][o.lane], 16)
                    else:
                        ins = o.fn(eng)
                        if o.signal:
                            ins.then_inc(esem[ename], 1)

            @block.tensor
            def _(eng):
                run("pe", eng)

            @block.scalar
            def _(eng):
                run("act", eng)

            @block.vector
            def _(eng):
                run("dve", eng)

            @block.gpsimd
            def _(eng):
                run("pool", eng)

            @block.sync
            def _(eng):
                run("sp", eng)

    def finish(self):
        self._schedule()
        fin = Op("sp", None, False)
        for e in ENGS:
            for o in self.ops[e]:
                if o.is_dma:
                    fin.deps.append((o, "raw"))
        fin.deps.sort(key=lambda t: t[0].laneval)
        self.ops["sp"].append(fin)
        self.emit()
        while self.stacks:
            self.stacks.pop().close()


class K:
    def __init__(self, nseq=NB_PER_CORE, debug=None):
        self.nseq = nseq
        self.debug = debug
        nc = bass.Bass("TRN2", target_bir_lowering=False)
        self.nc = nc
        self.P = Prog(nc)
        dt = nc.dram_tensor

        def inp(name, shape, dtype=F32):
            return dt(name, list(shape), dtype, kind="ExternalInput").ap()

        I = {}
        I["x"] = inp("x", [nseq, S, D])
        I["xT"] = inp("xT", [nseq, D, S])
        I["memT"] = inp("memT", [nseq, D, 256])
        I["pos"] = inp("pos", [nseq, 128, NT], I32)
        I["dsa_w_in"] = inp("dsa_w_in", [1, D, DSA_IN])
        I["dsa_idx_k_g"] = inp("dsa_idx_k_g", [1, 64])
        I["dsa_idx_k_b"] = inp("dsa_idx_k_b", [1, 64])
        I["gla_w_in"] = inp("gla_w_in", [1, D, GLA_IN])
        I["gla_w_gate"] = inp("gla_w_gate", [1, 16, 384])
        I["gla_b_gate"] = inp("gla_b_gate", [1, 384])
        I["gla_norm_g"] = inp("gla_norm_g", [1, 192])
        I["w_mem_kv"] = inp("w_mem_kv", [2, D, 512])
        I["w_out"] = inp("w_out", [2, D, D])
        for n in ("ln1_g", "ln1_b", "ln2_g", "ln2_b"):
            I[n] = inp(n, [2, D])
        I["moe_w_group"] = inp("moe_w_group", [2, D, 4])
        I["moe_b_group"] = inp("moe_b_group", [2, 4])
        I["moe_w_router"] = inp("moe_w_router", [2, 4, D, 8])
        I["moe_b_router"] = inp("moe_b_router", [2, 4, 8])
        I["moe_w13"] = inp("moe_w13", [2, 4, 8, D, 512])
        I["moe_w2"] = inp("moe_w2", [2, 4, 8, 256, D])
        self.I = I
        self.out = dt("out", [nseq, S, D], F32, kind="ExternalOutput").ap()
        import os
        self.dbg = dt("dbg", [S, D], F32, kind="ExternalOutput").ap() if os.environ.get("KDBG") else None
        self.dbgc = dt("dbgc", [D, S], F32, kind="ExternalOutput").ap() if os.environ.get("KDBG") else None
        self.x1s = dt("x1s", [S, D], F32).ap()
        self.x2s = dt("x2s", [S, D], F32).ap()
        self.combT_d = dt("combT_d", [32, S], F32).ap()
        self.Bx1s = [Buf() for _ in range(NT)]
        self.Bx2s = [Buf() for _ in range(NT)]
        self.Bcomb = [Buf() for _ in range(NT)]

    def consts(self):
        P, nc = self.P, self.nc
        self.ident = P.sbuf("ident", [128, 128], F32)
        self.identb = P.sbuf("identb", [128, 128], BF16)
        self.Bc = Buf("consts")
        B = self.Bc
        ident, identb = self.ident, self.identb
        P.pool(lambda e: e.memset(ident[:], 1.0), writes=[B])
        P.pool(lambda e: e.affine_select(out=ident[:], in_=ident[:], compare_op=ALU.is_equal, fill=0.0,
                                         base=0, pattern=[[-1, 128]], channel_multiplier=1),
               reads=[B], writes=[B])
        P.pool(lambda e: e.tensor_copy(out=identb[:], in_=ident[:]), reads=[B], writes=[B])
        self.xT_bf = P.sbuf("xT_bf", [128, 8, S], BF16)
        self.BxT = [Buf() for _ in range(NT)]
        self.x1T_bf = self.xT_bf
        self.Bx1T = self.BxT
        self.psT_extra = []
        self.psT = [P.psum("psT", [128, 1024]) for _ in range(1)]
        self.BpsT = [Buf() for _ in range(1)]
        self.comb_all = P.sbuf("comb_all", [128, NT, 32], F32)

    def bcast_row(self, name, src_row_ap, n, eng="sp"):
        t = self.P.sbuf(name, [128, n], F32)
        B = Buf(name)
        self.P.dma(eng, t[:], src_row_ap.partition_broadcast(128), writes=[B])
        return t, B

    def layernorm_tile(self, z, Bz, outt, Bo, g, Bg, bt, Bb, st, Bst):
        P = self.P
        stats, mv, rstd, nmr = st
        P.dve(lambda e: e.bn_stats(out=stats[:, 0:6], in_=z[:, 0:512]), reads=[Bz], writes=[Bst])
        P.dve(lambda e: e.bn_stats(out=stats[:, 6:12], in_=z[:, 512:1024]), reads=[Bz], writes=[Bst])
        P.dve(lambda e: e.bn_aggr(out=mv[:], in_=stats[:]), reads=[Bst], writes=[Bst])
        P.act(lambda e: e.activation(out=rstd[:], in_=mv[:, 1:2], func=AF.Ln, bias=LN_EPS, scale=1.0),
              reads=[Bst], writes=[Bst], cost=0.2)
        P.act(lambda e: e.activation(out=rstd[:], in_=rstd[:], func=AF.Exp, scale=-0.5),
              reads=[Bst], writes=[Bst], cost=0.2)
        P.dve(lambda e: e.scalar_tensor_tensor(out=nmr[:], in0=mv[:, 0:1], scalar=-1.0, in1=rstd[:],
                                               op0=ALU.mult, op1=ALU.mult), reads=[Bst], writes=[Bst])
        P.act(lambda e: e.activation(out=outt[:], in_=z[:], func=AF.Identity, bias=nmr[:], scale=rstd[:]),
              reads=[Bz, Bst], writes=[Bo])
        P.dve(lambda e: e.tensor_tensor(out=outt[:], in0=outt[:], in1=g[:], op=ALU.mult),
              reads=[Bo, Bg], writes=[Bo])
        P.dve(lambda e: e.tensor_tensor(out=outt[:], in0=outt[:], in1=bt[:], op=ALU.add),
              reads=[Bo, Bb], writes=[Bo])

    def ln_scratch(self, name):
        P = self.P
        return ((P.sbuf(name + "st", [128, 12], F32), P.sbuf(name + "mv", [128, 2], F32),
                 P.sbuf(name + "rs", [128, 1], F32), P.sbuf(name + "nm", [128, 1], F32)), Buf(name))

    def router_setup(self, li):
        P, I = self.P, self.I
        self.wr = P.sbuf("wr", [128, 8, 36], F32)
        self.Bwr = Buf("wr")
        wr = self.wr
        P.dma("sp", wr[:, :, 0:4], I["moe_w_group"][li].rearrange("(c p) g -> p c g", p=128), writes=[self.Bwr])
        for g in range(4):
            P.dma("sp", wr[:, :, 4 + 8 * g:12 + 8 * g],
                  I["moe_w_router"][li, g].rearrange("(c p) e -> p c e", p=128), writes=[self.Bwr])
        self.rbias = P.sbuf("rbias", [128, 36], F32)
        self.Brb = Buf("rbias")
        P.dma("sp", self.rbias[:, 0:4], I["moe_b_group"][li:li + 1, :].partition_broadcast(128), writes=[self.Brb])
        P.dma("sp", self.rbias[:, 4:36],
              I["moe_b_router"][li:li + 1].rearrange("o g e -> o (g e)").partition_broadcast(128),
              writes=[self.Brb])
        self.rt = []
        for k in range(2):
            d = dict(
                x1Tf=P.sbuf("x1Tf", [128, 1024], F32), lg=P.sbuf("lg", [128, 36], F32),
                sm=P.sbuf("rsm", [128, 16], F32), me=P.sbuf("rme", [128, 32], F32),
                top8=P.sbuf("top8", [128, 8], F32), ex=P.sbuf("rex", [128, 32], F32),
                comb=P.sbuf("comb", [128, 32], F32), cT=P.sbuf("cT", [32, 128], F32),
                B=Buf("rt"), BxTf=Buf("x1Tf"), BcT=Buf("cT"))
            self.rt.append(d)
        self.psR = self.psT[0]
        self.BpsR = self.BpsT[0]

    def transpose_to_bf(self, src, Bsrc, dstT, BdstT, i, also_f32=None):
        P = self.P
        psT, BpsT = self.psT[0], self.BpsT[0]
        ident = self.ident
        for c in range(8):
            P.pe(lambda e, c=c: e.transpose(out=psT[:, c * 128:(c + 1) * 128], in_=src[:, c * 128:(c + 1) * 128],
                                            identity=ident[:]),
                 reads=[Bsrc, self.Bc], writes=[BpsT] + self.psT_extra)
        if also_f32 is None:
            P.dve(lambda e: e.tensor_copy(out=dstT[:, :, i * 128:(i + 1) * 128],
                                          in_=psT[:].rearrange("p (c t) -> p c t", c=8)),
                  reads=[BpsT], writes=[BdstT])
        else:
            t, Bt = also_f32
            P.act(lambda e: e.copy(out=t[:], in_=psT[:]), reads=[BpsT], writes=[Bt])
            P.dve(lambda e: e.tensor_copy(out=dstT[:, :, i * 128:(i + 1) * 128],
                                          in_=t[:].rearrange("p (c t) -> p c t", c=8)),
                  reads=[Bt], writes=[BdstT])

    def router_tile(self, i, x1, Bx1):
        P = self.P
        r = self.rt[i % 2]
        B = r["B"]
        self.transpose_to_bf(x1, Bx1, self.x1T_bf, self.Bx1T[i], i, also_f32=(r["x1Tf"], r["BxTf"]))
        import os
        STOP = int(os.environ.get("RSTOP", "99"))
        if STOP < 1:
            return
        psR, BpsR = self.psR, self.BpsR
        x1Tf, wr = r["x1Tf"], self.wr
        for c in range(8):
            P.pe(lambda e, c=c: e.matmul(psR[:, 0:36], lhsT=x1Tf[:, c * 128:(c + 1) * 128], rhs=wr[:, c, :],
                                         start=(c == 0), stop=(c == 7)),
                 reads=[r["BxTf"], self.Bwr], writes=[BpsR])
        if STOP < 2:
            return
        lg, sm, me, top8, ex, comb, cT = r["lg"], r["sm"], r["me"], r["top8"], r["ex"], r["comb"], r["cT"]
        rb = self.rbias
        P.dve(lambda e: e.tensor_tensor(out=lg[:], in0=psR[:, 0:36], in1=rb[:], op=ALU.add),
              reads=[BpsR, self.Brb], writes=[B])
        P.dve(lambda e: e.reduce_max(out=sm[:, 0:1], in_=lg[:, 0:4], axis=AX.X), reads=[B], writes=[B])
        P.dve(lambda e: e.tensor_scalar(out=sm[:, 1:2], in0=sm[:, 0:1], scalar1=-1.0, scalar2=None, op0=ALU.mult),
              reads=[B], writes=[B])
        P.act(lambda e: e.activation(out=sm[:, 12:16], in_=lg[:, 0:4], func=AF.Exp, bias=sm[:, 1:2], scale=1.0),
              reads=[B], writes=[B])
        P.dve(lambda e: e.reduce_sum(out=sm[:, 2:3], in_=sm[:, 12:16], axis=AX.X), reads=[B], writes=[B])
        P.dve(lambda e: e.tensor_scalar(out=sm[:, 4:8], in0=lg[:, 0:4], scalar1=sm[:, 0:1], scalar2=None,
                                        op0=ALU.is_lt), reads=[B], writes=[B])
        P.dve(lambda e: e.tensor_scalar(out=sm[:, 4:8], in0=sm[:, 4:8], scalar1=-30000.0, scalar2=None,
                                        op0=ALU.mult), reads=[B], writes=[B])
        for g in range(4):
            P.dve(lambda e, g=g: e.tensor_scalar(out=me[:, 8 * g:8 * g + 8], in0=lg[:, 4 + 8 * g:12 + 8 * g],
                                                 scalar1=sm[:, 4 + g:5 + g], scalar2=None, op0=ALU.add),
                  reads=[B], writes=[B])
        P.dve(lambda e: e.max(out=top8[:], in_=me[:]), reads=[B], writes=[B])
        P.dve(lambda e: e.tensor_scalar(out=sm[:, 8:9], in0=top8[:, 0:1], scalar1=-1.0, scalar2=None, op0=ALU.mult),
              reads=[B], writes=[B])
        P.act(lambda e: e.activation(out=ex[:], in_=me[:], func=AF.Exp, bias=sm[:, 8:9], scale=1.0),
              reads=[B], writes=[B])
        P.act(lambda e: e.activation(out=sm[:, 9:10], in_=top8[:, 1:2], func=AF.Exp, bias=sm[:, 8:9], scale=1.0),
              reads=[B], writes=[B])
        P.dve(lambda e: e.scalar_tensor_tensor(out=sm[:, 11:12], in0=sm[:, 9:10], scalar=1.0, in1=sm[:, 2:3],
                                               op0=ALU.add, op1=ALU.mult), reads=[B], writes=[B])
        P.dve(lambda e: e.reciprocal(out=sm[:, 10:11], in_=sm[:, 11:12]), reads=[B], writes=[B])
        P.dve(lambda e: e.scalar_tensor_tensor(out=comb[:], in0=me[:], scalar=top8[:, 1:2], in1=ex[:],
                                               op0=ALU.is_ge, op1=ALU.mult), reads=[B], writes=[B])
        call = self.comb_all
        P.dve(lambda e: e.tensor_scalar(out=call[:, i, :], in0=comb[:], scalar1=sm[:, 10:11], scalar2=None,
                                        op0=ALU.mult), reads=[B], writes=[self.Bcomb[i]])

    def moe(self, li, b, last, after_experts=None):
        P, I = self.P, self.I
        with P.scope():
            yacc = P.sbuf("yacc", [128, NT, 1024], F32)
            Byacc = [[Buf() for _ in range(2)] for _ in range(NT)]
            w13b = [P.sbuf("w13b", [128, 8, 512], BF16) for _ in range(2)]
            w2b = [P.sbuf("w2b", [128, 2, 1024], BF16) for _ in range(2)]
            Bw13 = [Buf() for _ in range(2)]
            Bw2 = [Buf() for _ in range(2)]
            sa = [P.sbuf("sa", [128, 512], F32) for _ in range(2)]
            su = [P.sbuf("su", [128, 512], F32) for _ in range(2)]
            actT = [[P.sbuf("actT", [128, 512], BF16) for _ in range(2)] for _ in range(2)]
            Bsa = [Buf() for _ in range(2)]
            Bsu = [Buf() for _ in range(2)]
            Bact = [[Buf() for _ in range(2)] for _ in range(2)]
            hps = [P.psum("hps", [128, 512]) for _ in range(4)]
            Bh = [Buf() for _ in range(4)]
            yps_t = [P.psum("yps", [128, 512]) for _ in range(2)]
            psT0 = self.psT[0]
            yps = [yps_t[0][:, :], yps_t[1][:, :], psT0[:, 0:512], psT0[:, 512:1024]]
            By = [Buf() for _ in range(4)]
            x1T = self.x1T_bf
            call = self.comb_all
            MUL_ENG = MOE_MUL_ENG

            def load_w(e):
                s = e % 2
                g, ee = divmod(e, 8)
                P.dma("pool", w13b[s][:], I["moe_w13"][li, g, ee].rearrange("(c p) f -> p c f", p=128),
                      writes=[Bw13[s]])
                P.dma("pool", w2b[s][:], I["moe_w2"][li, g, ee].rearrange("(j p) d -> p j d", p=128),
                      writes=[Bw2[s]])

            units = [(e, tt) for e in range(32) for tt in range(4)]
            yrot = [0]

            def emit_h(k):
                e, tt = units[k]
                s, par = e % 2, k % 2
                tsl = slice(tt * 512, (tt + 1) * 512)
                for fc in range(4):
                    for c in range(8):
                        P.pe(lambda en: en.matmul(hps[fc][:], lhsT=w13b[s][:, c, fc * 128:(fc + 1) * 128],
                                                  rhs=x1T[:, c, tsl], start=(c == 0), stop=(c == 7)),
                             reads=[Bw13[s]] + self.Bx1T[tt * 4:tt * 4 + 4], writes=[Bh[fc]])
                for j in range(2):
                    P.act(lambda en: en.activation(out=sa[j][:], in_=hps[j][:], func=AF.Silu),
                          reads=[Bh[j]], writes=[Bsa[j]])
                    P.act(lambda en: en.copy(out=su[j][:], in_=hps[2 + j][:]), reads=[Bh[2 + j]], writes=[Bsu[j]])
                    P.op(MUL_ENG, lambda en: en.tensor_tensor(out=actT[par][j][:], in0=sa[j][:], in1=su[j][:],
                                                              op=ALU.mult),
                         reads=[Bsa[j], Bsu[j]], writes=[Bact[par][j]])

            def emit_y(k):
                e, tt = units[k]
                s, par = e % 2, k % 2
                for tch in range(4):
                    ti = tt * 4 + tch
                    for half in range(2):
                        r = yrot[0]
                        yrot[0] = (r + 1) % 4
                        for j in range(2):
                            P.pe(lambda en: en.matmul(yps[r], lhsT=actT[par][j][:, tch * 128:(tch + 1) * 128],
                                                      rhs=w2b[s][:, j, half * 512:(half + 1) * 512],
                                                      start=(j == 0), stop=(j == 1)),
                                 reads=[Bact[par][j], Bw2[s]], writes=[By[r]])
                        dst = yacc[:, ti, half * 512:(half + 1) * 512]
                        gate = call[:, ti, e:e + 1]
                        if e == 0:
                            P.dve(lambda en: en.tensor_scalar(out=dst, in0=yps[r], scalar1=gate, scalar2=None,
                                                              op0=ALU.mult),
                                  reads=[By[r], self.Bcomb[ti]], writes=[Byacc[ti][half]])
                        else:
                            P.dve(lambda en: en.scalar_tensor_tensor(out=dst, in0=yps[r], scalar=gate, in1=dst,
                                                                     op0=ALU.mult, op1=ALU.add),
                                  reads=[By[r], Byacc[ti][half], self.Bcomb[ti]], writes=[Byacc[ti][half]])

            load_w(0)
            for k in range(len(units)):
                emit_h(k)
                if k > 0:
                    emit_y(k - 1)
                e, tt = units[k]
                if tt == 0 and e + 1 < 32:
                    load_w(e + 1)
            emit_y(len(units) - 1)
            self.psT_extra = [By[2], By[3]]
            if after_experts is not None:
                after_experts()

            g2, Bg2 = self.bcast_row("g2", I["ln2_g"][li:li + 1, :], D)
            b2, Bb2 = self.bcast_row("b2", I["ln2_b"][li:li + 1, :], D)
            st2 = [self.ln_scratch("ln2a"), self.ln_scratch("ln2b")]
            xin = [P.sbuf("xin", [128, D], F32) for _ in range(2)]
            Bxin = [Buf() for _ in range(2)]
            xo = [P.sbuf("xo", [128, D], F32) for _ in range(2)]
            Bxo = [Buf() for _ in range(2)]
            for i in range(NT):
                p = i % 2
                P.dma("sp", xin[p][:], self.x1s[i * 128:(i + 1) * 128, :], reads=[self.Bx1s[i]], writes=[Bxin[p]])
                yv = yacc[:, i, :]
                P.dve(lambda en, p=p, yv=yv: en.scalar_tensor_tensor(out=xin[p][:], in0=xin[p][:], scalar=ALPHA,
                                                                      in1=yv, op0=ALU.mult, op1=ALU.add),
                      reads=[Bxin[p]] + Byacc[i], writes=[Bxin[p]])
                st, Bst = st2[p]
                self.layernorm_tile(xin[p], Bxin[p], xo[p], Bxo[p], g2, Bg2, b2, Bb2, st, Bst)
                if last:
                    P.dma("sp", self.out[b, i * 128:(i + 1) * 128, :], xo[p][:], reads=[Bxo[p]])
                else:
                    P.dma("sp", self.x2s[i * 128:(i + 1) * 128, :], xo[p][:], reads=[Bxo[p]], writes=[self.Bx2s[i]])
                    self.transpose_to_bf(xo[p], Bxo[p], self.xT_bf, self.BxT[i], i)
            self.psT_extra = []


def build_moe_test():
    k = K(nseq=1)
    P, I = k.P, k.I
    k.consts()
    with P.scope():
        k.router_setup(0)
        xt = [P.sbuf("xt", [128, D], F32) for _ in range(2)]
        Bxt = [Buf() for _ in range(2)]
        for i in range(NT):
            p = i % 2
            P.dma("sp", xt[p][:], I["x"][0, i * 128:(i + 1) * 128, :], writes=[Bxt[p]])
            P.dma("sp", k.x1s[i * 128:(i + 1) * 128, :], xt[p][:], reads=[Bxt[p]], writes=[k.Bx1s[i]])
            k.router_tile(i, xt[p], Bxt[p])
    k.moe(0, 0, True)
    P.finish()
    return k.nc


class Banks:
    def __init__(self, P, n):
        self.t = [P.psum("bank", [128, 512]) for _ in range(n)]
        self.B = [Buf() for _ in range(n)]
        self.i = 0

    def get(self):
        k = self.i
        self.i = (self.i + 1) % 4
        return self.t[k], self.B[k]

    def fixed(self, k):
        return self.t[4 + k], self.B[4 + k]


def _load_cast(P, dst, src, B):
    P.dma("pool", dst, src, writes=[B])


def k_load_xT(self, b):
    P, I = self.P, self.I
    P.dma("pool", self.xT_bf[:], I["xT"][b].rearrange("(c p) t -> p c t", p=128), writes=self.BxT)


def k_mem_attn(self, li, b, banks, w_in_ap, qm_off, catT, BcatT):
    P, I = self.P, self.I
    memT = P.sbuf("memT", [128, 8, 256], BF16)
    wkv = P.sbuf("wkv", [128, 8, 512], BF16)
    wqm = P.sbuf("wqm", [128, 8, 256], BF16)
    Bm, Bkv, Bqm = Buf(), Buf(), Buf()
    P.dma("pool", memT[:], I["memT"][b].rearrange("(c p) m -> p c m", p=128), writes=[Bm])
    P.dma("pool", wkv[:], I["w_mem_kv"][li].rearrange("(c p) f -> p c f", p=128), writes=[Bkv])
    P.dma("pool", wqm[:], w_in_ap[:, qm_off:qm_off + 256].rearrange("(c p) f -> p c f", p=128), writes=[Bqm])
    kmT = P.sbuf("kmT", [64, 4, 256], BF16)
    vma = P.sbuf("vma", [128, 2, 4, 128], BF16)
    qmT = P.sbuf("qmT", [64, 4, S], BF16)
    Bkm, Bvm, Bq = Buf(), Buf(), Buf()
    P.dve(lambda e: e.memset(vma[:], 1.0), writes=[Bvm])
    for h in range(4):
        ps, Bp = banks.get()
        for c in range(8):
            P.pe(lambda e, c=c, h=h, ps=ps: e.matmul(ps[0:64, 0:256], lhsT=wkv[:, c, h * 64:(h + 1) * 64],
                                                     rhs=memT[:, c, :], start=(c == 0), stop=(c == 7)),
                 reads=[Bkv, Bm], writes=[Bp])
        P.act(lambda e, h=h, ps=ps: e.copy(out=kmT[:, h, :], in_=ps[0:64, 0:256]), reads=[Bp], writes=[Bkm])
    for mc in range(2):
        ps, Bp = banks.get()
        for c in range(8):
            P.pe(lambda e, c=c, mc=mc, ps=ps: e.matmul(ps[:, 0:256], lhsT=memT[:, c, mc * 128:(mc + 1) * 128],
                                                       rhs=wkv[:, c, 256:512], start=(c == 0), stop=(c == 7)),
                 reads=[Bkv, Bm], writes=[Bp])
        P.act(lambda e, mc=mc, ps=ps: e.copy(out=vma[:, mc, :, 0:64],
                                             in_=ps[:, 0:256].rearrange("p (h d) -> p h d", h=4)),
              reads=[Bp], writes=[Bvm])
    for h in range(4):
        for tt in range(4):
            ps, Bp = banks.get()
            tsl = slice(tt * 512, (tt + 1) * 512)
            for c in range(8):
                P.pe(lambda e, c=c, h=h, ps=ps, tsl=tsl: e.matmul(ps[0:64, :], lhsT=wqm[:, c, h * 64:(h + 1) * 64],
                                                                  rhs=self.xT_bf[:, c, tsl], start=(c == 0),
                                                                  stop=(c == 7)),
                     reads=[Bqm] + self.BxT[tt * 4:tt * 4 + 4], writes=[Bp])
            P.act(lambda e, h=h, ps=ps, tsl=tsl: e.copy(out=qmT[:, h, tsl], in_=ps[0:64, :]), reads=[Bp], writes=[Bq])
    pT = [P.sbuf("pT", [128, 512], BF16) for _ in range(2)]
    BpT = [Buf(), Buf()]
    rec = P.sbuf("mrec", [64, 512], F32)
    Brec = Buf()
    for h in range(4):
        for tt in range(4):
            tsl = slice(tt * 512, (tt + 1) * 512)
            for mc in range(2):
                ps, Bp = banks.get()
                P.pe(lambda e, h=h, mc=mc, ps=ps, tsl=tsl: e.matmul(ps[:, :], lhsT=kmT[:, h, mc * 128:(mc + 1) * 128],
                                                                    rhs=qmT[:, h, tsl], start=True, stop=True),
                     reads=[Bkm, Bq], writes=[Bp])
                P.act(lambda e, mc=mc, ps=ps: e.activation(out=pT[mc][:], in_=ps[:, :], func=AF.Exp, scale=0.125),
                      reads=[Bp], writes=[BpT[mc]])
            po, Bpo = banks.get()
            for mc in range(2):
                P.pe(lambda e, h=h, mc=mc, po=po: e.matmul(po[:, :], lhsT=vma[:, mc, h, :], rhs=pT[mc][:],
                                                           start=(mc == 0), stop=(mc == 1)),
                     reads=[Bvm, BpT[mc]], writes=[Bpo])
            P.dve(lambda e, po=po: e.reciprocal(out=rec[:], in_=po[64:128, :]), reads=[Bpo], writes=[Brec])
            hp, ch = h % 2, 6 + h // 2
            P.dve(lambda e, po=po, hp=hp, ch=ch, tsl=tsl: e.tensor_tensor(
                out=catT[hp * 64:hp * 64 + 64, ch, tsl], in0=po[0:64, :], in1=rec[:], op=ALU.mult),
                reads=[Bpo, Brec], writes=[BcatT])


def k_xphase(self, li, b, banks, catT, BcatT, xsrc, Bxsrc):
    P, I = self.P, self.I
    wo = P.sbuf("wo", [128, 8, D], BF16)
    Bwo = Buf()
    P.dma("pool", wo[:], I["w_out"][li].rearrange("(c p) f -> p c f", p=128), writes=[Bwo])
    g1, Bg1 = self.bcast_row("g1", I["ln1_g"][li:li + 1, :], D)
    b1, Bb1 = self.bcast_row("b1", I["ln1_b"][li:li + 1, :], D)
    st2 = [self.ln_scratch("ln1a"), self.ln_scratch("ln1b")]
    self.router_setup(li)
    xin = [P.sbuf("xin1", [128, D], F32) for _ in range(2)]
    Bxin = [Buf(), Buf()]
    x1 = [P.sbuf("x1t", [128, D], F32) for _ in range(2)]
    Bx1 = [Buf(), Buf()]
    for i in range(NT):
        p = i % 2
        tsl = slice(i * 128, (i + 1) * 128)
        P.dma("sp", xin[p][:], xsrc[tsl, :], reads=[Bxsrc[i]], writes=[Bxin[p]])
        for half in range(2):
            ps, Bp = banks.get()
            for c in range(8):
                P.pe(lambda e, c=c, ps=ps, half=half, tsl=tsl: e.matmul(
                    ps[:, :], lhsT=catT[:, c, tsl], rhs=wo[:, c, half * 512:(half + 1) * 512],
                    start=(c == 0), stop=(c == 7)), reads=[BcatT, Bwo], writes=[Bp])
            hs = slice(half * 512, (half + 1) * 512)
            P.dve(lambda e, p=p, ps=ps, hs=hs: e.scalar_tensor_tensor(
                out=xin[p][:, hs], in0=xin[p][:, hs], scalar=ALPHA, in1=ps[:, :], op0=ALU.mult, op1=ALU.add),
                reads=[Bxin[p], Bp], writes=[Bxin[p]])
        st, Bst = st2[p]
        self.layernorm_tile(xin[p], Bxin[p], x1[p], Bx1[p], g1, Bg1, b1, Bb1, st, Bst)
        P.dma("sp", self.x1s[tsl, :], x1[p][:], reads=[Bx1[p]], writes=[self.Bx1s[i]])
        if self.dbg is not None:
            P.dma("sp", self.dbg[tsl, :], x1[p][:], reads=[Bx1[p]])
        self.router_tile(i, x1[p], Bx1[p])


def k_gla(self, b, banks, catT, BcatT):
    P, I = self.P, self.I
    W = I["gla_w_in"][0]
    w = P.sbuf("glaw", [128, 8, 2320], BF16)
    Bw = Buf()
    Bws = [Bw, Buf()]
    for hh in range(2):
        P.dma("pool", w[:, :, hh * 1160:(hh + 1) * 1160],
              W[:, hh * 1160:(hh + 1) * 1160].rearrange("(c p) f -> p c f", p=128), writes=[Bws[hh]])
    Bk = Buf()
    Lneg = P.sbuf("Lneg", [128, 128], F32)
    Uneg = P.sbuf("Uneg", [128, 128], F32)
    MG4 = P.sbuf("MG4", [128, 4, 128], F32)
    ones1 = P.sbuf("ones1", [1, 128], F32)
    P.pool(lambda e: e.memset(Lneg[:], -1.0 / 16.0), writes=[Bk])
    P.pool(lambda e: e.affine_select(out=Lneg[:], in_=Lneg[:], compare_op=ALU.is_ge, fill=0.0, base=0,
                                     pattern=[[1, 128]], channel_multiplier=-1), reads=[Bk], writes=[Bk])
    P.pool(lambda e: e.memset(Lneg[0:64, 64:128], 0.0), reads=[Bk], writes=[Bk])
    P.pool(lambda e: e.memset(Uneg[:], -1.0 / 16.0), reads=[Bk], writes=[Bk])
    P.pool(lambda e: e.affine_select(out=Uneg[:], in_=Uneg[:], compare_op=ALU.is_ge, fill=0.0, base=-1,
                                     pattern=[[-1, 128]], channel_multiplier=1), reads=[Bk], writes=[Bk])
    P.pool(lambda e: e.memset(Uneg[64:128, 0:64], 0.0), reads=[Bk], writes=[Bk])
    for h in range(4):
        P.pool(lambda e, h=h: e.memset(MG4[:, h, :], 1.0), reads=[Bk], writes=[Bk])
        P.pool(lambda e, h=h: e.affine_select(out=MG4[:, h, :], in_=MG4[:, h, :], compare_op=ALU.is_ge, fill=0.0,
                                              base=0, pattern=[[1, 128]], channel_multiplier=-1),
               reads=[Bk], writes=[Bk])
        P.pool(lambda e, h=h: e.memset(MG4[0:64, h, 64:128], 0.0), reads=[Bk], writes=[Bk])
    P.pool(lambda e: e.memset(ones1[:], 1.0), reads=[Bk], writes=[Bk])
    wg = P.sbuf("wg", [16, 384], F32)
    bg = P.sbuf("bg", [1, 384], F32)
    P.dma("sp", wg[:], I["gla_w_gate"][0], writes=[Bk])
    P.dma("sp", bg[:], I["gla_b_gate"][0:1, :], writes=[Bk])
    ng4 = P.sbuf("ng4", [128, 768], F32)
    for h in range(4):
        P.dma("sp", ng4[:, h * 192:(h + 1) * 192], I["gla_norm_g"][0:1, :].partition_broadcast(128), writes=[Bk])
    Sf = P.sbuf("Sf", [96, 4, 192], F32)
    Sb = P.sbuf("Sb", [96, 4, 192], BF16)
    Sb2 = P.sbuf("Sb2", [96, 4, 192], BF16)
    BS = [Buf() for _ in range(4)]
    BSb = [Buf() for _ in range(4)]
    BSb2 = [Buf() for _ in range(4)]
    P.dve(lambda e: e.memset(Sf[:], 0.0), writes=BS)
    P.dve(lambda e: e.memset(Sb[:], 0.0), writes=BSb)
    P.dve(lambda e: e.memset(Sb2[:], 0.0), writes=BSb2)
    def mkset():
        qeA = P.sbuf("qeA", [96, 4, 128], BF16)
        qeB = P.sbuf("qeB", [96, 4, 128], BF16)
        BqA, BqB = Buf(), Buf()
        P.dve(lambda e: e.memset(qeA[:], 0.0), writes=[BqA])
        P.dve(lambda e: e.memset(qeB[:], 0.0), writes=[BqB])
        tiles = (qeA, qeB, BqA, BqB,
                 P.sbuf("a1T", [16, 128], F32), P.sbuf("spl", [128, 384], F32), P.sbuf("eb", [96, 512], F32),
                 P.sbuf("enb", [96, 512], F32), P.sbuf("erb", [128, 384], F32), P.sbuf("qe", [96, 512], BF16),
                 P.sbuf("ke", [96, 512], BF16), P.sbuf("kdec", [128, 384], BF16), P.sbuf("vb", [128, 768], BF16),
                 P.sbuf("sr", [128, 768], F32), P.sbuf("at", [128, 4, 128], BF16), P.sbuf("sq", [128, 192], F32),
                 P.sbuf("ss", [128, 8], F32), P.sbuf("on", [128, 768], F32), P.sbuf("onb", [128, 768], BF16))
        return tiles + tuple(Buf() for _ in range(14))

    sets = [mkset(), mkset()]
    xT = self.xT_bf
    for i in range(NT):
        tsl = slice(i * 128, (i + 1) * 128)
        Bx = [self.BxT[i]]
        (qeA, qeB, BqA, BqB, a1T, sp_, eb, enb, erb, qe, ke, kdec, vb, sr, at, sq, ss, on, onb,
         Ba1, Bsp, Beb, Benb, Berb, Bqe, Bke, Bkd, Bvb, Bsr, Bat, Bsq, Bss, Bon) = sets[i % 2]

        def proj_tok(ps, n0, n1, width):
            for c in range(8):
                P.pe(lambda e, c=c: e.matmul(ps[:, 0:width], lhsT=xT[:, c, tsl], rhs=w[:, c, n0:n1],
                                             start=(c == 0), stop=(c == 7)), reads=Bws + Bx, writes=[None])

        pa, Bpa = banks.get()
        for c in range(8):
            P.pe(lambda e, c=c, pa=pa: e.matmul(pa[0:16, 0:128], lhsT=w[:, c, 2304:2320], rhs=xT[:, c, tsl],
                                                start=(c == 0), stop=(c == 7)), reads=Bws + Bx, writes=[Bpa])
        P.act(lambda e, pa=pa: e.copy(out=a1T[:], in_=pa[0:16, 0:128]), reads=[Bpa], writes=[Ba1])
        pz, Bpz = banks.get()
        P.pe(lambda e, pz=pz: e.matmul(pz[:, 0:384], lhsT=a1T[:], rhs=wg[:], start=True, stop=False),
             reads=[Ba1, Bk], writes=[Bpz])
        P.pe(lambda e, pz=pz: e.matmul(pz[:, 0:384], lhsT=ones1[:], rhs=bg[:], start=False, stop=True),
             reads=[Bk], writes=[Bpz])
        P.act(lambda e, pz=pz: e.activation(out=sp_[:], in_=pz[:, 0:384], func=AF.Exp, scale=-1.0),
              reads=[Bpz], writes=[Bsp])
        P.act(lambda e: e.activation(out=sp_[:], in_=sp_[:], func=AF.Ln, bias=1.0, scale=1.0),
              reads=[Bsp], writes=[Bsp])
        pb_, Bpb = banks.get()
        for h in range(4):
            P.pe(lambda e, h=h, pb_=pb_: e.matmul(pb_[0:96, h * 128:(h + 1) * 128], lhsT=sp_[:, h * 96:(h + 1) * 96],
                                                  rhs=Lneg[:], start=True, stop=True), reads=[Bsp, Bk], writes=[Bpb])
        P.act(lambda e, pb_=pb_: e.activation(out=eb[:], in_=pb_[0:96, :], func=AF.Exp), reads=[Bpb], writes=[Beb])
        P.act(lambda e, pb_=pb_: e.activation(out=enb[:], in_=pb_[0:96, :], func=AF.Exp, scale=-1.0),
              reads=[Bpb], writes=[Benb])
        prb, Bprb = banks.get()
        P.pe(lambda e, prb=prb: e.matmul(prb[:, 0:384], lhsT=Uneg[:], rhs=sp_[:], start=True, stop=True),
             reads=[Bsp, Bk], writes=[Bprb])
        P.act(lambda e, prb=prb: e.activation(out=erb[:], in_=prb[:, 0:384], func=AF.Exp), reads=[Bprb], writes=[Berb])
        pq, Bpq = banks.get()
        for h in range(4):
            for c in range(8):
                P.pe(lambda e, c=c, h=h, pq=pq: e.matmul(pq[0:96, h * 128:(h + 1) * 128], lhsT=w[:, c, h * 96:(h + 1) * 96],
                                                         rhs=xT[:, c, tsl], start=(c == 0), stop=(c == 7)),
                     reads=Bws + Bx, writes=[Bpq])
        P.dve(lambda e, pq=pq: e.scalar_tensor_tensor(out=qe[:], in0=pq[0:96, :], scalar=96.0 ** -0.5, in1=eb[:],
                                                      op0=ALU.mult, op1=ALU.mult), reads=[Bpq, Beb], writes=[Bqe])
        P.dve(lambda e: e.tensor_copy(out=qeA[:, :, 0:64], in_=qe[:].rearrange("p (h t) -> p h t", h=4)[:, :, 0:64]),
              reads=[Bqe], writes=[BqA])
        P.dve(lambda e: e.tensor_copy(out=qeB[:, :, 64:128], in_=qe[:].rearrange("p (h t) -> p h t", h=4)[:, :, 64:128]),
              reads=[Bqe], writes=[BqB])
        pk, Bpk = banks.get()
        for h in range(4):
            for c in range(8):
                P.pe(lambda e, c=c, h=h, pk=pk: e.matmul(pk[0:96, h * 128:(h + 1) * 128],
                                                         lhsT=w[:, c, 384 + h * 96:384 + (h + 1) * 96],
                                                         rhs=xT[:, c, tsl], start=(c == 0), stop=(c == 7)),
                     reads=Bws + Bx, writes=[Bpk])
        P.dve(lambda e, pk=pk: e.tensor_tensor(out=ke[:], in0=pk[0:96, :], in1=enb[:], op=ALU.mult),
              reads=[Bpk, Benb], writes=[Bke])
        pkt, Bpkt = banks.get()
        for c in range(8):
            P.pe(lambda e, c=c, pkt=pkt: e.matmul(pkt[:, 0:384], lhsT=xT[:, c, tsl], rhs=w[:, c, 384:768],
                                                  start=(c == 0), stop=(c == 7)), reads=Bws + Bx, writes=[Bpkt])
        P.dve(lambda e, pkt=pkt: e.tensor_tensor(out=kdec[:], in0=pkt[:, 0:384], in1=erb[:], op=ALU.mult),
              reads=[Bpkt, Berb], writes=[Bkd])
        for part in range(2):
            pv, Bpv = banks.get()
            for c in range(8):
                P.pe(lambda e, c=c, pv=pv, part=part: e.matmul(pv[:, 0:384], lhsT=xT[:, c, tsl],
                                                               rhs=w[:, c, 768 + part * 384:768 + (part + 1) * 384],
                                                               start=(c == 0), stop=(c == 7)),
                     reads=Bws + Bx, writes=[Bpv])
            P.act(lambda e, pv=pv, part=part: e.copy(out=vb[:, part * 384:(part + 1) * 384], in_=pv[:, 0:384]),
                  reads=[Bpv], writes=[Bvb])
        for part in range(2):
            pr, Bpr = banks.get()
            for c in range(8):
                P.pe(lambda e, c=c, pr=pr, part=part: e.matmul(pr[:, 0:384], lhsT=xT[:, c, tsl],
                                                               rhs=w[:, c, 1536 + part * 384:1536 + (part + 1) * 384],
                                                               start=(c == 0), stop=(c == 7)),
                     reads=Bws + Bx, writes=[Bpr])
            P.act(lambda e, pr=pr, part=part: e.activation(out=sr[:, part * 384:(part + 1) * 384], in_=pr[:, 0:384],
                                                           func=AF.Silu), reads=[Bpr], writes=[Bsr])
        pat, Bpat = banks.get()
        for h in range(4):
            P.pe(lambda e, h=h, pat=pat: e.matmul(pat[:, h * 128:(h + 1) * 128], lhsT=ke[:, h * 128:(h + 1) * 128],
                                                  rhs=qe[:, h * 128:(h + 1) * 128], start=True, stop=True),
                 reads=[Bke, Bqe], writes=[Bpat])
        P.dve(lambda e, pat=pat: e.tensor_tensor(out=at[:].rearrange("p h t -> p (h t)"), in0=pat[:, :],
                                                 in1=MG4[:].rearrange("p h t -> p (h t)"), op=ALU.mult),
              reads=[Bpat, Bk], writes=[Bat])
        po2 = [banks.fixed(0), banks.fixed(1)]
        for h in range(4):
            po, Bpo = po2[h // 2]
            osl = slice((h % 2) * 192, (h % 2) * 192 + 192)
            vsl = slice(h * 192, (h + 1) * 192)
            ksl = slice(h * 96, (h + 1) * 96)
            P.pe(lambda e, h=h, po=po, osl=osl, vsl=vsl: e.matmul(po[:, osl], lhsT=at[:, h, :], rhs=vb[:, vsl],
                                                                   start=True, stop=False),
                 reads=[Bat, Bvb], writes=[Bpo])
            P.pe(lambda e, h=h, po=po, osl=osl: e.matmul(po[:, osl], lhsT=qeA[:, h, :], rhs=Sb[:, h, :],
                                                          start=False, stop=False), reads=[BqA, BSb[h]], writes=[Bpo])
            pi1, Bpi1 = banks.get()
            P.pe(lambda e, pi1=pi1, ksl=ksl, vsl=vsl: e.matmul(pi1[0:96, 0:192], lhsT=kdec[0:64, ksl], rhs=vb[0:64, vsl],
                                                               start=True, stop=True), reads=[Bkd, Bvb], writes=[Bpi1])
            P.dve(lambda e, h=h, pi1=pi1: e.scalar_tensor_tensor(out=Sf[:, h, :], in0=Sf[:, h, :],
                                                                 scalar=eb[:, h * 128 + 63:h * 128 + 64],
                                                                 in1=pi1[0:96, 0:192], op0=ALU.mult, op1=ALU.add),
                  reads=[BS[h], Beb, Bpi1], writes=[BS[h]])
            P.act(lambda e, h=h: e.copy(out=Sb2[:, h, :], in_=Sf[:, h, :]), reads=[BS[h]], writes=[BSb2[h]])
            P.pe(lambda e, h=h, po=po, osl=osl: e.matmul(po[:, osl], lhsT=qeB[:, h, :], rhs=Sb2[:, h, :],
                                                          start=False, stop=True), reads=[BqB, BSb2[h]], writes=[Bpo])
            pi2, Bpi2 = banks.get()
            P.pe(lambda e, pi2=pi2, ksl=ksl, vsl=vsl: e.matmul(pi2[0:96, 0:192], lhsT=kdec[64:128, ksl],
                                                               rhs=vb[64:128, vsl], start=True, stop=True),
                 reads=[Bkd, Bvb], writes=[Bpi2])
            P.dve(lambda e, h=h, pi2=pi2: e.scalar_tensor_tensor(out=Sf[:, h, :], in0=Sf[:, h, :],
                                                                 scalar=eb[:, h * 128 + 127:h * 128 + 128],
                                                                 in1=pi2[0:96, 0:192], op0=ALU.mult, op1=ALU.add),
                  reads=[BS[h], Beb, Bpi2], writes=[BS[h]])
            P.act(lambda e, h=h: e.copy(out=Sb[:, h, :], in_=Sf[:, h, :]), reads=[BS[h]], writes=[BSb[h]])
            P.act(lambda e, po=po, osl=osl: e.activation(out=sq[:], in_=po[:, osl], func=AF.Square),
                  reads=[Bpo], writes=[Bsq])
            P.dve(lambda e, h=h: e.reduce_sum(out=ss[:, h:h + 1], in_=sq[:], axis=AX.X), reads=[Bsq], writes=[Bss])
        P.act(lambda e: e.activation(out=ss[:, 4:8], in_=ss[:, 0:4], func=AF.Ln, bias=RMS_EPS, scale=1.0 / 192.0),
              reads=[Bss], writes=[Bss], cost=0.2)
        P.act(lambda e: e.activation(out=ss[:, 4:8], in_=ss[:, 4:8], func=AF.Exp, scale=-0.5),
              reads=[Bss], writes=[Bss], cost=0.2)
        for h in range(4):
            po, Bpo = po2[h // 2]
            osl = slice((h % 2) * 192, (h % 2) * 192 + 192)
            vsl = slice(h * 192, (h + 1) * 192)
            P.dve(lambda e, h=h, po=po, osl=osl, vsl=vsl: e.scalar_tensor_tensor(
                out=on[:, vsl], in0=po[:, osl], scalar=ss[:, 4 + h:5 + h], in1=ng4[:, vsl],
                op0=ALU.mult, op1=ALU.mult), reads=[Bpo, Bss, Bk], writes=[Bon])
        Bonb = Bsq
        P.dve(lambda e: e.tensor_tensor(out=onb[:], in0=on[:], in1=sr[:], op=ALU.mult), reads=[Bon, Bsr], writes=[Bonb])
        psT, BpsT = self.psT[0], self.BpsT[0]
        psTb = psT[:].bitcast(BF16)
        for c in range(6):
            P.pe(lambda e, c=c: e.transpose(out=psTb[:, c * 128:(c + 1) * 128], in_=onb[:, c * 128:(c + 1) * 128],
                                            identity=self.identb[:]), reads=[Bonb, self.Bc], writes=[BpsT])
        P.dve(lambda e: e.tensor_copy(out=catT[:, 0:6, tsl], in_=psTb[:, 0:768].rearrange("p (c t) -> p c t", c=6)),
              reads=[BpsT], writes=[BcatT])


K.load_xT = k_load_xT
K.mem_attn = k_mem_attn
K.xphase = k_xphase
K.gla = k_gla


def k_rope_tables(self, b):
    P, I = self.P, self.I
    B = Buf()
    cosT = P.sbuf("cosT", [128, NT, 32], F32)
    sinT = P.sbuf("sinT", [128, NT, 32], F32)
    with P.scope():
        self._rope_tables_body(b, cosT, sinT, B)
    return cosT, sinT, B


def k_rope_tables_body(self, b, cosT, sinT, B):
    P, I = self.P, self.I
    pi = P.sbuf("posi", [128, NT], I32)
    pf = P.sbuf("posf", [128, NT], F32)
    io = P.sbuf("iot", [128, 32], I32)
    inv = P.sbuf("inv", [128, 32], F32)
    ang = P.sbuf("ang", [128, NT, 32], F32)
    t1 = P.sbuf("rt1", [128, NT, 32], F32)
    P.dma("sp", pi[:], I["pos"][b], writes=[B])
    P.dve(lambda e: e.tensor_copy(out=pf[:], in_=pi[:]), reads=[B], writes=[B])
    P.pool(lambda e: e.iota(io[:], pattern=[[1, 32]], base=0, channel_multiplier=0), reads=[B], writes=[B])
    P.dve(lambda e: e.tensor_copy(out=inv[:], in_=io[:]), reads=[B], writes=[B])
    P.act(lambda e: e.activation(out=inv[:], in_=inv[:], func=AF.Exp, scale=-math.log(10000.0) / 32.0),
          reads=[B], writes=[B])
    for c in range(NT):
        P.dve(lambda e, c=c: e.tensor_scalar(out=ang[:, c, :], in0=inv[:], scalar1=pf[:, c:c + 1], scalar2=None,
                                             op0=ALU.mult), reads=[B], writes=[B])
    MAGIC = 12582912.0
    TWO_PI = 2.0 * math.pi
    for which, dst in ((0, sinT), (1, cosT)):
        src = ang
        if which == 1:
            P.dve(lambda e: e.tensor_scalar(out=ang[:], in0=ang[:], scalar1=math.pi / 2.0, scalar2=None, op0=ALU.add),
                  reads=[B], writes=[B])
        P.dve(lambda e: e.tensor_scalar(out=t1[:], in0=ang[:], scalar1=1.0 / TWO_PI, scalar2=MAGIC,
                                        op0=ALU.mult, op1=ALU.add), reads=[B], writes=[B])
        P.dve(lambda e: e.tensor_scalar(out=t1[:], in0=t1[:], scalar1=-MAGIC, scalar2=None, op0=ALU.add),
              reads=[B], writes=[B])
        P.dve(lambda e: e.scalar_tensor_tensor(out=t1[:], in0=t1[:], scalar=-TWO_PI, in1=ang[:],
                                               op0=ALU.mult, op1=ALU.add), reads=[B], writes=[B])
        P.act(lambda e, dst=dst: e.activation(out=dst[:], in_=t1[:], func=AF.Sin, scale=0.999999),
              reads=[B], writes=[B])


def k_rope(self, src, Bsrc, dst, Bdst, nh, cosT, sinT, Btab, i, tmp, Btmp):
    P = self.P
    s3 = src.rearrange("p (h d) -> p h d", h=nh)
    d3 = dst.rearrange("p (h d) -> p h d", h=nh)
    cb = cosT[:, i, :].unsqueeze(1).to_broadcast([128, nh, 32])
    sb = sinT[:, i, :].unsqueeze(1).to_broadcast([128, nh, 32])
    ta = tmp[:, 0:nh * 32].rearrange("p (h d) -> p h d", h=nh)
    tb = tmp[:, 512:512 + nh * 32].rearrange("p (h d) -> p h d", h=nh)
    x1, x2 = s3[:, :, 0:32], s3[:, :, 32:64]
    P.dve(lambda e: e.tensor_tensor(out=ta, in0=x1, in1=cb, op=ALU.mult), reads=[Bsrc, Btab], writes=[Btmp])
    P.dve(lambda e: e.tensor_tensor(out=tb, in0=x2, in1=sb, op=ALU.mult), reads=[Bsrc, Btab], writes=[Btmp])
    P.dve(lambda e: e.tensor_tensor(out=d3[:, :, 0:32], in0=ta, in1=tb, op=ALU.subtract), reads=[Btmp], writes=[Bdst])
    P.dve(lambda e: e.tensor_tensor(out=ta, in0=x2, in1=cb, op=ALU.mult), reads=[Bsrc, Btab, Bdst], writes=[Btmp])
    P.dve(lambda e: e.tensor_tensor(out=tb, in0=x1, in1=sb, op=ALU.mult), reads=[Bsrc, Btab], writes=[Btmp])
    P.dve(lambda e: e.tensor_tensor(out=d3[:, :, 32:64], in0=ta, in1=tb, op=ALU.add), reads=[Btmp], writes=[Bdst])


def k_dsa(self, b, banks, catT, BcatT):
    P, I = self.P, self.I
    W = I["dsa_w_in"][0]
    xT = self.xT_bf
    psT, BpsT = self.psT[0], self.BpsT[0]
    ident = self.ident
    cosT, sinT, Btab = self.rope_tables(b)
    tmp = P.sbuf("ropetmp", [128, 1024], F32)
    Btmp = Buf()
    maskT = [P.sbuf("maskT", [128, (NT - j) * 128], BF16) for j in range(NT)]
    BmT = [Buf() for _ in range(NT)]
    negm = P.sbuf("negm", [128, 128], F32)
    Bk = Buf()
    P.pool(lambda e: e.memset(negm[:], 0.0), writes=[Bk])
    P.pool(lambda e: e.affine_select(out=negm[:], in_=negm[:], compare_op=ALU.is_ge, fill=NEG, base=0,
                                     pattern=[[-1, 128]], channel_multiplier=1), reads=[Bk], writes=[Bk])
    with P.scope():
        wi_ = P.sbuf("dsw1", [128, 8, 584], BF16)
        Bw = Buf()
        P.dma("pool", wi_[:], W[:, 2304:2888].rearrange("(c p) f -> p c f", p=128), writes=[Bw])
        kg, Bkg = self.bcast_row("kg", I["dsa_idx_k_g"][0:1, :], 64)
        kb, Bkb = self.bcast_row("kb", I["dsa_idx_k_b"][0:1, :], 64)
        qiT = P.sbuf("qiT", [128, 4, S], BF16)
        kiT = P.sbuf("kiT", [128, S], BF16)
        wis = P.sbuf("wis", [128, NT, 8], F32)
        BqiT, BkiT, Bwis = Buf(), Buf(), Buf()
        qiR = P.sbuf("qiR", [128, 512], F32)
        kiN = P.sbuf("kiN", [128, 64], F32)
        kiR = P.sbuf("kiR", [128, 128], F32)
        BqiR, BkiN, BkiR = Buf(), Buf(), Buf()
        st, Bst = self.ln_scratch("kln")
        stats, mv, rstd, nmr = st
        for i in range(NT):
            tsl = slice(i * 128, (i + 1) * 128)
            Bx = [self.BxT[i]]
            pq, Bpq = banks.get()
            for c in range(8):
                P.pe(lambda e, c=c, pq=pq: e.matmul(pq[:, 0:512], lhsT=xT[:, c, tsl], rhs=wi_[:, c, 0:512],
                                                    start=(c == 0), stop=(c == 7)), reads=[Bw] + Bx, writes=[Bpq])
            pk, Bpk = banks.get()
            for c in range(8):
                P.pe(lambda e, c=c, pk=pk: e.matmul(pk[:, 0:72], lhsT=xT[:, c, tsl], rhs=wi_[:, c, 512:584],
                                                    start=(c == 0), stop=(c == 7)), reads=[Bw] + Bx, writes=[Bpk])
            self.rope(pq[:, 0:512], Bpq, qiR[:, :], BqiR, 8, cosT, sinT, Btab, i, tmp, Btmp)
            P.dve(lambda e, pk=pk, i=i: e.tensor_scalar(out=wis[:, i, :], in0=pk[:, 64:72],
                                                        scalar1=(8.0 ** -0.5) * (64.0 ** -0.5), scalar2=None, op0=ALU.mult),
                  reads=[Bpk], writes=[Bwis])
            P.dve(lambda e, pk=pk: e.bn_stats(out=stats[:, 0:6], in_=pk[:, 0:64]), reads=[Bpk], writes=[Bst])
            P.dve(lambda e: e.bn_aggr(out=mv[:], in_=stats[:, 0:6]), reads=[Bst], writes=[Bst])
            P.act(lambda e: e.activation(out=rstd[:], in_=mv[:, 1:2], func=AF.Ln, bias=LN_EPS, scale=1.0),
                  reads=[Bst], writes=[Bst], cost=0.2)
            P.act(lambda e: e.activation(out=rstd[:], in_=rstd[:], func=AF.Exp, scale=-0.5),
                  reads=[Bst], writes=[Bst], cost=0.2)
            P.dve(lambda e, pk=pk: e.tensor_scalar(out=kiN[:], in0=pk[:, 0:64], scalar1=mv[:, 0:1], scalar2=rstd[:],
                                                   op0=ALU.subtract, op1=ALU.mult), reads=[Bpk, Bst], writes=[BkiN])
            P.dve(lambda e: e.tensor_tensor(out=kiN[:], in0=kiN[:], in1=kg[:], op=ALU.mult), reads=[BkiN, Bkg], writes=[BkiN])
            P.dve(lambda e: e.tensor_tensor(out=kiN[:], in0=kiN[:], in1=kb[:], op=ALU.add), reads=[BkiN, Bkb], writes=[BkiN])
            self.rope(kiN[:, :], BkiN, kiR[:, 0:64], BkiR, 1, cosT, sinT, Btab, i, tmp, Btmp)
            P.dve(lambda e: e.tensor_copy(out=kiR[:, 64:128], in_=kiR[:, 0:64]), reads=[BkiR], writes=[BkiR])
            for c in range(4):
                P.pe(lambda e, c=c: e.transpose(out=psT[:, c * 128:(c + 1) * 128], in_=qiR[:, c * 128:(c + 1) * 128],
                                                identity=ident[:]), reads=[BqiR, self.Bc], writes=[BpsT])
            P.pe(lambda e: e.transpose(out=psT[:, 512:640], in_=kiR[:, :], identity=ident[:]),
                 reads=[BkiR, self.Bc], writes=[BpsT])
            P.act(lambda e, tsl=tsl: e.copy(out=qiT[:, :, tsl], in_=psT[:, 0:512].rearrange("p (c t) -> p c t", c=4)),
                  reads=[BpsT], writes=[BqiT])
            P.act(lambda e, tsl=tsl: e.copy(out=kiT[:, tsl], in_=psT[:, 512:640]), reads=[BpsT], writes=[BkiT])
        acc2 = [P.sbuf("acc", [128, S], F32) for _ in range(4)]
        Bacc2 = [Buf() for _ in range(4)]
        junk = P.sbuf("junk", [128, S], BF16)
        Bjunk = Buf()
        mk = P.sbuf("mk", [128, S], F32)
        Bmk = Buf()
        bs2 = [P.sbuf("bs", [128, 8], F32) for _ in range(2)]
        Bbs2 = [Buf(), Buf()]
        rrc = [0]
        NIT = 24

        dg2 = [P.sbuf("dg", [128, 8, 128], F32) for _ in range(2)]
        Bdg2 = [Buf(), Buf()]
        rl = [P.sbuf("rl", [128, 512], F32) for _ in range(4)]
        Brl = [Buf() for _ in range(4)]

        def scores(i, slot):
            acc, Bacc = acc2[slot], Bacc2[slot]
            dg, Bdg = dg2[slot % 2], Bdg2[slot % 2]
            Wd = (i + 1) * 128
            tsl = slice(i * 128, (i + 1) * 128)
            npc = (Wd + 511) // 512
            for h in range(8):
                P.dve(lambda e: e.tensor_scalar(out=dg[:, h, :], in0=ident[:], scalar1=wis[:, i, h:h + 1], scalar2=None,
                                                op0=ALU.mult), reads=[self.Bc, Bwis], writes=[Bdg], cost=0.15)
            for n in range(npc):
                c0, c1 = n * 512, min(Wd, (n + 1) * 512)
                pacc, Bpacc = banks.fixed(n % 2)
                for h in range(8):
                    hp, hc = (h % 2) * 64, h // 2
                    ps, Bp = banks.get()
                    P.pe(lambda e: e.matmul(ps[:, 0:c1 - c0], lhsT=qiT[hp:hp + 64, hc, tsl], rhs=kiT[hp:hp + 64, c0:c1],
                                            start=True, stop=True), reads=[BqiT, BkiT], writes=[Bp])
                    r_ = rrc[0]
                    rrc[0] = (r_ + 1) % 4
                    P.act(lambda e: e.activation(out=rl[r_][:, 0:c1 - c0], in_=ps[:, 0:c1 - c0], func=AF.Relu),
                          reads=[Bp], writes=[Brl[r_]])
                    last = (h == 7) and (n != npc - 1)
                    P.pe(lambda e: e.matmul(pacc[:, 0:c1 - c0], lhsT=dg[:, h, :], rhs=rl[r_][:, 0:c1 - c0],
                                            start=(h == 0), stop=last), reads=[Bdg, Brl[r_]], writes=[Bpacc], cost=0.9)
                if n == npc - 1:
                    off = i * 128 - c0
                    P.pe(lambda e: e.matmul(pacc[:, off:off + 128], lhsT=ident[:], rhs=negm[:], start=False, stop=True),
                         reads=[self.Bc, Bk], writes=[Bpacc], cost=0.4)
                P.act(lambda e: e.copy(out=acc[:, c0:c1], in_=pacc[:, 0:c1 - c0]), reads=[Bpacc], writes=[Bacc])

        def bis_init(i, slot):
            acc, Bacc, bs, Bbs = acc2[slot], Bacc2[slot], bs2[slot % 2], Bbs2[slot % 2]
            Wd = (i + 1) * 128
            P.dve(lambda e: e.reduce_max(out=bs[:, 5:6], in_=acc[:, 0:Wd], axis=AX.X), reads=[Bacc], writes=[Bbs])
            P.dve(lambda e: e.tensor_reduce(out=bs[:, 0:1], in_=acc[:, 0:i * 128], axis=AX.X, op=ALU.min),
                  reads=[Bacc], writes=[Bbs])
            P.dve(lambda e: e.tensor_tensor(out=bs[:, 1:2], in0=bs[:, 5:6], in1=bs[:, 0:1], op=ALU.subtract),
                  reads=[Bbs], writes=[Bbs])

        def bis_iter(i, slot, k):
            acc, Bacc, bs, Bbs = acc2[slot], Bacc2[slot], bs2[slot % 2], Bbs2[slot % 2]
            Wd = (i + 1) * 128
            ck = 2.0 ** -(k + 1)
            P.dve(lambda e: e.scalar_tensor_tensor(out=bs[:, 2:3], in0=bs[:, 1:2], scalar=ck, in1=bs[:, 0:1],
                                                   op0=ALU.mult, op1=ALU.add), reads=[Bbs], writes=[Bbs], cost=0.12)
            P.dve(lambda e: e.tensor_scalar(out=junk[:, 0:Wd], in0=acc[:, 0:Wd], scalar1=bs[:, 2:3], scalar2=0.0,
                                            op0=ALU.is_ge, op1=ALU.add, accum_out=bs[:, 3:4]),
                  reads=[Bacc, Bbs], writes=[Bbs, Bjunk], cost=Wd / 960.0 + 0.15)
            P.dve(lambda e: e.tensor_scalar(out=bs[:, 4:5], in0=bs[:, 3:4], scalar1=255.5, scalar2=ck,
                                            op0=ALU.is_ge, op1=ALU.mult), reads=[Bbs], writes=[Bbs], cost=0.12)
            P.dve(lambda e: e.scalar_tensor_tensor(out=bs[:, 0:1], in0=bs[:, 4:5], scalar=bs[:, 1:2], in1=bs[:, 0:1],
                                                   op0=ALU.mult, op1=ALU.add), reads=[Bbs], writes=[Bbs], cost=0.12)

        def finish_tile(i, slot, use_thr):
            acc, Bacc, bs, Bbs = acc2[slot], Bacc2[slot], bs2[slot % 2], Bbs2[slot % 2]
            Wd = (i + 1) * 128
            if use_thr:
                P.dve(lambda e: e.tensor_scalar(out=mk[:, 0:Wd], in0=acc[:, 0:Wd], scalar1=bs[:, 0:1], scalar2=None,
                                                op0=ALU.is_ge), reads=[Bacc, Bbs], writes=[Bmk])
            else:
                P.dve(lambda e: e.tensor_scalar(out=mk[:, 0:Wd], in0=acc[:, 0:Wd], scalar1=-1.0e29, scalar2=None,
                                                op0=ALU.is_ge), reads=[Bacc], writes=[Bmk])
            for j0 in range(0, i + 1, 8):
                js = list(range(j0, min(i + 1, j0 + 8)))
                for j in js:
                    P.pe(lambda e: e.transpose(out=psT[:, (j - j0) * 128:(j - j0 + 1) * 128],
                                               in_=mk[:, j * 128:(j + 1) * 128], identity=ident[:]),
                         reads=[Bmk, self.Bc], writes=[BpsT])
                for j in js:
                    P.act(lambda e: e.copy(out=maskT[j][:, (i - j) * 128:(i - j + 1) * 128],
                                           in_=psT[:, (j - j0) * 128:(j - j0 + 1) * 128]),
                          reads=[BpsT], writes=[BmT[j]])

        pairs = [(ia, ia + 1) for ia in range(2, NT, 2)]
        for i in range(2):
            scores(i, i)
        scores(pairs[0][0], 2)
        scores(pairs[0][1], 3)
        for i in range(2):
            finish_tile(i, i, False)
        for pi_, (ia, ib) in enumerate(pairs):
            sa, sb = (2, 3) if pi_ % 2 == 0 else (0, 1)
            if pi_ + 1 < len(pairs):
                na, nb = (0, 1) if pi_ % 2 == 0 else (2, 3)
                scores(pairs[pi_ + 1][0], na)
                scores(pairs[pi_ + 1][1], nb)
            bis_init(ia, sa)
            bis_init(ib, sb)
            for k in range(NIT):
                bis_iter(ia, sa, k)
                bis_iter(ib, sb, k)
            finish_tile(ia, sa, True)
            finish_tile(ib, sb, True)
    with P.scope():
        sets = []
        w_shared = P.sbuf("dsw2", [128, 8, 768], BF16)
        Bw_shared = Buf()
        for k_ in range(2):
            d = dict(w=w_shared, Bw=Bw_shared,
                     qT=P.sbuf("qT", [128, 2, S], BF16), kT=P.sbuf("kT", [128, 2, S], BF16),
                     va=P.sbuf("va", [128, NT, 4, 128], BF16),
                     BqT=[Buf() for _ in range(NT)], BkT=[Buf() for _ in range(NT)], Bva=[Buf() for _ in range(NT)],
                     qR=P.sbuf("qR", [128, 512], BF16), BqR=Buf())
            va_ = d["va"]
            P.dve(lambda e: e.memset(va_[:], 1.0), writes=d["Bva"])
            sets.append(d)
        tmp2 = [tmp, tmp]
        Btmp2 = [Btmp, Btmp]
        NR = 4
        LOOK = 3
        pe_ = [P.sbuf("pe_", [128, 512], BF16) for _ in range(NR)]
        pm_ = [P.sbuf("pm_", [128, 512], BF16) for _ in range(NR)]
        Bpe = [Buf() for _ in range(NR)]
        Bpm = [Buf() for _ in range(NR)]
        rec = [P.sbuf("arec", [64, 512], F32) for _ in range(2)]
        Brec = [Buf(), Buf()]
        blocks = [(h, tt, j) for h in range(4) for tt in range(4) for j in range(4 * tt + 4)]
        ropecnt = [0]

        def load_group(hg):
            d = sets[hg % 2]
            w2_ = d["w"]
            for part in range(3):
                P.dma("pool", w2_[:, :, part * 256:(part + 1) * 256],
                      W[:, part * 768 + hg * 256:part * 768 + (hg + 1) * 256].rearrange("(c p) f -> p c f", p=128),
                      writes=[d["Bw"]])

        def proj_tile(hg, i):
            d = sets[hg % 2]
            w2_, qT, kT, va, qR, BqR, Bw = d["w"], d["qT"], d["kT"], d["va"], d["qR"], d["BqR"], d["Bw"]
            tsl = slice(i * 128, (i + 1) * 128)
            Bx = [self.BxT[i]]
            for part in range(2):
                pq, Bpq = banks.get()
                for c in range(8):
                    P.pe(lambda e: e.matmul(pq[:, 0:256], lhsT=xT[:, c, tsl], rhs=w2_[:, c, part * 256:(part + 1) * 256],
                                            start=(c == 0), stop=(c == 7)), reads=[Bw] + Bx, writes=[Bpq])
                tk = ropecnt[0] % 2
                ropecnt[0] += 1
                self.rope(pq[:, 0:256], Bpq, qR[:, part * 256:(part + 1) * 256], BqR, 4, cosT, sinT, Btab, i,
                          tmp2[tk], Btmp2[tk])
            pv, Bpv = banks.get()
            for c in range(8):
                P.pe(lambda e: e.matmul(pv[:, 0:256], lhsT=xT[:, c, tsl], rhs=w2_[:, c, 512:768],
                                        start=(c == 0), stop=(c == 7)), reads=[Bw] + Bx, writes=[Bpv])
            P.act(lambda e: e.copy(out=va[:, i, :, 0:64], in_=pv[:, 0:256].rearrange("p (h d) -> p h d", h=4)),
                  reads=[Bpv], writes=[d["Bva"][i]])
            psTb = psT[:].bitcast(BF16)
            for c in range(4):
                P.pe(lambda e: e.transpose(out=psTb[:, c * 128:(c + 1) * 128], in_=qR[:, c * 128:(c + 1) * 128],
                                           identity=self.identb[:]), reads=[BqR, self.Bc], writes=[BpsT])
            P.act(lambda e: e.copy(out=qT[:, :, tsl], in_=psTb[:, 0:256].rearrange("p (c t) -> p c t", c=2)),
                  reads=[BpsT], writes=[d["BqT"][i]])
            P.act(lambda e: e.copy(out=kT[:, :, tsl], in_=psTb[:, 256:512].rearrange("p (c t) -> p c t", c=2)),
                  reads=[BpsT], writes=[d["BkT"][i]])

        def stage_a(hg, idx):
            d = sets[hg % 2]
            qT, kT = d["qT"], d["kT"]
            h, tt, j = blocks[idx]
            hp, hc = (h % 2) * 64, h // 2
            t0 = max(tt * 512, j * 128)
            c0 = t0 - tt * 512
            t1 = (tt + 1) * 512
            r_ = idx % NR
            ps, Bp = banks.get()
            P.pe(lambda e: e.matmul(ps[:, c0:512], lhsT=kT[hp:hp + 64, hc, j * 128:(j + 1) * 128],
                                    rhs=qT[hp:hp + 64, hc, t0:t1], start=True, stop=True),
                 reads=[d["BkT"][j]] + d["BqT"][tt * 4:tt * 4 + 4], writes=[Bp])
            P.act(lambda e: e.activation(out=pe_[r_][:, c0:512], in_=ps[:, c0:512], func=AF.Exp, scale=0.125),
                  reads=[Bp], writes=[Bpe[r_]])
            P.dve(lambda e: e.tensor_tensor(out=pm_[r_][:, c0:512], in0=pe_[r_][:, c0:512],
                                            in1=maskT[j][:, t0 - j * 128:t1 - j * 128], op=ALU.mult),
                  reads=[Bpe[r_], BmT[j]], writes=[Bpm[r_]])

        def stage_b(hg, idx):
            d = sets[hg % 2]
            va = d["va"]
            h, tt, j = blocks[idx]
            g = h * 4 + tt
            nblk = 4 * tt + 4
            t0 = max(tt * 512, j * 128)
            c0 = t0 - tt * 512
            r_ = idx % NR
            po, Bpo = banks.fixed(g % 2)
            P.pe(lambda e: e.matmul(po[:, c0:512], lhsT=va[:, j, h, :], rhs=pm_[r_][:, c0:512],
                                    start=(j == 0), stop=(j == nblk - 1)), reads=[d["Bva"][j], Bpm[r_]], writes=[Bpo])
            if j == nblk - 1:
                gh = hg * 4 + h
                ghp, gch = (gh % 2) * 64, gh // 2
                rc, Brc = rec[g % 2], Brec[g % 2]
                P.act(lambda e: e.activation(out=rc[:], in_=po[64:128, :], func=AF.Ln), reads=[Bpo], writes=[Brc])
                P.act(lambda e: e.activation(out=rc[:], in_=rc[:], func=AF.Exp, scale=-1.0), reads=[Brc], writes=[Brc])
                P.dve(lambda e: e.tensor_tensor(out=catT[ghp:ghp + 64, gch, tt * 512:(tt + 1) * 512],
                                                in0=po[0:64, :], in1=rc[:], op=ALU.mult),
                      reads=[Bpo, Brc], writes=[BcatT])

        load_group(0)
        for i in range(NT):
            proj_tile(0, i)
        for hg in range(3):
            nxt = hg + 1 if hg + 1 < 3 else None
            if nxt is not None:
                load_group(nxt)
            nsteps = len(blocks) + LOOK
            every = nsteps // NT
            pi_ = 0
            for idx in range(nsteps):
                if idx < len(blocks):
                    stage_a(hg, idx)
                if idx >= LOOK:
                    stage_b(hg, idx - LOOK)
                if nxt is not None and idx % every == every - 1 and pi_ < NT:
                    proj_tile(nxt, pi_)
                    pi_ += 1
            if nxt is not None:
                while pi_ < NT:
                    proj_tile(nxt, pi_)
                    pi_ += 1


def k_layer(self, li, b, first, last, after_experts=None):
    P, I = self.P, self.I
    with P.scope():
        catT = P.sbuf("catT", [128, 8, S], BF16)
        BcatT = Buf()
        banks = Banks(P, 6)
        with P.scope():
            if li % 2 == 0:
                self.dsa(b, banks, catT, BcatT)
                w_in, qm_off = I["dsa_w_in"][0], 2888
            else:
                self.gla(b, banks, catT, BcatT)
                w_in, qm_off = I["gla_w_in"][0], 2320
        with P.scope():
          self.mem_attn(li, b, banks, w_in, qm_off, catT, BcatT)
          if self.dbgc is not None:
            if True:
                stg = [P.sbuf("stg", [128, 512], F32) for _ in range(2)]
                Bstg = [Buf(), Buf()]
                kk = 0
                for c in range(8):
                    for tt in range(4):
                        p = kk % 2
                        kk += 1
                        P.dve(lambda e, p=p, c=c, tt=tt: e.tensor_copy(out=stg[p][:], in_=catT[:, c, tt * 512:(tt + 1) * 512]),
                              reads=[BcatT], writes=[Bstg[p]])
                        P.dma("sp", self.dbgc[c * 128:(c + 1) * 128, tt * 512:(tt + 1) * 512], stg[p][:], reads=[Bstg[p]])
          if first:
              xsrc, Bxsrc = I["x"][b], [Buf() for _ in range(NT)]
          else:
              xsrc, Bxsrc = self.x2s, self.Bx2s
          self.xphase(li, b, banks, catT, BcatT, xsrc, Bxsrc)
    self.moe(li, b, last, after_experts)


K.rope_tables = k_rope_tables
K._rope_tables_body = k_rope_tables_body
K.rope = k_rope
K.dsa = k_dsa
K.layer = k_layer


def build_full(nseq=NB_PER_CORE, layers=(0, 1)):
    k = K(nseq=nseq)
    k.consts()
    k.load_xT(0)
    for b in range(nseq):
        for li in layers:
            nxt = None
            if li == layers[-1] and b + 1 < nseq:
                nxt = (lambda bb=b + 1: k.load_xT(bb))
            k.layer(li, b, li == layers[0], li == layers[-1], nxt)
        k.P.barrier()
    k.P.finish()
    return k.nc


_NC_CACHE = {}


def kernel(**inputs):
    n = 8
    if "nc" not in _NC_CACHE:
        _NC_CACHE["nc"] = build_full()
    nc = _NC_CACHE["nc"]
    x = np.ascontiguousarray(inputs["x"], dtype=np.float32)
    mem = np.asarray(inputs["mem"], dtype=np.float32)
    pos = np.asarray(inputs["positions"], dtype=np.int32)
    shared = {k_: np.ascontiguousarray(v) for k_, v in inputs.items() if k_ not in ("x", "mem", "positions")}
    in_maps = []
    for c in range(n):
        sl = slice(c * NB_PER_CORE, (c + 1) * NB_PER_CORE)
        m = dict(shared)
        m["x"] = x[sl]
        m["xT"] = np.ascontiguousarray(x[sl].transpose(0, 2, 1))
        m["memT"] = np.ascontiguousarray(mem[sl].transpose(0, 2, 1))
        m["pos"] = np.ascontiguousarray(pos[sl].reshape(NB_PER_CORE, NT, 128).transpose(0, 2, 1))
        in_maps.append(m)
    res = run_bass_kernel_spmd(nc, in_maps, core_ids=list(range(n)))
    return np.concatenate([r["out"] for r in res.results], axis=0).astype(np.float32)
```

```python
import contextlib
import math
import types
import numpy as np
import concourse.bass as bass
import concourse.mybir as mybir
from concourse.bass_utils import run_bass_kernel_spmd

F32 = mybir.dt.float32
BF16 = mybir.dt.bfloat16
I32 = mybir.dt.int32
AF = mybir.ActivationFunctionType
ALU = mybir.AluOpType
AX = mybir.AxisListType

ENGS = ("pe", "act", "dve", "pool", "sp")

D = 1024
S = 2048
NT = S // 128
NB_PER_CORE = 2
DEPTH = 2
ALPHA = (2 * DEPTH) ** 0.25
LN_EPS = 1e-5
RMS_EPS = 1e-6
DSA_IN = 3144
GLA_IN = 2576
NEG = -1.0e30
MOE_MUL_ENG = "pool"
SCHED = True
SCHED_WINDOW = 48


class Buf:
    __slots__ = ("name", "writer", "readers")

    def __init__(self, name=""):
        self.name = name
        self.writer = None
        self.readers = []


class Op:
    __slots__ = ("eng", "fn", "deps", "is_dma", "signal", "sigval", "lane", "laneval",
                 "lane_prev", "barriered", "gid", "cost", "t0", "t1")

    def __init__(self, eng, fn, is_dma):
        self.eng = eng
        self.fn = fn
        self.deps = []
        self.is_dma = is_dma
        self.signal = False
        self.sigval = 0
        self.lane = None
        self.laneval = 0
        self.lane_prev = 0
        self.barriered = False
        self.gid = 0
        self.cost = None
        self.t0 = None
        self.t1 = None


def _freeze(fn):
    if fn is None or fn.__closure__ is None:
        return fn
    cells = []
    for c in fn.__closure__:
        try:
            cells.append(types.CellType(c.cell_contents))
        except ValueError:
            cells.append(c)
    return types.FunctionType(fn.__code__, fn.__globals__, fn.__name__, fn.__defaults__, tuple(cells))


class Prog:
    def __init__(self, nc):
        self.nc = nc
        self.ops = {e: [] for e in ENGS}
        self.stacks = [contextlib.ExitStack()]
        self.n_lanes = {"sp": 16, "pool": 12, "act": 8}
        self._uid = 0
        self.seq = []

    def sbuf(self, name, shape, dtype):
        self._uid += 1
        return self.stacks[-1].enter_context(
            self.nc.sbuf_tensor(f"{name}_{self._uid}", list(shape), dtype))

    def psum(self, name, shape, dtype=F32):
        self._uid += 1
        return self.stacks[-1].enter_context(
            self.nc.psum_tensor(f"{name}_{self._uid}", list(shape), dtype))

    @contextlib.contextmanager
    def scope(self):
        self.stacks.append(contextlib.ExitStack())
        try:
            yield
        finally:
            self.barrier()
            self.stacks.pop().close()

    def op(self, eng, fn, reads=(), writes=(), dma=False, cost=None):
        o = Op(eng, _freeze(fn), dma)
        o.cost = cost
        o.gid = len(self.seq)
        seen = set()
        for b in reads:
            if b.writer is not None and id(b.writer) not in seen:
                o.deps.append((b.writer, "raw"))
                seen.add(id(b.writer))
        for b in writes:
            if b.writer is not None and id(b.writer) not in seen:
                o.deps.append((b.writer, "waw"))
                seen.add(id(b.writer))
            for r in b.readers:
                if id(r) not in seen:
                    o.deps.append((r, "war"))
                    seen.add(id(r))
        for b in reads:
            b.readers.append(o)
        for b in writes:
            b.writer = o
            b.readers = []
        self.seq.append(o)
        return o

    def pe(self, fn, reads=(), writes=(), cost=None):
        return self.op("pe", fn, reads, writes, cost=cost)

    def act(self, fn, reads=(), writes=(), cost=None):
        return self.op("act", fn, reads, writes, cost=cost)

    def dve(self, fn, reads=(), writes=(), cost=None):
        return self.op("dve", fn, reads, writes, cost=cost)

    def pool(self, fn, reads=(), writes=(), cost=None):
        return self.op("pool", fn, reads, writes, cost=cost)

    def dma(self, eng, out, in_, reads=(), writes=(), **kw):
        return self.op(eng, lambda e: e.dma_start(out=out, in_=in_, **kw), reads, writes, dma=True)

    def barrier(self):
        self.seq.append(None)

    def _materialise_fence(self):
        lasts = []
        for e in ENGS:
            for o in reversed(self.ops[e]):
                if o.fn is not None and not o.is_dma:
                    lasts.append(o)
                    break
        dmas = [o for e in ENGS for o in self.ops[e] if o.is_dma and not o.barriered]
        for e in ENGS:
            o = Op(e, None, False)
            for l in lasts:
                if l.eng != e:
                    o.deps.append((l, "raw"))
            for d in dmas:
                o.deps.append((d, "raw"))
            self.ops[e].append(o)
        for d in dmas:
            d.barriered = True

    DEFCOST = {"pe": 0.3, "act": 0.5, "dve": 0.6, "pool": 0.9, "sp": 0.05}

    def _schedule_segment(self, seg):
        if not SCHED or len(seg) < 3:
            for o in seg:
                self.ops[o.eng].append(o)
            return
        inseg = set(id(o) for o in seg)
        queues = {e: [o for o in seg if o.eng == e] for e in ENGS}
        heads = {e: 0 for e in ENGS}
        done = set()
        free = {e: 0.0 for e in ENGS}
        remaining = len(seg)
        SYNC = 2.0
        while remaining:
            best = None
            for e in ENGS:
                q = queues[e]
                h = heads[e]
                while h < len(q) and id(q[h]) in done:
                    h += 1
                heads[e] = h
                if h >= len(q):
                    continue
                lim = min(len(q), h + SCHED_WINDOW)
                for k in range(h, lim):
                    o = q[k]
                    if id(o) in done:
                        continue
                    rt = 0.0
                    ok = True
                    for d, kind in o.deps:
                        if id(d) not in inseg:
                            continue
                        if id(d) not in done:
                            ok = False
                            break
                        if d.eng == o.eng and not d.is_dma and (o.eng == "pe" or kind != "raw"):
                            t = d.t0
                        else:
                            t = d.t1 + SYNC
                        if t > rt:
                            rt = t
                    if not ok:
                        continue
                    st = rt if rt > free[e] else free[e]
                    key = (st, o.gid)
                    if best is None or key < best[0]:
                        best = (key, e, o)
                    if st <= free[e]:
                        break
            assert best is not None, "scheduler deadlock"
            (st, _), e, o = best
            c = o.cost if o.cost is not None else (3.0 if o.is_dma else self.DEFCOST[e])
            o.t0 = st
            if o.is_dma:
                o.t1 = st + c
                free[e] = st + 0.1
            else:
                o.t1 = st + c
                free[e] = o.t1
            done.add(id(o))
            self.ops[e].append(o)
            remaining -= 1

    def _schedule(self):
        seg = []
        for it in self.seq:
            if it is None:
                self._schedule_segment(seg)
                seg = []
                self._materialise_fence()
            else:
                seg.append(it)
        self._schedule_segment(seg)

    def emit(self):
        nc = self.nc

        def needs_wait(o, d, kind):
            return not ((not d.is_dma) and d.eng == o.eng and (o.eng == "pe" or kind != "raw"))

        for e in ENGS:
            for o in self.ops[e]:
                for d, kind in o.deps:
                    if needs_wait(o, d, kind) and not d.is_dma:
                        d.signal = True
        for e in ENGS:
            cnt = 0
            lane_vals = [0] * self.n_lanes.get(e, 1)
            li = 0
            for o in self.ops[e]:
                if o.is_dma:
                    o.lane = li
                    o.lane_prev = lane_vals[li]
                    lane_vals[li] += 16
                    o.laneval = lane_vals[li]
                    li = (li + 1) % len(lane_vals)
                elif o.signal:
                    cnt += 1
                    o.sigval = cnt
        with contextlib.ExitStack() as st:
            esem = {e: st.enter_context(nc.semaphore(f"s_{e}")) for e in ("pe", "act", "dve", "pool")}
            lsem = {e: [st.enter_context(nc.semaphore(f"l_{e}{i}")) for i in range(n)]
                    for e, n in self.n_lanes.items()}
            block = st.enter_context(nc.Block())

            def run(ename, eng):
                seen = {}

                def wait(sem, key, val):
                    if seen.get(key, 0) >= val:
                        return
                    seen[key] = val
                    eng.wait_ge(sem, val)

                for o in self.ops[ename]:
                    for d, kind in o.deps:
                        if not needs_wait(o, d, kind):
                            continue
                        if d.is_dma:
                            wait(lsem[d.eng][d.lane], ("l", d.eng, d.lane), d.laneval)
                        else:
                            wait(esem[d.eng], ("e", d.eng), d.sigval)
                    if o.fn is None:
                        continue
                    if o.is_dma:
                        if o.lane_prev > 0:
                            wait(lsem[ename][o.lane], ("l", ename, o.lane), o.lane_prev)
                        o.fn(eng).then_inc(lsem[ename][o.lane], 16)
                    else:
                        ins = o.fn(eng)
                        if o.signal:
                            ins.then_inc(esem[ename], 1)

            @block.tensor
            def _(eng):
                run("pe", eng)

            @block.scalar
            def _(eng):
                run("act", eng)

            @block.vector
            def _(eng):
                run("dve", eng)

            @block.gpsimd
            def _(eng):
                run("pool", eng)

            @block.sync
            def _(eng):
                run("sp", eng)

    def finish(self):
        self._schedule()
        fin = Op("sp", None, False)
        for e in ENGS:
            for o in self.ops[e]:
                if o.is_dma:
                    fin.deps.append((o, "raw"))
        fin.deps.sort(key=lambda t: t[0].laneval)
        self.ops["sp"].append(fin)
        self.emit()
        while self.stacks:
            self.stacks.pop().close()


class K:
    def __init__(self, nseq=NB_PER_CORE, debug=None):
        self.nseq = nseq
        self.debug = debug
        nc = bass.Bass("TRN2", target_bir_lowering=False)
        self.nc = nc
        self.P = Prog(nc)
        dt = nc.dram_tensor

        def inp(name, shape, dtype=F32):
            return dt(name, list(shape), dtype, kind="ExternalInput").ap()

        I = {}
        I["x"] = inp("x", [nseq, S, D])
        I["xT"] = inp("xT", [nseq, D, S])
        I["memT"] = inp("memT", [nseq, D, 256])
        I["pos"] = inp("pos", [nseq, 128, NT], I32)
        I["dsa_w_in"] = inp("dsa_w_in", [1, D, DSA_IN])
        I["dsa_idx_k_g"] = inp("dsa_idx_k_g", [1, 64])
        I["dsa_idx_k_b"] = inp("dsa_idx_k_b", [1, 64])
        I["gla_w_in"] = inp("gla_w_in", [1, D, GLA_IN])
        I["gla_w_gate"] = inp("gla_w_gate", [1, 16, 384])
        I["gla_b_gate"] = inp("gla_b_gate", [1, 384])
        I["gla_norm_g"] = inp("gla_norm_g", [1, 192])
        I["w_mem_kv"] = inp("w_mem_kv", [2, D, 512])
        I["w_out"] = inp("w_out", [2, D, D])
        for n in ("ln1_g", "ln1_b", "ln2_g", "ln2_b"):
            I[n] = inp(n, [2, D])
        I["moe_w_group"] = inp("moe_w_group", [2, D, 4])
        I["moe_b_group"] = inp("moe_b_group", [2, 4])
        I["moe_w_router"] = inp("moe_w_router", [2, 4, D, 8])
        I["moe_b_router"] = inp("moe_b_router", [2, 4, 8])
        I["moe_w13"] = inp("moe_w13", [2, 4, 8, D, 512])
        I["moe_w2"] = inp("moe_w2", [2, 4, 8, 256, D])
        self.I = I
        self.out = dt("out", [nseq, S, D], F32, kind="ExternalOutput").ap()
        import os
        self.dbg = dt("dbg", [S, D], F32, kind="ExternalOutput").ap() if os.environ.get("KDBG") else None
        self.dbgc = dt("dbgc", [D, S], F32, kind="ExternalOutput").ap() if os.environ.get("KDBG") else None
        self.x1s = dt("x1s", [S, D], F32).ap()
        self.x2s = dt("x2s", [S, D], F32).ap()
        self.combT_d = dt("combT_d", [32, S], F32).ap()
        self.Bx1s = [Buf() for _ in range(NT)]
        self.Bx2s = [Buf() for _ in range(NT)]
        self.Bcomb = [Buf() for _ in range(NT)]

    def consts(self):
        P, nc = self.P, self.nc
        self.ident = P.sbuf("ident", [128, 128], F32)
        self.identb = P.sbuf("identb", [128, 128], BF16)
        self.Bc = Buf("consts")
        B = self.Bc
        ident, identb = self.ident, self.identb
        P.pool(lambda e: e.memset(ident[:], 1.0), writes=[B])
        P.pool(lambda e: e.affine_select(out=ident[:], in_=ident[:], compare_op=ALU.is_equal, fill=0.0,
                                         base=0, pattern=[[-1, 128]], channel_multiplier=1),
               reads=[B], writes=[B])
        P.pool(lambda e: e.tensor_copy(out=identb[:], in_=ident[:]), reads=[B], writes=[B])
        self.xT_bf = P.sbuf("xT_bf", [128, 8, S], BF16)
        self.BxT = [Buf() for _ in range(NT)]
        self.x1T_bf = self.xT_bf
        self.Bx1T = self.BxT
        self.psT_extra = []
        self.psT = [P.psum("psT", [128, 1024]) for _ in range(1)]
        self.BpsT = [Buf() for _ in range(1)]
        self.comb_all = P.sbuf("comb_all", [128, NT, 32], F32)

    def bcast_row(self, name, src_row_ap, n, eng="sp"):
        t = self.P.sbuf(name, [128, n], F32)
        B = Buf(name)
        self.P.dma(eng, t[:], src_row_ap.partition_broadcast(128), writes=[B])
        return t, B

    def layernorm_tile(self, z, Bz, outt, Bo, g, Bg, bt, Bb, st, Bst):
        P = self.P
        stats, mv, rstd, nmr = st
        P.dve(lambda e: e.bn_stats(out=stats[:, 0:6], in_=z[:, 0:512]), reads=[Bz], writes=[Bst])
        P.dve(lambda e: e.bn_stats(out=stats[:, 6:12], in_=z[:, 512:1024]), reads=[Bz], writes=[Bst])
        P.dve(lambda e: e.bn_aggr(out=mv[:], in_=stats[:]), reads=[Bst], writes=[Bst])
        P.act(lambda e: e.activation(out=rstd[:], in_=mv[:, 1:2], func=AF.Ln, bias=LN_EPS, scale=1.0),
              reads=[Bst], writes=[Bst], cost=0.2)
        P.act(lambda e: e.activation(out=rstd[:], in_=rstd[:], func=AF.Exp, scale=-0.5),
              reads=[Bst], writes=[Bst], cost=0.2)
        P.dve(lambda e: e.scalar_tensor_tensor(out=nmr[:], in0=mv[:, 0:1], scalar=-1.0, in1=rstd[:],
                                               op0=ALU.mult, op1=ALU.mult), reads=[Bst], writes=[Bst])
        P.act(lambda e: e.activation(out=outt[:], in_=z[:], func=AF.Identity, bias=nmr[:], scale=rstd[:]),
              reads=[Bz, Bst], writes=[Bo])
        P.dve(lambda e: e.tensor_tensor(out=outt[:], in0=outt[:], in1=g[:], op=ALU.mult),
              reads=[Bo, Bg], writes=[Bo])
        P.dve(lambda e: e.tensor_tensor(out=outt[:], in0=outt[:], in1=bt[:], op=ALU.add),
              reads=[Bo, Bb], writes=[Bo])

    def ln_scratch(self, name):
        P = self.P
        return ((P.sbuf(name + "st", [128, 12], F32), P.sbuf(name + "mv", [128, 2], F32),
                 P.sbuf(name + "rs", [128, 1], F32), P.sbuf(name + "nm", [128, 1], F32)), Buf(name))

    def router_setup(self, li):
        P, I = self.P, self.I
        self.wr = P.sbuf("wr", [128, 8, 36], F32)
        self.Bwr = Buf("wr")
        wr = self.wr
        P.dma("sp", wr[:, :, 0:4], I["moe_w_group"][li].rearrange("(c p) g -> p c g", p=128), writes=[self.Bwr])
        for g in range(4):
            P.dma("sp", wr[:, :, 4 + 8 * g:12 + 8 * g],
                  I["moe_w_router"][li, g].rearrange("(c p) e -> p c e", p=128), writes=[self.Bwr])
        self.rbias = P.sbuf("rbias", [128, 36], F32)
        self.Brb = Buf("rbias")
        P.dma("sp", self.rbias[:, 0:4], I["moe_b_group"][li:li + 1, :].partition_broadcast(128), writes=[self.Brb])
        P.dma("sp", self.rbias[:, 4:36],
              I["moe_b_router"][li:li + 1].rearrange("o g e -> o (g e)").partition_broadcast(128),
              writes=[self.Brb])
        self.rt = []
        for k in range(2):
            d = dict(
                x1Tf=P.sbuf("x1Tf", [128, 1024], F32), lg=P.sbuf("lg", [128, 36], F32),
                sm=P.sbuf("rsm", [128, 16], F32), me=P.sbuf("rme", [128, 32], F32),
                top8=P.sbuf("top8", [128, 8], F32), ex=P.sbuf("rex", [128, 32], F32),
                comb=P.sbuf("comb", [128, 32], F32), cT=P.sbuf("cT", [32, 128], F32),
                B=Buf("rt"), BxTf=Buf("x1Tf"), BcT=Buf("cT"))
            self.rt.append(d)
        self.psR = self.psT[0]
        self.BpsR = self.BpsT[0]

    def transpose_to_bf(self, src, Bsrc, dstT, BdstT, i, also_f32=None):
        P = self.P
        psT, BpsT = self.psT[0], self.BpsT[0]
        ident = self.ident
        for c in range(8):
            P.pe(lambda e, c=c: e.transpose(out=psT[:, c * 128:(c + 1) * 128], in_=src[:, c * 128:(c + 1) * 128],
                                            identity=ident[:]),
                 reads=[Bsrc, self.Bc], writes=[BpsT] + self.psT_extra)
        if also_f32 is None:
            P.dve(lambda e: e.tensor_copy(out=dstT[:, :, i * 128:(i + 1) * 128],
                                          in_=psT[:].rearrange("p (c t) -> p c t", c=8)),
                  reads=[BpsT], writes=[BdstT])
        else:
            t, Bt = also_f32
            P.act(lambda e: e.copy(out=t[:], in_=psT[:]), reads=[BpsT], writes=[Bt])
            P.dve(lambda e: e.tensor_copy(out=dstT[:, :, i * 128:(i + 1) * 128],
                                          in_=t[:].rearrange("p (c t) -> p c t", c=8)),
                  reads=[Bt], writes=[BdstT])

    def router_tile(self, i, x1, Bx1):
        P = self.P
        r = self.rt[i % 2]
        B = r["B"]
        self.transpose_to_bf(x1, Bx1, self.x1T_bf, self.Bx1T[i], i, also_f32=(r["x1Tf"], r["BxTf"]))
        import os
        STOP = int(os.environ.get("RSTOP", "99"))
        if STOP < 1:
            return
        psR, BpsR = self.psR, self.BpsR
        x1Tf, wr = r["x1Tf"], self.wr
        for c in range(8):
            P.pe(lambda e, c=c: e.matmul(psR[:, 0:36], lhsT=x1Tf[:, c * 128:(c + 1) * 128], rhs=wr[:, c, :],
                                         start=(c == 0), stop=(c == 7)),
                 reads=[r["BxTf"], self.Bwr], writes=[BpsR])
        if STOP < 2:
            return
        lg, sm, me, top8, ex, comb, cT = r["lg"], r["sm"], r["me"], r["top8"], r["ex"], r["comb"], r["cT"]
        rb = self.rbias
        P.dve(lambda e: e.tensor_tensor(out=lg[:], in0=psR[:, 0:36], in1=rb[:], op=ALU.add),
              reads=[BpsR, self.Brb], writes=[B])
        P.dve(lambda e: e.reduce_max(out=sm[:, 0:1], in_=lg[:, 0:4], axis=AX.X), reads=[B], writes=[B])
        P.dve(lambda e: e.tensor_scalar(out=sm[:, 1:2], in0=sm[:, 0:1], scalar1=-1.0, scalar2=None, op0=ALU.mult),
              reads=[B], writes=[B])
        P.act(lambda e: e.activation(out=sm[:, 12:16], in_=lg[:, 0:4], func=AF.Exp, bias=sm[:, 1:2], scale=1.0),
              reads=[B], writes=[B])
        P.dve(lambda e: e.reduce_sum(out=sm[:, 2:3], in_=sm[:, 12:16], axis=AX.X), reads=[B], writes=[B])
        P.dve(lambda e: e.tensor_scalar(out=sm[:, 4:8], in0=lg[:, 0:4], scalar1=sm[:, 0:1], scalar2=None,
                                        op0=ALU.is_lt), reads=[B], writes=[B])
        P.dve(lambda e: e.tensor_scalar(out=sm[:, 4:8], in0=sm[:, 4:8], scalar1=-30000.0, scalar2=None,
                                        op0=ALU.mult), reads=[B], writes=[B])
        for g in range(4):
            P.dve(lambda e, g=g: e.tensor_scalar(out=me[:, 8 * g:8 * g + 8], in0=lg[:, 4 + 8 * g:12 + 8 * g],
                                                 scalar1=sm[:, 4 + g:5 + g], scalar2=None, op0=ALU.add),
                  reads=[B], writes=[B])
        P.dve(lambda e: e.max(out=top8[:], in_=me[:]), reads=[B], writes=[B])
        P.dve(lambda e: e.tensor_scalar(out=sm[:, 8:9], in0=top8[:, 0:1], scalar1=-1.0, scalar2=None, op0=ALU.mult),
              reads=[B], writes=[B])
        P.act(lambda e: e.activation(out=ex[:], in_=me[:], func=AF.Exp, bias=sm[:, 8:9], scale=1.0),
              reads=[B], writes=[B])
        P.act(lambda e: e.activation(out=sm[:, 9:10], in_=top8[:, 1:2], func=AF.Exp, bias=sm[:, 8:9], scale=1.0),
              reads=[B], writes=[B])
        P.dve(lambda e: e.scalar_tensor_tensor(out=sm[:, 11:12], in0=sm[:, 9:10], scalar=1.0, in1=sm[:, 2:3],
                                               op0=ALU.add, op1=ALU.mult), reads=[B], writes=[B])
        P.dve(lambda e: e.reciprocal(out=sm[:, 10:11], in_=sm[:, 11:12]), reads=[B], writes=[B])
        P.dve(lambda e: e.scalar_tensor_tensor(out=comb[:], in0=me[:], scalar=top8[:, 1:2], in1=ex[:],
                                               op0=ALU.is_ge, op1=ALU.mult), reads=[B], writes=[B])
        call = self.comb_all
        P.dve(lambda e: e.tensor_scalar(out=call[:, i, :], in0=comb[:], scalar1=sm[:, 10:11], scalar2=None,
                                        op0=ALU.mult), reads=[B], writes=[self.Bcomb[i]])

    def moe(self, li, b, last, after_experts=None):
        P, I = self.P, self.I
        with P.scope():
            yacc = P.sbuf("yacc", [128, NT, 1024], F32)
            Byacc = [[Buf() for _ in range(2)] for _ in range(NT)]
            w13b = [P.sbuf("w13b", [128, 8, 512], BF16) for _ in range(2)]
            w2b = [P.sbuf("w2b", [128, 2, 1024], BF16) for _ in range(2)]
            Bw13 = [Buf() for _ in range(2)]
            Bw2 = [Buf() for _ in range(2)]
            sa = [P.sbuf("sa", [128, 512], F32) for _ in range(2)]
            su = [P.sbuf("su", [128, 512], F32) for _ in range(2)]
            actT = [[P.sbuf("actT", [128, 512], BF16) for _ in range(2)] for _ in range(2)]
            Bsa = [Buf() for _ in range(2)]
            Bsu = [Buf() for _ in range(2)]
            Bact = [[Buf() for _ in range(2)] for _ in range(2)]
            hps = [P.psum("hps", [128, 512]) for _ in range(4)]
            Bh = [Buf() for _ in range(4)]
            yps_t = [P.psum("yps", [128, 512]) for _ in range(2)]
            psT0 = self.psT[0]
            yps = [yps_t[0][:, :], yps_t[1][:, :], psT0[:, 0:512], psT0[:, 512:1024]]
            By = [Buf() for _ in range(4)]
            x1T = self.x1T_bf
            call = self.comb_all
            MUL_ENG = MOE_MUL_ENG

            def load_w(e):
                s = e % 2
                g, ee = divmod(e, 8)
                P.dma("pool", w13b[s][:], I["moe_w13"][li, g, ee].rearrange("(c p) f -> p c f", p=128),
                      writes=[Bw13[s]])
                P.dma("pool", w2b[s][:], I["moe_w2"][li, g, ee].rearrange("(j p) d -> p j d", p=128),
                      writes=[Bw2[s]])

            units = [(e, tt) for e in range(32) for tt in range(4)]
            yrot = [0]

            def emit_h(k):
                e, tt = units[k]
                s, par = e % 2, k % 2
                tsl = slice(tt * 512, (tt + 1) * 512)
                for fc in range(4):
                    for c in range(8):
                        P.pe(lambda en: en.matmul(hps[fc][:], lhsT=w13b[s][:, c, fc * 128:(fc + 1) * 128],
                                                  rhs=x1T[:, c, tsl], start=(c == 0), stop=(c == 7)),
                             reads=[Bw13[s]] + self.Bx1T[tt * 4:tt * 4 + 4], writes=[Bh[fc]])
                for j in range(2):
                    P.act(lambda en: en.activation(out=sa[j][:], in_=hps[j][:], func=AF.Silu),
                          reads=[Bh[j]], writes=[Bsa[j]])
                    P.act(lambda en: en.copy(out=su[j][:], in_=hps[2 + j][:]), reads=[Bh[2 + j]], writes=[Bsu[j]])
                    P.op(MUL_ENG, lambda en: en.tensor_tensor(out=actT[par][j][:], in0=sa[j][:], in1=su[j][:],
                                                              op=ALU.mult),
                         reads=[Bsa[j], Bsu[j]], writes=[Bact[par][j]])

            def emit_y(k):
                e, tt = units[k]
                s, par = e % 2, k % 2
                for tch in range(4):
                    ti = tt * 4 + tch
                    for half in range(2):
                        r = yrot[0]
                        yrot[0] = (r + 1) % 4
                        for j in range(2):
                            P.pe(lambda en: en.matmul(yps[r], lhsT=actT[par][j][:, tch * 128:(tch + 1) * 128],
                                                      rhs=w2b[s][:, j, half * 512:(half + 1) * 512],
                                                      start=(j == 0), stop=(j == 1)),
                                 reads=[Bact[par][j], Bw2[s]], writes=[By[r]])
                        dst = yacc[:, ti, half * 512:(half + 1) * 512]
                        gate = call[:, ti, e:e + 1]
                        if e == 0:
                            P.dve(lambda en: en.tensor_scalar(out=dst, in0=yps[r], scalar1=gate, scalar2=None,
                                                              op0=ALU.mult),
                                  reads=[By[r], self.Bcomb[ti]], writes=[Byacc[ti][half]])
                        else:
                            P.dve(lambda en: en.scalar_tensor_tensor(out=dst, in0=yps[r], scalar=gate, in1=dst,
                                                                     op0=ALU.mult, op1=ALU.add),
                                  reads=[By[r], Byacc[ti][half], self.Bcomb[ti]], writes=[Byacc[ti][half]])

            load_w(0)
            for k in range(len(units)):
                emit_h(k)
                if k > 0:
                    emit_y(k - 1)
                e, tt = units[k]
                if tt == 0 and e + 1 < 32:
                    load_w(e + 1)
            emit_y(len(units) - 1)
            self.psT_extra = [By[2], By[3]]
            if after_experts is not None:
                after_experts()

            g2, Bg2 = self.bcast_row("g2", I["ln2_g"][li:li + 1, :], D)
            b2, Bb2 = self.bcast_row("b2", I["ln2_b"][li:li + 1, :], D)
            st2 = [self.ln_scratch("ln2a"), self.ln_scratch("ln2b")]
            xin = [P.sbuf("xin", [128, D], F32) for _ in range(2)]
            Bxin = [Buf() for _ in range(2)]
            xo = [P.sbuf("xo", [128, D], F32) for _ in range(2)]
            Bxo = [Buf() for _ in range(2)]
            for i in range(NT):
                p = i % 2
                P.dma("sp", xin[p][:], self.x1s[i * 128:(i + 1) * 128, :], reads=[self.Bx1s[i]], writes=[Bxin[p]])
                yv = yacc[:, i, :]
                P.dve(lambda en, p=p, yv=yv: en.scalar_tensor_tensor(out=xin[p][:], in0=xin[p][:], scalar=ALPHA,
                                                                      in1=yv, op0=ALU.mult, op1=ALU.add),
                      reads=[Bxin[p]] + Byacc[i], writes=[Bxin[p]])
                st, Bst = st2[p]
                self.layernorm_tile(xin[p], Bxin[p], xo[p], Bxo[p], g2, Bg2, b2, Bb2, st, Bst)
                if last:
                    P.dma("sp", self.out[b, i * 128:(i + 1) * 128, :], xo[p][:], reads=[Bxo[p]])
                else:
                    P.dma("sp", self.x2s[i * 128:(i + 1) * 128, :], xo[p][:], reads=[Bxo[p]], writes=[self.Bx2s[i]])
                    self.transpose_to_bf(xo[p], Bxo[p], self.xT_bf, self.BxT[i], i)
            self.psT_extra = []


def build_moe_test():
    k = K(nseq=1)
    P, I = k.P, k.I
    k.consts()
    with P.scope():
        k.router_setup(0)
        xt = [P.sbuf("xt", [128, D], F32) for _ in range(2)]
        Bxt = [Buf() for _ in range(2)]
        for i in range(NT):
            p = i % 2
            P.dma("sp", xt[p][:], I["x"][0, i * 128:(i + 1) * 128, :], writes=[Bxt[p]])
            P.dma("sp", k.x1s[i * 128:(i + 1) * 128, :], xt[p][:], reads=[Bxt[p]], writes=[k.Bx1s[i]])
            k.router_tile(i, xt[p], Bxt[p])
    k.moe(0, 0, True)
    P.finish()
    return k.nc


class Banks:
    def __init__(self, P, n):
        self.t = [P.psum("bank", [128, 512]) for _ in range(n)]
        self.B = [Buf() for _ in range(n)]
        self.i = 0

    def get(self):
        k = self.i
        self.i = (self.i + 1) % 4
        return self.t[k], self.B[k]

    def fixed(self, k):
        return self.t[4 + k], self.B[4 + k]


def _load_cast(P, dst, src, B):
    P.dma("pool", dst, src, writes=[B])


def k_load_xT(self, b):
    P, I = self.P, self.I
    P.dma("pool", self.xT_bf[:], I["xT"][b].rearrange("(c p) t -> p c t", p=128), writes=self.BxT)


def k_mem_attn(self, li, b, banks, w_in_ap, qm_off, catT, BcatT):
    P, I = self.P, self.I
    memT = P.sbuf("memT", [128, 8, 256], BF16)
    wkv = P.sbuf("wkv", [128, 8, 512], BF16)
    wqm = P.sbuf("wqm", [128, 8, 256], BF16)
    Bm, Bkv, Bqm = Buf(), Buf(), Buf()
    P.dma("pool", memT[:], I["memT"][b].rearrange("(c p) m -> p c m", p=128), writes=[Bm])
    P.dma("pool", wkv[:], I["w_mem_kv"][li].rearrange("(c p) f -> p c f", p=128), writes=[Bkv])
    P.dma("pool", wqm[:], w_in_ap[:, qm_off:qm_off + 256].rearrange("(c p) f -> p c f", p=128), writes=[Bqm])
    kmT = P.sbuf("kmT", [64, 4, 256], BF16)
    vma = P.sbuf("vma", [128, 2, 4, 128], BF16)
    qmT = P.sbuf("qmT", [64, 4, S], BF16)
    Bkm, Bvm, Bq = Buf(), Buf(), Buf()
    P.dve(lambda e: e.memset(vma[:], 1.0), writes=[Bvm])
    for h in range(4):
        ps, Bp = banks.get()
        for c in range(8):
            P.pe(lambda e, c=c, h=h, ps=ps: e.matmul(ps[0:64, 0:256], lhsT=wkv[:, c, h * 64:(h + 1) * 64],
                                                     rhs=memT[:, c, :], start=(c == 0), stop=(c == 7)),
                 reads=[Bkv, Bm], writes=[Bp])
        P.act(lambda e, h=h, ps=ps: e.copy(out=kmT[:, h, :], in_=ps[0:64, 0:256]), reads=[Bp], writes=[Bkm])
    for mc in range(2):
        ps, Bp = banks.get()
        for c in range(8):
            P.pe(lambda e, c=c, mc=mc, ps=ps: e.matmul(ps[:, 0:256], lhsT=memT[:, c, mc * 128:(mc + 1) * 128],
                                                       rhs=wkv[:, c, 256:512], start=(c == 0), stop=(c == 7)),
                 reads=[Bkv, Bm], writes=[Bp])
        P.act(lambda e, mc=mc, ps=ps: e.copy(out=vma[:, mc, :, 0:64],
                                             in_=ps[:, 0:256].rearrange("p (h d) -> p h d", h=4)),
              reads=[Bp], writes=[Bvm])
    for h in range(4):
        for tt in range(4):
            ps, Bp = banks.get()
            tsl = slice(tt * 512, (tt + 1) * 512)
            for c in range(8):
                P.pe(lambda e, c=c, h=h, ps=ps, tsl=tsl: e.matmul(ps[0:64, :], lhsT=wqm[:, c, h * 64:(h + 1) * 64],
                                                                  rhs=self.xT_bf[:, c, tsl], start=(c == 0),
                                                                  stop=(c == 7)),
                     reads=[Bqm] + self.BxT[tt * 4:tt * 4 + 4], writes=[Bp])
            P.act(lambda e, h=h, ps=ps, tsl=tsl: e.copy(out=qmT[:, h, tsl], in_=ps[0:64, :]), reads=[Bp], writes=[Bq])
    pT = [P.sbuf("pT", [128, 512], BF16) for _ in range(2)]
    BpT = [Buf(), Buf()]
    rec = P.sbuf("mrec", [64, 512], F32)
    Brec = Buf()
    for h in range(4):
        for tt in range(4):
            tsl = slice(tt * 512, (tt + 1) * 512)
            for mc in range(2):
                ps, Bp = banks.get()
                P.pe(lambda e, h=h, mc=mc, ps=ps, tsl=tsl: e.matmul(ps[:, :], lhsT=kmT[:, h, mc * 128:(mc + 1) * 128],
                                                                    rhs=qmT[:, h, tsl], start=True, stop=True),
                     reads=[Bkm, Bq], writes=[Bp])
                P.act(lambda e, mc=mc, ps=ps: e.activation(out=pT[mc][:], in_=ps[:, :], func=AF.Exp, scale=0.125),
                      reads=[Bp], writes=[BpT[mc]])
            po, Bpo = banks.get()
            for mc in range(2):
                P.pe(lambda e, h=h, mc=mc, po=po: e.matmul(po[:, :], lhsT=vma[:, mc, h, :], rhs=pT[mc][:],
                                                           start=(mc == 0), stop=(mc == 1)),
                     reads=[Bvm, BpT[mc]], writes=[Bpo])
            P.dve(lambda e, po=po: e.reciprocal(out=rec[:], in_=po[64:128, :]), reads=[Bpo], writes=[Brec])
            hp, ch = h % 2, 6 + h // 2
            P.dve(lambda e, po=po, hp=hp, ch=ch, tsl=tsl: e.tensor_tensor(
                out=catT[hp * 64:hp * 64 + 64, ch, tsl], in0=po[0:64, :], in1=rec[:], op=ALU.mult),
                reads=[Bpo, Brec], writes=[BcatT])


def k_xphase(self, li, b, banks, catT, BcatT, xsrc, Bxsrc):
    P, I = self.P, self.I
    wo = P.sbuf("wo", [128, 8, D], BF16)
    Bwo = Buf()
    P.dma("pool", wo[:], I["w_out"][li].rearrange("(c p) f -> p c f", p=128), writes=[Bwo])
    g1, Bg1 = self.bcast_row("g1", I["ln1_g"][li:li + 1, :], D)
    b1, Bb1 = self.bcast_row("b1", I["ln1_b"][li:li + 1, :], D)
    st2 = [self.ln_scratch("ln1a"), self.ln_scratch("ln1b")]
    self.router_setup(li)
    xin = [P.sbuf("xin1", [128, D], F32) for _ in range(2)]
    Bxin = [Buf(), Buf()]
    x1 = [P.sbuf("x1t", [128, D], F32) for _ in range(2)]
    Bx1 = [Buf(), Buf()]
    for i in range(NT):
        p = i % 2
        tsl = slice(i * 128, (i + 1) * 128)
        P.dma("sp", xin[p][:], xsrc[tsl, :], reads=[Bxsrc[i]], writes=[Bxin[p]])
        for half in range(2):
            ps, Bp = banks.get()
            for c in range(8):
                P.pe(lambda e, c=c, ps=ps, half=half, tsl=tsl: e.matmul(
                    ps[:, :], lhsT=catT[:, c, tsl], rhs=wo[:, c, half * 512:(half + 1) * 512],
                    start=(c == 0), stop=(c == 7)), reads=[BcatT, Bwo], writes=[Bp])
            hs = slice(half * 512, (half + 1) * 512)
            P.dve(lambda e, p=p, ps=ps, hs=hs: e.scalar_tensor_tensor(
                out=xin[p][:, hs], in0=xin[p][:, hs], scalar=ALPHA, in1=ps[:, :], op0=ALU.mult, op1=ALU.add),
                reads=[Bxin[p], Bp], writes=[Bxin[p]])
        st, Bst = st2[p]
        self.layernorm_tile(xin[p], Bxin[p], x1[p], Bx1[p], g1, Bg1, b1, Bb1, st, Bst)
        P.dma("sp", self.x1s[tsl, :], x1[p][:], reads=[Bx1[p]], writes=[self.Bx1s[i]])
        if self.dbg is not None:
            P.dma("sp", self.dbg[tsl, :], x1[p][:], reads=[Bx1[p]])
        self.router_tile(i, x1[p], Bx1[p])


def k_gla(self, b, banks, catT, BcatT):
    P, I = self.P, self.I
    W = I["gla_w_in"][0]
    w = P.sbuf("glaw", [128, 8, 2320], BF16)
    Bw = Buf()
    Bws = [Bw, Buf()]
    for hh in range(2):
        P.dma("pool", w[:, :, hh * 1160:(hh + 1) * 1160],
              W[:, hh * 1160:(hh + 1) * 1160].rearrange("(c p) f -> p c f", p=128), writes=[Bws[hh]])
    Bk = Buf()
    Lneg = P.sbuf("Lneg", [128, 128], F32)
    Uneg = P.sbuf("Uneg", [128, 128], F32)
    MG4 = P.sbuf("MG4", [128, 4, 128], F32)
    ones1 = P.sbuf("ones1", [1, 128], F32)
    P.pool(lambda e: e.memset(Lneg[:], -1.0 / 16.0), writes=[Bk])
    P.pool(lambda e: e.affine_select(out=Lneg[:], in_=Lneg[:], compare_op=ALU.is_ge, fill=0.0, base=0,
                                     pattern=[[1, 128]], channel_multiplier=-1), reads=[Bk], writes=[Bk])
    P.pool(lambda e: e.memset(Lneg[0:64, 64:128], 0.0), reads=[Bk], writes=[Bk])
    P.pool(lambda e: e.memset(Uneg[:], -1.0 / 16.0), reads=[Bk], writes=[Bk])
    P.pool(lambda e: e.affine_select(out=Uneg[:], in_=Uneg[:], compare_op=ALU.is_ge, fill=0.0, base=-1,
                                     pattern=[[-1, 128]], channel_multiplier=1), reads=[Bk], writes=[Bk])
    P.pool(lambda e: e.memset(Uneg[64:128, 0:64], 0.0), reads=[Bk], writes=[Bk])
    for h in range(4):
        P.pool(lambda e, h=h: e.memset(MG4[:, h, :], 1.0), reads=[Bk], writes=[Bk])
        P.pool(lambda e, h=h: e.affine_select(out=MG4[:, h, :], in_=MG4[:, h, :], compare_op=ALU.is_ge, fill=0.0,
                                              base=0, pattern=[[1, 128]], channel_multiplier=-1),
               reads=[Bk], writes=[Bk])
        P.pool(lambda e, h=h: e.memset(MG4[0:64, h, 64:128], 0.0), reads=[Bk], writes=[Bk])
    P.pool(lambda e: e.memset(ones1[:], 1.0), reads=[Bk], writes=[Bk])
    wg = P.sbuf("wg", [16, 384], F32)
    bg = P.sbuf("bg", [1, 384], F32)
    P.dma("sp", wg[:], I["gla_w_gate"][0], writes=[Bk])
    P.dma("sp", bg[:], I["gla_b_gate"][0:1, :], writes=[Bk])
    ng4 = P.sbuf("ng4", [128, 768], F32)
    for h in range(4):
        P.dma("sp", ng4[:, h * 192:(h + 1) * 192], I["gla_norm_g"][0:1, :].partition_broadcast(128), writes=[Bk])
    Sf = P.sbuf("Sf", [96, 4, 192], F32)
    Sb = P.sbuf("Sb", [96, 4, 192], BF16)
    Sb2 = P.sbuf("Sb2", [96, 4, 192], BF16)
    BS = [Buf() for _ in range(4)]
    BSb = [Buf() for _ in range(4)]
    BSb2 = [Buf() for _ in range(4)]
    P.dve(lambda e: e.memset(Sf[:], 0.0), writes=BS)
    P.dve(lambda e: e.memset(Sb[:], 0.0), writes=BSb)
    P.dve(lambda e: e.memset(Sb2[:], 0.0), writes=BSb2)
    def mkset():
        qeA = P.sbuf("qeA", [96, 4, 128], BF16)
        qeB = P.sbuf("qeB", [96, 4, 128], BF16)
        BqA, BqB = Buf(), Buf()
        P.dve(lambda e: e.memset(qeA[:], 0.0), writes=[BqA])
        P.dve(lambda e: e.memset(qeB[:], 0.0), writes=[BqB])
        tiles = (qeA, qeB, BqA, BqB,
                 P.sbuf("a1T", [16, 128], F32), P.sbuf("spl", [128, 384], F32), P.sbuf("eb", [96, 512], F32),
                 P.sbuf("enb", [96, 512], F32), P.sbuf("erb", [128, 384], F32), P.sbuf("qe", [96, 512], BF16),
                 P.sbuf("ke", [96, 512], BF16), P.sbuf("kdec", [128, 384], BF16), P.sbuf("vb", [128, 768], BF16),
                 P.sbuf("sr", [128, 768], F32), P.sbuf("at", [128, 4, 128], BF16), P.sbuf("sq", [128, 192], F32),
                 P.sbuf("ss", [128, 8], F32), P.sbuf("on", [128, 768], F32), P.sbuf("onb", [128, 768], BF16))
        return tiles + tuple(Buf() for _ in range(14))

    sets = [mkset(), mkset()]
    xT = self.xT_bf
    for i in range(NT):
        tsl = slice(i * 128, (i + 1) * 128)
        Bx = [self.BxT[i]]
        (qeA, qeB, BqA, BqB, a1T, sp_, eb, enb, erb, qe, ke, kdec, vb, sr, at, sq, ss, on, onb,
         Ba1, Bsp, Beb, Benb, Berb, Bqe, Bke, Bkd, Bvb, Bsr, Bat, Bsq, Bss, Bon) = sets[i % 2]

        def proj_tok(ps, n0, n1, width):
            for c in range(8):
                P.pe(lambda e, c=c: e.matmul(ps[:, 0:width], lhsT=xT[:, c, tsl], rhs=w[:, c, n0:n1],
                                             start=(c == 0), stop=(c == 7)), reads=Bws + Bx, writes=[None])

        pa, Bpa = banks.get()
        for c in range(8):
            P.pe(lambda e, c=c, pa=pa: e.matmul(pa[0:16, 0:128], lhsT=w[:, c, 2304:2320], rhs=xT[:, c, tsl],
                                                start=(c == 0), stop=(c == 7)), reads=Bws + Bx, writes=[Bpa])
        P.act(lambda e, pa=pa: e.copy(out=a1T[:], in_=pa[0:16, 0:128]), reads=[Bpa], writes=[Ba1])
        pz, Bpz = banks.get()
        P.pe(lambda e, pz=pz: e.matmul(pz[:, 0:384], lhsT=a1T[:], rhs=wg[:], start=True, stop=False),
             reads=[Ba1, Bk], writes=[Bpz])
        P.pe(lambda e, pz=pz: e.matmul(pz[:, 0:384], lhsT=ones1[:], rhs=bg[:], start=False, stop=True),
             reads=[Bk], writes=[Bpz])
        P.act(lambda e, pz=pz: e.activation(out=sp_[:], in_=pz[:, 0:384], func=AF.Exp, scale=-1.0),
              reads=[Bpz], writes=[Bsp])
        P.act(lambda e: e.activation(out=sp_[:], in_=sp_[:], func=AF.Ln, bias=1.0, scale=1.0),
              reads=[Bsp], writes=[Bsp])
        pb_, Bpb = banks.get()
        for h in range(4):
            P.pe(lambda e, h=h, pb_=pb_: e.matmul(pb_[0:96, h * 128:(h + 1) * 128], lhsT=sp_[:, h * 96:(h + 1) * 96],
                                                  rhs=Lneg[:], start=True, stop=True), reads=[Bsp, Bk], writes=[Bpb])
        P.act(lambda e, pb_=pb_: e.activation(out=eb[:], in_=pb_[0:96, :], func=AF.Exp), reads=[Bpb], writes=[Beb])
        P.act(lambda e, pb_=pb_: e.activation(out=enb[:], in_=pb_[0:96, :], func=AF.Exp, scale=-1.0),
              reads=[Bpb], writes=[Benb])
        prb, Bprb = banks.get()
        P.pe(lambda e, prb=prb: e.matmul(prb[:, 0:384], lhsT=Uneg[:], rhs=sp_[:], start=True, stop=True),
             reads=[Bsp, Bk], writes=[Bprb])
        P.act(lambda e, prb=prb: e.activation(out=erb[:], in_=prb[:, 0:384], func=AF.Exp), reads=[Bprb], writes=[Berb])
        pq, Bpq = banks.get()
        for h in range(4):
            for c in range(8):
                P.pe(lambda e, c=c, h=h, pq=pq: e.matmul(pq[0:96, h * 128:(h + 1) * 128], lhsT=w[:, c, h * 96:(h + 1) * 96],
                                                         rhs=xT[:, c, tsl], start=(c == 0), stop=(c == 7)),
                     reads=Bws + Bx, writes=[Bpq])
        P.dve(lambda e, pq=pq: e.scalar_tensor_tensor(out=qe[:], in0=pq[0:96, :], scalar=96.0 ** -0.5, in1=eb[:],
                                                      op0=ALU.mult, op1=ALU.mult), reads=[Bpq, Beb], writes=[Bqe])
        P.dve(lambda e: e.tensor_copy(out=qeA[:, :, 0:64], in_=qe[:].rearrange("p (h t) -> p h t", h=4)[:, :, 0:64]),
              reads=[Bqe], writes=[BqA])
        P.dve(lambda e: e.tensor_copy(out=qeB[:, :, 64:128], in_=qe[:].rearrange("p (h t) -> p h t", h=4)[:, :, 64:128]),
              reads=[Bqe], writes=[BqB])
        pk, Bpk = banks.get()
        for h in range(4):
            for c in range(8):
                P.pe(lambda e, c=c, h=h, pk=pk: e.matmul(pk[0:96, h * 128:(h + 1) * 128],
                                                         lhsT=w[:, c, 384 + h * 96:384 + (h + 1) * 96],
                                                         rhs=xT[:, c, tsl], start=(c == 0), stop=(c == 7)),
                     reads=Bws + Bx, writes=[Bpk])
        P.dve(lambda e, pk=pk: e.tensor_tensor(out=ke[:], in0=pk[0:96, :], in1=enb[:], op=ALU.mult),
              reads=[Bpk, Benb], writes=[Bke])
        pkt, Bpkt = banks.get()
        for c in range(8):
            P.pe(lambda e, c=c, pkt=pkt: e.matmul(pkt[:, 0:384], lhsT=xT[:, c, tsl], rhs=w[:, c, 384:768],
                                                  start=(c == 0), stop=(c == 7)), reads=Bws + Bx, writes=[Bpkt])
        P.dve(lambda e, pkt=pkt: e.tensor_tensor(out=kdec[:], in0=pkt[:, 0:384], in1=erb[:], op=ALU.mult),
              reads=[Bpkt, Berb], writes=[Bkd])
        for part in range(2):
            pv, Bpv = banks.get()
            for c in range(8):
                P.pe(lambda e, c=c, pv=pv, part=part: e.matmul(pv[:, 0:384], lhsT=xT[:, c, tsl],
                                                               rhs=w[:, c, 768 + part * 384:768 + (part + 1) * 384],
                                                               start=(c == 0), stop=(c == 7)),
                     reads=Bws + Bx, writes=[Bpv])
            P.act(lambda e, pv=pv, part=part: e.copy(out=vb[:, part * 384:(part + 1) * 384], in_=pv[:, 0:384]),
                  reads=[Bpv], writes=[Bvb])
        for part in range(2):
            pr, Bpr = banks.get()
            for c in range(8):
                P.pe(lambda e, c=c, pr=pr, part=part: e.matmul(pr[:, 0:384], lhsT=xT[:, c, tsl],
                                                               rhs=w[:, c, 1536 + part * 384:1536 + (part + 1) * 384],
                                                               start=(c == 0), stop=(c == 7)),
                     reads=Bws + Bx, writes=[Bpr])
            P.act(lambda e, pr=pr, part=part: e.activation(out=sr[:, part * 384:(part + 1) * 384], in_=pr[:, 0:384],
                                                           func=AF.Silu), reads=[Bpr], writes=[Bsr])
        pat, Bpat = banks.get()
        for h in range(4):
            P.pe(lambda e, h=h, pat=pat: e.matmul(pat[:, h * 128:(h + 1) * 128], lhsT=ke[:, h * 128:(h + 1) * 128],
                                                  rhs=qe[:, h * 128:(h + 1) * 128], start=True, stop=True),
                 reads=[Bke, Bqe], writes=[Bpat])
        P.dve(lambda e, pat=pat: e.tensor_tensor(out=at[:].rearrange("p h t -> p (h t)"), in0=pat[:, :],
                                                 in1=MG4[:].rearrange("p h t -> p (h t)"), op=ALU.mult),
              reads=[Bpat, Bk], writes=[Bat])
        po2 = [banks.fixed(0), banks.fixed(1)]
        for h in range(4):
            po, Bpo = po2[h // 2]
            osl = slice((h % 2) * 192, (h % 2) * 192 + 192)
            vsl = slice(h * 192, (h + 1) * 192)
            ksl = slice(h * 96, (h + 1) * 96)
            P.pe(lambda e, h=h, po=po, osl=osl, vsl=vsl: e.matmul(po[:, osl], lhsT=at[:, h, :], rhs=vb[:, vsl],
                                                                   start=True, stop=False),
                 reads=[Bat, Bvb], writes=[Bpo])
            P.pe(lambda e, h=h, po=po, osl=osl: e.matmul(po[:, osl], lhsT=qeA[:, h, :], rhs=Sb[:, h, :],
                                                          start=False, stop=False), reads=[BqA, BSb[h]], writes=[Bpo])
            pi1, Bpi1 = banks.get()
            P.pe(lambda e, pi1=pi1, ksl=ksl, vsl=vsl: e.matmul(pi1[0:96, 0:192], lhsT=kdec[0:64, ksl], rhs=vb[0:64, vsl],
                                                               start=True, stop=True), reads=[Bkd, Bvb], writes=[Bpi1])
            P.dve(lambda e, h=h, pi1=pi1: e.scalar_tensor_tensor(out=Sf[:, h, :], in0=Sf[:, h, :],
                                                                 scalar=eb[:, h * 128 + 63:h * 128 + 64],
                                                                 in1=pi1[0:96, 0:192], op0=ALU.mult, op1=ALU.add),
                  reads=[BS[h], Beb, Bpi1], writes=[BS[h]])
            P.act(lambda e, h=h: e.copy(out=Sb2[:, h, :], in_=Sf[:, h, :]), reads=[BS[h]], writes=[BSb2[h]])
            P.pe(lambda e, h=h, po=po, osl=osl: e.matmul(po[:, osl], lhsT=qeB[:, h, :], rhs=Sb2[:, h, :],
                                                          start=False, stop=True), reads=[BqB, BSb2[h]], writes=[Bpo])
            pi2, Bpi2 = banks.get()
            P.pe(lambda e, pi2=pi2, ksl=ksl, vsl=vsl: e.matmul(pi2[0:96, 0:192], lhsT=kdec[64:128, ksl],
                                                               rhs=vb[64:128, vsl], start=True, stop=True),
                 reads=[Bkd, Bvb], writes=[Bpi2])
            P.dve(lambda e, h=h, pi2=pi2: e.scalar_tensor_tensor(out=Sf[:, h, :], in0=Sf[:, h, :],
                                                                 scalar=eb[:, h * 128 + 127:h * 128 + 128],
                                                                 in1=pi2[0:96, 0:192], op0=ALU.mult, op1=ALU.add),
                  reads=[BS[h], Beb, Bpi2], writes=[BS[h]])
            P.act(lambda e, h=h: e.copy(out=Sb[:, h, :], in_=Sf[:, h, :]), reads=[BS[h]], writes=[BSb[h]])
            P.act(lambda e, po=po, osl=osl: e.activation(out=sq[:], in_=po[:, osl], func=AF.Square),
                  reads=[Bpo], writes=[Bsq])
            P.dve(lambda e, h=h: e.reduce_sum(out=ss[:, h:h + 1], in_=sq[:], axis=AX.X), reads=[Bsq], writes=[Bss])
        P.act(lambda e: e.activation(out=ss[:, 4:8], in_=ss[:, 0:4], func=AF.Ln, bias=RMS_EPS, scale=1.0 / 192.0),
              reads=[Bss], writes=[Bss], cost=0.2)
        P.act(lambda e: e.activation(out=ss[:, 4:8], in_=ss[:, 4:8], func=AF.Exp, scale=-0.5),
              reads=[Bss], writes=[Bss], cost=0.2)
        for h in range(4):
            po, Bpo = po2[h // 2]
            osl = slice((h % 2) * 192, (h % 2) * 192 + 192)
            vsl = slice(h * 192, (h + 1) * 192)
            P.dve(lambda e, h=h, po=po, osl=osl, vsl=vsl: e.scalar_tensor_tensor(
                out=on[:, vsl], in0=po[:, osl], scalar=ss[:, 4 + h:5 + h], in1=ng4[:, vsl],
                op0=ALU.mult, op1=ALU.mult), reads=[Bpo, Bss, Bk], writes=[Bon])
        Bonb = Bsq
        P.dve(lambda e: e.tensor_tensor(out=onb[:], in0=on[:], in1=sr[:], op=ALU.mult), reads=[Bon, Bsr], writes=[Bonb])
        psT, BpsT = self.psT[0], self.BpsT[0]
        psTb = psT[:].bitcast(BF16)
        for c in range(6):
            P.pe(lambda e, c=c: e.transpose(out=psTb[:, c * 128:(c + 1) * 128], in_=onb[:, c * 128:(c + 1) * 128],
                                            identity=self.identb[:]), reads=[Bonb, self.Bc], writes=[BpsT])
        P.dve(lambda e: e.tensor_copy(out=catT[:, 0:6, tsl], in_=psTb[:, 0:768].rearrange("p (c t) -> p c t", c=6)),
              reads=[BpsT], writes=[BcatT])


K.load_xT = k_load_xT
K.mem_attn = k_mem_attn
K.xphase = k_xphase
K.gla = k_gla


def k_rope_tables(self, b):
    P, I = self.P, self.I
    B = Buf()
    cosT = P.sbuf("cosT", [128, NT, 32], F32)
    sinT = P.sbuf("sinT", [128, NT, 32], F32)
    with P.scope():
        self._rope_tables_body(b, cosT, sinT, B)
    return cosT, sinT, B


def k_rope_tables_body(self, b, cosT, sinT, B):
    P, I = self.P, self.I
    pi = P.sbuf("posi", [128, NT], I32)
    pf = P.sbuf("posf", [128, NT], F32)
    io = P.sbuf("iot", [128, 32], I32)
    inv = P.sbuf("inv", [128, 32], F32)
    ang = P.sbuf("ang", [128, NT, 32], F32)
    t1 = P.sbuf("rt1", [128, NT, 32], F32)
    P.dma("sp", pi[:], I["pos"][b], writes=[B])
    P.dve(lambda e: e.tensor_copy(out=pf[:], in_=pi[:]), reads=[B], writes=[B])
    P.pool(lambda e: e.iota(io[:], pattern=[[1, 32]], base=0, channel_multiplier=0), reads=[B], writes=[B])
    P.dve(lambda e: e.tensor_copy(out=inv[:], in_=io[:]), reads=[B], writes=[B])
    P.act(lambda e: e.activation(out=inv[:], in_=inv[:], func=AF.Exp, scale=-math.log(10000.0) / 32.0),
          reads=[B], writes=[B])
    for c in range(NT):
        P.dve(lambda e, c=c: e.tensor_scalar(out=ang[:, c, :], in0=inv[:], scalar1=pf[:, c:c + 1], scalar2=None,
                                             op0=ALU.mult), reads=[B], writes=[B])
    MAGIC = 12582912.0
    TWO_PI = 2.0 * math.pi
    for which, dst in ((0, sinT), (1, cosT)):
        src = ang
        if which == 1:
            P.dve(lambda e: e.tensor_scalar(out=ang[:], in0=ang[:], scalar1=math.pi / 2.0, scalar2=None, op0=ALU.add),
                  reads=[B], writes=[B])
        P.dve(lambda e: e.tensor_scalar(out=t1[:], in0=ang[:], scalar1=1.0 / TWO_PI, scalar2=MAGIC,
                                        op0=ALU.mult, op1=ALU.add), reads=[B], writes=[B])
        P.dve(lambda e: e.tensor_scalar(out=t1[:], in0=t1[:], scalar1=-MAGIC, scalar2=None, op0=ALU.add),
              reads=[B], writes=[B])
        P.dve(lambda e: e.scalar_tensor_tensor(out=t1[:], in0=t1[:], scalar=-TWO_PI, in1=ang[:],
                                               op0=ALU.mult, op1=ALU.add), reads=[B], writes=[B])
        P.act(lambda e, dst=dst: e.activation(out=dst[:], in_=t1[:], func=AF.Sin, scale=0.999999),
              reads=[B], writes=[B])


def k_rope(self, src, Bsrc, dst, Bdst, nh, cosT, sinT, Btab, i, tmp, Btmp):
    P = self.P
    s3 = src.rearrange("p (h d) -> p h d", h=nh)
    d3 = dst.rearrange("p (h d) -> p h d", h=nh)
    cb = cosT[:, i, :].unsqueeze(1).to_broadcast([128, nh, 32])
    sb = sinT[:, i, :].unsqueeze(1).to_broadcast([128, nh, 32])
    ta = tmp[:, 0:nh * 32].rearrange("p (h d) -> p h d", h=nh)
    tb = tmp[:, 512:512 + nh * 32].rearrange("p (h d) -> p h d", h=nh)
    x1, x2 = s3[:, :, 0:32], s3[:, :, 32:64]
    P.dve(lambda e: e.tensor_tensor(out=ta, in0=x1, in1=cb, op=ALU.mult), reads=[Bsrc, Btab], writes=[Btmp])
    P.dve(lambda e: e.tensor_tensor(out=tb, in0=x2, in1=sb, op=ALU.mult), reads=[Bsrc, Btab], writes=[Btmp])
    P.dve(lambda e: e.tensor_tensor(out=d3[:, :, 0:32], in0=ta, in1=tb, op=ALU.subtract), reads=[Btmp], writes=[Bdst])
    P.dve(lambda e: e.tensor_tensor(out=ta, in0=x2, in1=cb, op=ALU.mult), reads=[Bsrc, Btab, Bdst], writes=[Btmp])
    P.dve(lambda e: e.tensor_tensor(out=tb, in0=x1, in1=sb, op=ALU.mult), reads=[Bsrc, Btab], writes=[Btmp])
    P.dve(lambda e: e.tensor_tensor(out=d3[:, :, 32:64], in0=ta, in1=tb, op=ALU.add), reads=[Btmp], writes=[Bdst])


def k_dsa(self, b, banks, catT, BcatT):
    P, I = self.P, self.I
    W = I["dsa_w_in"][0]
    xT = self.xT_bf
    psT, BpsT = self.psT[0], self.BpsT[0]
    ident = self.ident
    cosT, sinT, Btab = self.rope_tables(b)
    tmp = P.sbuf("ropetmp", [128, 1024], F32)
    Btmp = Buf()
    maskT = [P.sbuf("maskT", [128, (NT - j) * 128], BF16) for j in range(NT)]
    BmT = [Buf() for _ in range(NT)]
    negm = P.sbuf("negm", [128, 128], F32)
    Bk = Buf()
    P.pool(lambda e: e.memset(negm[:], 0.0), writes=[Bk])
    P.pool(lambda e: e.affine_select(out=negm[:], in_=negm[:], compare_op=ALU.is_ge, fill=NEG, base=0,
                                     pattern=[[-1, 128]], channel_multiplier=1), reads=[Bk], writes=[Bk])
    with P.scope():
        wi_ = P.sbuf("dsw1", [128, 8, 584], BF16)
        Bw = Buf()
        P.dma("pool", wi_[:], W[:, 2304:2888].rearrange("(c p) f -> p c f", p=128), writes=[Bw])
        kg, Bkg = self.bcast_row("kg", I["dsa_idx_k_g"][0:1, :], 64)
        kb, Bkb = self.bcast_row("kb", I["dsa_idx_k_b"][0:1, :], 64)
        qiT = P.sbuf("qiT", [128, 4, S], BF16)
        kiT = P.sbuf("kiT", [128, S], BF16)
        wis = P.sbuf("wis", [128, NT, 8], F32)
        BqiT, BkiT, Bwis = Buf(), Buf(), Buf()
        qiR = P.sbuf("qiR", [128, 512], F32)
        kiN = P.sbuf("kiN", [128, 64], F32)
        kiR = P.sbuf("kiR", [128, 128], F32)
        BqiR, BkiN, BkiR = Buf(), Buf(), Buf()
        st, Bst = self.ln_scratch("kln")
        stats, mv, rstd, nmr = st
        for i in range(NT):
            tsl = slice(i * 128, (i + 1) * 128)
            Bx = [self.BxT[i]]
            pq, Bpq = banks.get()
            for c in range(8):
                P.pe(lambda e, c=c, pq=pq: e.matmul(pq[:, 0:512], lhsT=xT[:, c, tsl], rhs=wi_[:, c, 0:512],
                                                    start=(c == 0), stop=(c == 7)), reads=[Bw] + Bx, writes=[Bpq])
            pk, Bpk = banks.get()
            for c in range(8):
                P.pe(lambda e, c=c, pk=pk: e.matmul(pk[:, 0:72], lhsT=xT[:, c, tsl], rhs=wi_[:, c, 512:584],
                                                    start=(c == 0), stop=(c == 7)), reads=[Bw] + Bx, writes=[Bpk])
            self.rope(pq[:, 0:512], Bpq, qiR[:, :], BqiR, 8, cosT, sinT, Btab, i, tmp, Btmp)
            P.dve(lambda e, pk=pk, i=i: e.tensor_scalar(out=wis[:, i, :], in0=pk[:, 64:72],
                                                        scalar1=(8.0 ** -0.5) * (64.0 ** -0.5), scalar2=None, op0=ALU.mult),
                  reads=[Bpk], writes=[Bwis])
            P.dve(lambda e, pk=pk: e.bn_stats(out=stats[:, 0:6], in_=pk[:, 0:64]), reads=[Bpk], writes=[Bst])
            P.dve(lambda e: e.bn_aggr(out=mv[:], in_=stats[:, 0:6]), reads=[Bst], writes=[Bst])
            P.act(lambda e: e.activation(out=rstd[:], in_=mv[:, 1:2], func=AF.Ln, bias=LN_EPS, scale=1.0),
                  reads=[Bst], writes=[Bst], cost=0.2)
            P.act(lambda e: e.activation(out=rstd[:], in_=rstd[:], func=AF.Exp, scale=-0.5),
                  reads=[Bst], writes=[Bst], cost=0.2)
            P.dve(lambda e, pk=pk: e.tensor_scalar(out=kiN[:], in0=pk[:, 0:64], scalar1=mv[:, 0:1], scalar2=rstd[:],
                                                   op0=ALU.subtract, op1=ALU.mult), reads=[Bpk, Bst], writes=[BkiN])
            P.dve(lambda e: e.tensor_tensor(out=kiN[:], in0=kiN[:], in1=kg[:], op=ALU.mult), reads=[BkiN, Bkg], writes=[BkiN])
            P.dve(lambda e: e.tensor_tensor(out=kiN[:], in0=kiN[:], in1=kb[:], op=ALU.add), reads=[BkiN, Bkb], writes=[BkiN])
            self.rope(kiN[:, :], BkiN, kiR[:, 0:64], BkiR, 1, cosT, sinT, Btab, i, tmp, Btmp)
            P.dve(lambda e: e.tensor_copy(out=kiR[:, 64:128], in_=kiR[:, 0:64]), reads=[BkiR], writes=[BkiR])
            for c in range(4):
                P.pe(lambda e, c=c: e.transpose(out=psT[:, c * 128:(c + 1) * 128], in_=qiR[:, c * 128:(c + 1) * 128],
                                                identity=ident[:]), reads=[BqiR, self.Bc], writes=[BpsT])
            P.pe(lambda e: e.transpose(out=psT[:, 512:640], in_=kiR[:, :], identity=ident[:]),
                 reads=[BkiR, self.Bc], writes=[BpsT])
            P.act(lambda e, tsl=tsl: e.copy(out=qiT[:, :, tsl], in_=psT[:, 0:512].rearrange("p (c t) -> p c t", c=4)),
                  reads=[BpsT], writes=[BqiT])
            P.act(lambda e, tsl=tsl: e.copy(out=kiT[:, tsl], in_=psT[:, 512:640]), reads=[BpsT], writes=[BkiT])
        acc2 = [P.sbuf("acc", [128, S], F32) for _ in range(4)]
        Bacc2 = [Buf() for _ in range(4)]
        junk = P.sbuf("junk", [128, S], BF16)
        Bjunk = Buf()
        mk = P.sbuf("mk", [128, S], F32)
        Bmk = Buf()
        bs2 = [P.sbuf("bs", [128, 8], F32) for _ in range(2)]
        Bbs2 = [Buf(), Buf()]
        rrc = [0]
        NIT = 24

        dg2 = [P.sbuf("dg", [128, 8, 128], F32) for _ in range(2)]
        Bdg2 = [Buf(), Buf()]
        rl = [P.sbuf("rl", [128, 512], F32) for _ in range(4)]
        Brl = [Buf() for _ in range(4)]

        def scores(i, slot):
            acc, Bacc = acc2[slot], Bacc2[slot]
            dg, Bdg = dg2[slot % 2], Bdg2[slot % 2]
            Wd = (i + 1) * 128
            tsl = slice(i * 128, (i + 1) * 128)
            npc = (Wd + 511) // 512
            for h in range(8):
                P.dve(lambda e: e.tensor_scalar(out=dg[:, h, :], in0=ident[:], scalar1=wis[:, i, h:h + 1], scalar2=None,
                                                op0=ALU.mult), reads=[self.Bc, Bwis], writes=[Bdg], cost=0.15)
            for n in range(npc):
                c0, c1 = n * 512, min(Wd, (n + 1) * 512)
                pacc, Bpacc = banks.fixed(n % 2)
                for h in range(8):
                    hp, hc = (h % 2) * 64, h // 2
                    ps, Bp = banks.get()
                    P.pe(lambda e: e.matmul(ps[:, 0:c1 - c0], lhsT=qiT[hp:hp + 64, hc, tsl], rhs=kiT[hp:hp + 64, c0:c1],
                                            start=True, stop=True), reads=[BqiT, BkiT], writes=[Bp])
                    r_ = rrc[0]
                    rrc[0] = (r_ + 1) % 4
                    P.act(lambda e: e.activation(out=rl[r_][:, 0:c1 - c0], in_=ps[:, 0:c1 - c0], func=AF.Relu),
                          reads=[Bp], writes=[Brl[r_]])
                    last = (h == 7) and (n != npc - 1)
                    P.pe(lambda e: e.matmul(pacc[:, 0:c1 - c0], lhsT=dg[:, h, :], rhs=rl[r_][:, 0:c1 - c0],
                                            start=(h == 0), stop=last), reads=[Bdg, Brl[r_]], writes=[Bpacc], cost=0.9)
                if n == npc - 1:
                    off = i * 128 - c0
                    P.pe(lambda e: e.matmul(pacc[:, off:off + 128], lhsT=ident[:], rhs=negm[:], start=False, stop=True),
                         reads=[self.Bc, Bk], writes=[Bpacc], cost=0.4)
                P.act(lambda e: e.copy(out=acc[:, c0:c1], in_=pacc[:, 0:c1 - c0]), reads=[Bpacc], writes=[Bacc])

        def bis_init(i, slot):
            acc, Bacc, bs, Bbs = acc2[slot], Bacc2[slot], bs2[slot % 2], Bbs2[slot % 2]
            Wd = (i + 1) * 128
            P.dve(lambda e: e.reduce_max(out=bs[:, 5:6], in_=acc[:, 0:Wd], axis=AX.X), reads=[Bacc], writes=[Bbs])
            P.dve(lambda e: e.tensor_reduce(out=bs[:, 0:1], in_=acc[:, 0:i * 128], axis=AX.X, op=ALU.min),
                  reads=[Bacc], writes=[Bbs])
            P.dve(lambda e: e.tensor_tensor(out=bs[:, 1:2], in0=bs[:, 5:6], in1=bs[:, 0:1], op=ALU.subtract),
                  reads=[Bbs], writes=[Bbs])

        def bis_iter(i, slot, k):
            acc, Bacc, bs, Bbs = acc2[slot], Bacc2[slot], bs2[slot % 2], Bbs2[slot % 2]
            Wd = (i + 1) * 128
            ck = 2.0 ** -(k + 1)
            P.dve(lambda e: e.scalar_tensor_tensor(out=bs[:, 2:3], in0=bs[:, 1:2], scalar=ck, in1=bs[:, 0:1],
                                                   op0=ALU.mult, op1=ALU.add), reads=[Bbs], writes=[Bbs], cost=0.12)
            P.dve(lambda e: e.tensor_scalar(out=junk[:, 0:Wd], in0=acc[:, 0:Wd], scalar1=bs[:, 2:3], scalar2=0.0,
                                            op0=ALU.is_ge, op1=ALU.add, accum_out=bs[:, 3:4]),
                  reads=[Bacc, Bbs], writes=[Bbs, Bjunk], cost=Wd / 960.0 + 0.15)
            P.dve(lambda e: e.tensor_scalar(out=bs[:, 4:5], in0=bs[:, 3:4], scalar1=255.5, scalar2=ck,
                                            op0=ALU.is_ge, op1=ALU.mult), reads=[Bbs], writes=[Bbs], cost=0.12)
            P.dve(lambda e: e.scalar_tensor_tensor(out=bs[:, 0:1], in0=bs[:, 4:5], scalar=bs[:, 1:2], in1=bs[:, 0:1],
                                                   op0=ALU.mult, op1=ALU.add), reads=[Bbs], writes=[Bbs], cost=0.12)

        def finish_tile(i, slot, use_thr):
            acc, Bacc, bs, Bbs = acc2[slot], Bacc2[slot], bs2[slot % 2], Bbs2[slot % 2]
            Wd = (i + 1) * 128
            if use_thr:
                P.dve(lambda e: e.tensor_scalar(out=mk[:, 0:Wd], in0=acc[:, 0:Wd], scalar1=bs[:, 0:1], scalar2=None,
                                                op0=ALU.is_ge), reads=[Bacc, Bbs], writes=[Bmk])
            else:
                P.dve(lambda e: e.tensor_scalar(out=mk[:, 0:Wd], in0=acc[:, 0:Wd], scalar1=-1.0e29, scalar2=None,
                                                op0=ALU.is_ge), reads=[Bacc], writes=[Bmk])
            for j0 in range(0, i + 1, 8):
                js = list(range(j0, min(i + 1, j0 + 8)))
                for j in js:
                    P.pe(lambda e: e.transpose(out=psT[:, (j - j0) * 128:(j - j0 + 1) * 128],
                                               in_=mk[:, j * 128:(j + 1) * 128], identity=ident[:]),
                         reads=[Bmk, self.Bc], writes=[BpsT])
                for j in js:
                    P.act(lambda e: e.copy(out=maskT[j][:, (i - j) * 128:(i - j + 1) * 128],
                                           in_=psT[:, (j - j0) * 128:(j - j0 + 1) * 128]),
                          reads=[BpsT], writes=[BmT[j]])

        pairs = [(ia, ia + 1) for ia in range(2, NT, 2)]
        for i in range(2):
            scores(i, i)
        scores(pairs[0][0], 2)
        scores(pairs[0][1], 3)
        for i in range(2):
            finish_tile(i, i, False)
        for pi_, (ia, ib) in enumerate(pairs):
            sa, sb = (2, 3) if pi_ % 2 == 0 else (0, 1)
            if pi_ + 1 < len(pairs):
                na, nb = (0, 1) if pi_ % 2 == 0 else (2, 3)
                scores(pairs[pi_ + 1][0], na)
                scores(pairs[pi_ + 1][1], nb)
            bis_init(ia, sa)
            bis_init(ib, sb)
            for k in range(NIT):
                bis_iter(ia, sa, k)
                bis_iter(ib, sb, k)
            finish_tile(ia, sa, True)
            finish_tile(ib, sb, True)
    with P.scope():
        sets = []
        w_shared = P.sbuf("dsw2", [128, 8, 768], BF16)
        Bw_shared = Buf()
        for k_ in range(2):
            d = dict(w=w_shared, Bw=Bw_shared,
                     qT=P.sbuf("qT", [128, 2, S], BF16), kT=P.sbuf("kT", [128, 2, S], BF16),
                     va=P.sbuf("va", [128, NT, 4, 128], BF16),
                     BqT=[Buf() for _ in range(NT)], BkT=[Buf() for _ in range(NT)], Bva=[Buf() for _ in range(NT)],
                     qR=P.sbuf("qR", [128, 512], BF16), BqR=Buf())
            va_ = d["va"]
            P.dve(lambda e: e.memset(va_[:], 1.0), writes=d["Bva"])
            sets.append(d)
        tmp2 = [tmp, tmp]
        Btmp2 = [Btmp, Btmp]
        NR = 4
        LOOK = 3
        pe_ = [P.sbuf("pe_", [128, 512], BF16) for _ in range(NR)]
        pm_ = [P.sbuf("pm_", [128, 512], BF16) for _ in range(NR)]
        Bpe = [Buf() for _ in range(NR)]
        Bpm = [Buf() for _ in range(NR)]
        rec = [P.sbuf("arec", [64, 512], F32) for _ in range(2)]
        Brec = [Buf(), Buf()]
        blocks = [(h, tt, j) for h in range(4) for tt in range(4) for j in range(4 * tt + 4)]
        ropecnt = [0]

        def load_group(hg):
            d = sets[hg % 2]
            w2_ = d["w"]
            for part in range(3):
                P.dma("pool", w2_[:, :, part * 256:(part + 1) * 256],
                      W[:, part * 768 + hg * 256:part * 768 + (hg + 1) * 256].rearrange("(c p) f -> p c f", p=128),
                      writes=[d["Bw"]])

        def proj_tile(hg, i):
            d = sets[hg % 2]
            w2_, qT, kT, va, qR, BqR, Bw = d["w"], d["qT"], d["kT"], d["va"], d["qR"], d["BqR"], d["Bw"]
            tsl = slice(i * 128, (i + 1) * 128)
            Bx = [self.BxT[i]]
            for part in range(2):
                pq, Bpq = banks.get()
                for c in range(8):
                    P.pe(lambda e: e.matmul(pq[:, 0:256], lhsT=xT[:, c, tsl], rhs=w2_[:, c, part * 256:(part + 1) * 256],
                                            start=(c == 0), stop=(c == 7)), reads=[Bw] + Bx, writes=[Bpq])
                tk = ropecnt[0] % 2
                ropecnt[0] += 1
                self.rope(pq[:, 0:256], Bpq, qR[:, part * 256:(part + 1) * 256], BqR, 4, cosT, sinT, Btab, i,
                          tmp2[tk], Btmp2[tk])
            pv, Bpv = banks.get()
            for c in range(8):
                P.pe(lambda e: e.matmul(pv[:, 0:256], lhsT=xT[:, c, tsl], rhs=w2_[:, c, 512:768],
                                        start=(c == 0), stop=(c == 7)), reads=[Bw] + Bx, writes=[Bpv])
            P.act(lambda e: e.copy(out=va[:, i, :, 0:64], in_=pv[:, 0:256].rearrange("p (h d) -> p h d", h=4)),
                  reads=[Bpv], writes=[d["Bva"][i]])
            psTb = psT[:].bitcast(BF16)
            for c in range(4):
                P.pe(lambda e: e.transpose(out=psTb[:, c * 128:(c + 1) * 128], in_=qR[:, c * 128:(c + 1) * 128],
                                           identity=self.identb[:]), reads=[BqR, self.Bc], writes=[BpsT])
            P.act(lambda e: e.copy(out=qT[:, :, tsl], in_=psTb[:, 0:256].rearrange("p (c t) -> p c t", c=2)),
                  reads=[BpsT], writes=[d["BqT"][i]])
            P.act(lambda e: e.copy(out=kT[:, :, tsl], in_=psTb[:, 256:512].rearrange("p (c t) -> p c t", c=2)),
                  reads=[BpsT], writes=[d["BkT"][i]])

        def stage_a(hg, idx):
            d = sets[hg % 2]
            qT, kT = d["qT"], d["kT"]
            h, tt, j = blocks[idx]
            hp, hc = (h % 2) * 64, h // 2
            t0 = max(tt * 512, j * 128)
            c0 = t0 - tt * 512
            t1 = (tt + 1) * 512
            r_ = idx % NR
            ps, Bp = banks.get()
            P.pe(lambda e: e.matmul(ps[:, c0:512], lhsT=kT[hp:hp + 64, hc, j * 128:(j + 1) * 128],
                                    rhs=qT[hp:hp + 64, hc, t0:t1], start=True, stop=True),
                 reads=[d["BkT"][j]] + d["BqT"][tt * 4:tt * 4 + 4], writes=[Bp])
            P.act(lambda e: e.activation(out=pe_[r_][:, c0:512], in_=ps[:, c0:512], func=AF.Exp, scale=0.125),
                  reads=[Bp], writes=[Bpe[r_]])
            P.dve(lambda e: e.tensor_tensor(out=pm_[r_][:, c0:512], in0=pe_[r_][:, c0:512],
                                            in1=maskT[j][:, t0 - j * 128:t1 - j * 128], op=ALU.mult),
                  reads=[Bpe[r_], BmT[j]], writes=[Bpm[r_]])

        def stage_b(hg, idx):
            d = sets[hg % 2]
            va = d["va"]
            h, tt, j = blocks[idx]
            g = h * 4 + tt
            nblk = 4 * tt + 4
            t0 = max(tt * 512, j * 128)
            c0 = t0 - tt * 512
            r_ = idx % NR
            po, Bpo = banks.fixed(g % 2)
            P.pe(lambda e: e.matmul(po[:, c0:512], lhsT=va[:, j, h, :], rhs=pm_[r_][:, c0:512],
                                    start=(j == 0), stop=(j == nblk - 1)), reads=[d["Bva"][j], Bpm[r_]], writes=[Bpo])
            if j == nblk - 1:
                gh = hg * 4 + h
                ghp, gch = (gh % 2) * 64, gh // 2
                rc, Brc = rec[g % 2], Brec[g % 2]
                P.act(lambda e: e.activation(out=rc[:], in_=po[64:128, :], func=AF.Ln), reads=[Bpo], writes=[Brc])
                P.act(lambda e: e.activation(out=rc[:], in_=rc[:], func=AF.Exp, scale=-1.0), reads=[Brc], writes=[Brc])
                P.dve(lambda e: e.tensor_tensor(out=catT[ghp:ghp + 64, gch, tt * 512:(tt + 1) * 512],
                                                in0=po[0:64, :], in1=rc[:], op=ALU.mult),
                      reads=[Bpo, Brc], writes=[BcatT])

        load_group(0)
        for i in range(NT):
            proj_tile(0, i)
        for hg in range(3):
            nxt = hg + 1 if hg + 1 < 3 else None
            if nxt is not None:
                load_group(nxt)
            nsteps = len(blocks) + LOOK
            every = nsteps // NT
            pi_ = 0
            for idx in range(nsteps):
                if idx < len(blocks):
                    stage_a(hg, idx)
                if idx >= LOOK:
                    stage_b(hg, idx - LOOK)
                if nxt is not None and idx % every == every - 1 and pi_ < NT:
                    proj_tile(nxt, pi_)
                    pi_ += 1
            if nxt is not None:
                while pi_ < NT:
                    proj_tile(nxt, pi_)
                    pi_ += 1


def k_layer(self, li, b, first, last, after_experts=None):
    P, I = self.P, self.I
    with P.scope():
        catT = P.sbuf("catT", [128, 8, S], BF16)
        BcatT = Buf()
        banks = Banks(P, 6)
        with P.scope():
            if li % 2 == 0:
                self.dsa(b, banks, catT, BcatT)
                w_in, qm_off = I["dsa_w_in"][0], 2888
            else:
                self.gla(b, banks, catT, BcatT)
                w_in, qm_off = I["gla_w_in"][0], 2320
        with P.scope():
          self.mem_attn(li, b, banks, w_in, qm_off, catT, BcatT)
          if self.dbgc is not None:
            if True:
                stg = [P.sbuf("stg", [128, 512], F32) for _ in range(2)]
                Bstg = [Buf(), Buf()]
                kk = 0
                for c in range(8):
                    for tt in range(4):
                        p = kk % 2
                        kk += 1
                        P.dve(lambda e, p=p, c=c, tt=tt: e.tensor_copy(out=stg[p][:], in_=catT[:, c, tt * 512:(tt + 1) * 512]),
                              reads=[BcatT], writes=[Bstg[p]])
                        P.dma("sp", self.dbgc[c * 128:(c + 1) * 128, tt * 512:(tt + 1) * 512], stg[p][:], reads=[Bstg[p]])
          if first:
              xsrc, Bxsrc = I["x"][b], [Buf() for _ in range(NT)]
          else:
              xsrc, Bxsrc = self.x2s, self.Bx2s
          self.xphase(li, b, banks, catT, BcatT, xsrc, Bxsrc)
    self.moe(li, b, last, after_experts)


K.rope_tables = k_rope_tables
K._rope_tables_body = k_rope_tables_body
K.rope = k_rope
K.dsa = k_dsa
K.layer = k_layer


def build_full(nseq=NB_PER_CORE, layers=(0, 1)):
    k = K(nseq=nseq)
    k.consts()
    k.load_xT(0)
    for b in range(nseq):
        for li in layers:
            nxt = None
            if li == layers[-1] and b + 1 < nseq:
                nxt = (lambda bb=b + 1: k.load_xT(bb))
            k.layer(li, b, li == layers[0], li == layers[-1], nxt)
        k.P.barrier()
    k.P.finish()
    return k.nc


_NC_CACHE = {}


def kernel(**inputs):
    n = 8
    if "nc" not in _NC_CACHE:
        _NC_CACHE["nc"] = build_full()
    nc = _NC_CACHE["nc"]
    x = np.ascontiguousarray(inputs["x"], dtype=np.float32)
    mem = np.asarray(inputs["mem"], dtype=np.float32)
    pos = np.asarray(inputs["positions"], dtype=np.int32)
    shared = {k_: np.ascontiguousarray(v) for k_, v in inputs.items() if k_ not in ("x", "mem", "positions")}
    in_maps = []
    for c in range(n):
        sl = slice(c * NB_PER_CORE, (c + 1) * NB_PER_CORE)
        m = dict(shared)
        m["x"] = x[sl]
        m["xT"] = np.ascontiguousarray(x[sl].transpose(0, 2, 1))
        m["memT"] = np.ascontiguousarray(mem[sl].transpose(0, 2, 1))
        m["pos"] = np.ascontiguousarray(pos[sl].reshape(NB_PER_CORE, NT, 128).transpose(0, 2, 1))
        in_maps.append(m)
    res = run_bass_kernel_spmd(nc, in_maps, core_ids=list(range(n)))
    return np.concatenate([r["out"] for r in res.results], axis=0).astype(np.float32)
```

```python
import contextlib
import math
import types
import numpy as np
import concourse.bass as bass
import concourse.mybir as mybir
from concourse.bass_utils import run_bass_kernel_spmd

F32 = mybir.dt.float32
BF16 = mybir.dt.bfloat16
I32 = mybir.dt.int32
AF = mybir.ActivationFunctionType
ALU = mybir.AluOpType
AX = mybir.AxisListType

ENGS = ("pe", "act", "dve", "pool", "sp")

D = 1024
S = 2048
NT = S // 128
NB_PER_CORE = 2
DEPTH = 2
ALPHA = (2 * DEPTH) ** 0.25
LN_EPS = 1e-5
RMS_EPS = 1e-6
DSA_IN = 3144
GLA_IN = 2576
NEG = -1.0e30
MOE_MUL_ENG = "pool"
SCHED = True
SCHED_WINDOW = 96


class Buf:
    __slots__ = ("name", "writer", "readers")

    def __init__(self, name=""):
        self.name = name
        self.writer = None
        self.readers = []


class Op:
    __slots__ = ("eng", "fn", "deps", "is_dma", "signal", "sigval", "lane", "laneval",
                 "lane_prev", "barriered", "gid", "cost", "t0", "t1")

    def __init__(self, eng, fn, is_dma):
        self.eng = eng
        self.fn = fn
        self.deps = []
        self.is_dma = is_dma
        self.signal = False
        self.sigval = 0
        self.lane = None
        self.laneval = 0
        self.lane_prev = 0
        self.barriered = False
        self.gid = 0
        self.cost = None
        self.t0 = None
        self.t1 = None


def _freeze(fn):
    if fn is None or fn.__closure__ is None:
        return fn
    cells = []
    for c in fn.__closure__:
        try:
            cells.append(types.CellType(c.cell_contents))
        except ValueError:
            cells.append(c)
    return types.FunctionType(fn.__code__, fn.__globals__, fn.__name__, fn.__defaults__, tuple(cells))


class Prog:
    def __init__(self, nc):
        self.nc = nc
        self.ops = {e: [] for e in ENGS}
        self.stacks = [contextlib.ExitStack()]
        self.n_lanes = {"sp": 16, "pool": 12, "act": 8}
        self._uid = 0
        self.seq = []

    def sbuf(self, name, shape, dtype):
        self._uid += 1
        return self.stacks[-1].enter_context(
            self.nc.sbuf_tensor(f"{name}_{self._uid}", list(shape), dtype))

    def psum(self, name, shape, dtype=F32):
        self._uid += 1
        return self.stacks[-1].enter_context(
            self.nc.psum_tensor(f"{name}_{self._uid}", list(shape), dtype))

    @contextlib.contextmanager
    def scope(self):
        self.stacks.append(contextlib.ExitStack())
        try:
            yield
        finally:
            self.barrier()
            self.stacks.pop().close()

    def op(self, eng, fn, reads=(), writes=(), dma=False, cost=None):
        o = Op(eng, _freeze(fn), dma)
        o.cost = cost
        o.gid = len(self.seq)
        seen = set()
        for b in reads:
            if b.writer is not None and id(b.writer) not in seen:
                o.deps.append((b.writer, "raw"))
                seen.add(id(b.writer))
        for b in writes:
            if b.writer is not None and id(b.writer) not in seen:
                o.deps.append((b.writer, "waw"))
                seen.add(id(b.writer))
            for r in b.readers:
                if id(r) not in seen:
                    o.deps.append((r, "war"))
                    seen.add(id(r))
        for b in reads:
            b.readers.append(o)
        for b in writes:
            b.writer = o
            b.readers = []
        self.seq.append(o)
        return o

    def pe(self, fn, reads=(), writes=(), cost=None):
        return self.op("pe", fn, reads, writes, cost=cost)

    def act(self, fn, reads=(), writes=(), cost=None):
        return self.op("act", fn, reads, writes, cost=cost)

    def dve(self, fn, reads=(), writes=(), cost=None):
        return self.op("dve", fn, reads, writes, cost=cost)

    def pool(self, fn, reads=(), writes=(), cost=None):
        return self.op("pool", fn, reads, writes, cost=cost)

    def dma(self, eng, out, in_, reads=(), writes=(), **kw):
        return self.op(eng, lambda e: e.dma_start(out=out, in_=in_, **kw), reads, writes, dma=True)

    def barrier(self):
        self.seq.append(None)

    def _materialise_fence(self):
        lasts = []
        for e in ENGS:
            for o in reversed(self.ops[e]):
                if o.fn is not None and not o.is_dma:
                    lasts.append(o)
                    break
        dmas = [o for e in ENGS for o in self.ops[e] if o.is_dma and not o.barriered]
        for e in ENGS:
            o = Op(e, None, False)
            for l in lasts:
                if l.eng != e:
                    o.deps.append((l, "raw"))
            for d in dmas:
                o.deps.append((d, "raw"))
            self.ops[e].append(o)
        for d in dmas:
            d.barriered = True

    DEFCOST = {"pe": 0.3, "act": 0.5, "dve": 0.6, "pool": 0.9, "sp": 0.05}

    def _schedule_segment(self, seg):
        if not SCHED or len(seg) < 3:
            for o in seg:
                self.ops[o.eng].append(o)
            return
        inseg = set(id(o) for o in seg)
        queues = {e: [o for o in seg if o.eng == e] for e in ENGS}
        heads = {e: 0 for e in ENGS}
        done = set()
        free = {e: 0.0 for e in ENGS}
        remaining = len(seg)
        SYNC = 2.0
        while remaining:
            best = None
            for e in ENGS:
                q = queues[e]
                h = heads[e]
                while h < len(q) and id(q[h]) in done:
                    h += 1
                heads[e] = h
                if h >= len(q):
                    continue
                lim = min(len(q), h + SCHED_WINDOW)
                for k in range(h, lim):
                    o = q[k]
                    if id(o) in done:
                        continue
                    rt = 0.0
                    ok = True
                    for d, kind in o.deps:
                        if id(d) not in inseg:
                            continue
                        if id(d) not in done:
                            ok = False
                            break
                        if d.eng == o.eng and not d.is_dma and (o.eng == "pe" or kind != "raw"):
                            t = d.t0
                        else:
                            t = d.t1 + SYNC
                        if t > rt:
                            rt = t
                    if not ok:
                        continue
                    st = rt if rt > free[e] else free[e]
                    key = (st, o.gid)
                    if best is None or key < best[0]:
                        best = (key, e, o)
                    if st <= free[e]:
                        break
            assert best is not None, "scheduler deadlock"
            (st, _), e, o = best
            c = o.cost if o.cost is not None else (3.0 if o.is_dma else self.DEFCOST[e])
            o.t0 = st
            if o.is_dma:
                o.t1 = st + c
                free[e] = st + 0.1
            else:
                o.t1 = st + c
                free[e] = o.t1
            done.add(id(o))
            self.ops[e].append(o)
            remaining -= 1

    def _schedule(self):
        seg = []
        for it in self.seq:
            if it is None:
                self._schedule_segment(seg)
                seg = []
                self._materialise_fence()
            else:
                seg.append(it)
        self._schedule_segment(seg)

    def emit(self):
        nc = self.nc

        def needs_wait(o, d, kind):
            return not ((not d.is_dma) and d.eng == o.eng and (o.eng == "pe" or kind != "raw"))

        for e in ENGS:
            for o in self.ops[e]:
                for d, kind in o.deps:
                    if needs_wait(o, d, kind) and not d.is_dma:
                        d.signal = True
        for e in ENGS:
            cnt = 0
            lane_vals = [0] * self.n_lanes.get(e, 1)
            li = 0
            for o in self.ops[e]:
                if o.is_dma:
                    o.lane = li
                    o.lane_prev = lane_vals[li]
                    lane_vals[li] += 16
                    o.laneval = lane_vals[li]
                    li = (li + 1) % len(lane_vals)
                elif o.signal:
                    cnt += 1
                    o.sigval = cnt
        with contextlib.ExitStack() as st:
            esem = {e: st.enter_context(nc.semaphore(f"s_{e}")) for e in ("pe", "act", "dve", "pool")}
            lsem = {e: [st.enter_context(nc.semaphore(f"l_{e}{i}")) for i in range(n)]
                    for e, n in self.n_lanes.items()}
            block = st.enter_context(nc.Block())

            def run(ename, eng):
                seen = {}

                def wait(sem, key, val):
                    if seen.get(key, 0) >= val:
                        return
                    seen[key] = val
                    eng.wait_ge(sem, val)

                for o in self.ops[ename]:
                    for d, kind in o.deps:
                        if not needs_wait(o, d, kind):
                            continue
                        if d.is_dma:
                            wait(lsem[d.eng][d.lane], ("l", d.eng, d.lane), d.laneval)
                        else:
                            wait(esem[d.eng], ("e", d.eng), d.sigval)
                    if o.fn is None:
                        continue
                    if o.is_dma:
                        if o.lane_prev > 0:
                            wait(lsem[ename][o.lane], ("l", ename, o.lane), o.lane_prev)
                        o.fn(eng).then_inc(lsem[ename][o.lane], 16)
                    else:
                        ins = o.fn(eng)
                        if o.signal:
                            ins.then_inc(esem[ename], 1)

            @block.tensor
            def _(eng):
                run("pe", eng)

            @block.scalar
            def _(eng):
                run("act", eng)

            @block.vector
            def _(eng):
                run("dve", eng)

            @block.gpsimd
            def _(eng):
                run("pool", eng)

            @block.sync
            def _(eng):
                run("sp", eng)

    def finish(self):
        self._schedule()
        fin = Op("sp", None, False)
        for e in ENGS:
            for o in self.ops[e]:
                if o.is_dma:
                    fin.deps.append((o, "raw"))
        fin.deps.sort(key=lambda t: t[0].laneval)
        self.ops["sp"].append(fin)
        self.emit()
        while self.stacks:
            self.stacks.pop().close()


class K:
    def __init__(self, nseq=NB_PER_CORE, debug=None):
        self.nseq = nseq
        self.debug = debug
        nc = bass.Bass("TRN2", target_bir_lowering=False)
        self.nc = nc
        self.P = Prog(nc)
        dt = nc.dram_tensor

        def inp(name, shape, dtype=F32):
            return dt(name, list(shape), dtype, kind="ExternalInput").ap()

        I = {}
        I["x"] = inp("x", [nseq, S, D])
        I["xT"] = inp("xT", [nseq, D, S])
        I["memT"] = inp("memT", [nseq, D, 256])
        I["pos"] = inp("pos", [nseq, 128, NT], I32)
        I["dsa_w_in"] = inp("dsa_w_in", [1, D, DSA_IN])
        I["dsa_idx_k_g"] = inp("dsa_idx_k_g", [1, 64])
        I["dsa_idx_k_b"] = inp("dsa_idx_k_b", [1, 64])
        I["gla_w_in"] = inp("gla_w_in", [1, D, GLA_IN])
        I["gla_w_gate"] = inp("gla_w_gate", [1, 16, 384])
        I["gla_b_gate"] = inp("gla_b_gate", [1, 384])
        I["gla_norm_g"] = inp("gla_norm_g", [1, 192])
        I["w_mem_kv"] = inp("w_mem_kv", [2, D, 512])
        I["w_out"] = inp("w_out", [2, D, D])
        for n in ("ln1_g", "ln1_b", "ln2_g", "ln2_b"):
            I[n] = inp(n, [2, D])
        I["moe_w_group"] = inp("moe_w_group", [2, D, 4])
        I["moe_b_group"] = inp("moe_b_group", [2, 4])
        I["moe_w_router"] = inp("moe_w_router", [2, 4, D, 8])
        I["moe_b_router"] = inp("moe_b_router", [2, 4, 8])
        I["moe_w13"] = inp("moe_w13", [2, 4, 8, D, 512])
        I["moe_w2"] = inp("moe_w2", [2, 4, 8, 256, D])
        self.I = I
        self.out = dt("out", [nseq, S, D], F32, kind="ExternalOutput").ap()
        import os
        self.dbg = dt("dbg", [S, D], F32, kind="ExternalOutput").ap() if os.environ.get("KDBG") else None
        self.dbgc = dt("dbgc", [D, S], F32, kind="ExternalOutput").ap() if os.environ.get("KDBG") else None
        self.x1s = dt("x1s", [S, D], F32).ap()
        self.x2s = dt("x2s", [S, D], F32).ap()
        self.combT_d = dt("combT_d", [32, S], F32).ap()
        self.Bx1s = [Buf() for _ in range(NT)]
        self.Bx2s = [Buf() for _ in range(NT)]
        self.Bcomb = [Buf() for _ in range(NT)]

    def consts(self):
        P, nc = self.P, self.nc
        self.ident = P.sbuf("ident", [128, 128], F32)
        self.identb = P.sbuf("identb", [128, 128], BF16)
        self.Bc = Buf("consts")
        B = self.Bc
        ident, identb = self.ident, self.identb
        P.pool(lambda e: e.memset(ident[:], 1.0), writes=[B])
        P.pool(lambda e: e.affine_select(out=ident[:], in_=ident[:], compare_op=ALU.is_equal, fill=0.0,
                                         base=0, pattern=[[-1, 128]], channel_multiplier=1),
               reads=[B], writes=[B])
        P.pool(lambda e: e.tensor_copy(out=identb[:], in_=ident[:]), reads=[B], writes=[B])
        self.xT_bf = P.sbuf("xT_bf", [128, 8, S], BF16)
        self.BxT = [Buf() for _ in range(NT)]
        self.x1T_bf = self.xT_bf
        self.Bx1T = self.BxT
        self.psT_extra = []
        self.psT = [P.psum("psT", [128, 1024]) for _ in range(1)]
        self.BpsT = [Buf() for _ in range(1)]
        self.comb_all = P.sbuf("comb_all", [128, NT, 32], F32)

    def bcast_row(self, name, src_row_ap, n, eng="sp"):
        t = self.P.sbuf(name, [128, n], F32)
        B = Buf(name)
        self.P.dma(eng, t[:], src_row_ap.partition_broadcast(128), writes=[B])
        return t, B

    def layernorm_tile(self, z, Bz, outt, Bo, g, Bg, bt, Bb, st, Bst):
        P = self.P
        stats, mv, rstd, nmr = st
        P.dve(lambda e: e.bn_stats(out=stats[:, 0:6], in_=z[:, 0:512]), reads=[Bz], writes=[Bst])
        P.dve(lambda e: e.bn_stats(out=stats[:, 6:12], in_=z[:, 512:1024]), reads=[Bz], writes=[Bst])
        P.dve(lambda e: e.bn_aggr(out=mv[:], in_=stats[:]), reads=[Bst], writes=[Bst])
        P.act(lambda e: e.activation(out=rstd[:], in_=mv[:, 1:2], func=AF.Ln, bias=LN_EPS, scale=1.0),
              reads=[Bst], writes=[Bst], cost=0.2)
        P.act(lambda e: e.activation(out=rstd[:], in_=rstd[:], func=AF.Exp, scale=-0.5),
              reads=[Bst], writes=[Bst], cost=0.2)
        P.dve(lambda e: e.scalar_tensor_tensor(out=nmr[:], in0=mv[:, 0:1], scalar=-1.0, in1=rstd[:],
                                               op0=ALU.mult, op1=ALU.mult), reads=[Bst], writes=[Bst])
        P.act(lambda e: e.activation(out=outt[:], in_=z[:], func=AF.Identity, bias=nmr[:], scale=rstd[:]),
              reads=[Bz, Bst], writes=[Bo])
        P.dve(lambda e: e.tensor_tensor(out=outt[:], in0=outt[:], in1=g[:], op=ALU.mult),
              reads=[Bo, Bg], writes=[Bo])
        P.dve(lambda e: e.tensor_tensor(out=outt[:], in0=outt[:], in1=bt[:], op=ALU.add),
              reads=[Bo, Bb], writes=[Bo])

    def ln_scratch(self, name):
        P = self.P
        return ((P.sbuf(name + "st", [128, 12], F32), P.sbuf(name + "mv", [128, 2], F32),
                 P.sbuf(name + "rs", [128, 1], F32), P.sbuf(name + "nm", [128, 1], F32)), Buf(name))

    def router_setup(self, li):
        P, I = self.P, self.I
        self.wr = P.sbuf("wr", [128, 8, 36], F32)
        self.Bwr = Buf("wr")
        wr = self.wr
        P.dma("sp", wr[:, :, 0:4], I["moe_w_group"][li].rearrange("(c p) g -> p c g", p=128), writes=[self.Bwr])
        for g in range(4):
            P.dma("sp", wr[:, :, 4 + 8 * g:12 + 8 * g],
                  I["moe_w_router"][li, g].rearrange("(c p) e -> p c e", p=128), writes=[self.Bwr])
        self.rbias = P.sbuf("rbias", [128, 36], F32)
        self.Brb = Buf("rbias")
        P.dma("sp", self.rbias[:, 0:4], I["moe_b_group"][li:li + 1, :].partition_broadcast(128), writes=[self.Brb])
        P.dma("sp", self.rbias[:, 4:36],
              I["moe_b_router"][li:li + 1].rearrange("o g e -> o (g e)").partition_broadcast(128),
              writes=[self.Brb])
        self.rt = []
        for k in range(2):
            d = dict(
                x1Tf=P.sbuf("x1Tf", [128, 1024], F32), lg=P.sbuf("lg", [128, 36], F32),
                sm=P.sbuf("rsm", [128, 16], F32), me=P.sbuf("rme", [128, 32], F32),
                top8=P.sbuf("top8", [128, 8], F32), ex=P.sbuf("rex", [128, 32], F32),
                comb=P.sbuf("comb", [128, 32], F32), cT=P.sbuf("cT", [32, 128], F32),
                B=Buf("rt"), BxTf=Buf("x1Tf"), BcT=Buf("cT"))
            self.rt.append(d)
        self.psR = self.psT[0]
        self.BpsR = self.BpsT[0]

    def transpose_to_bf(self, src, Bsrc, dstT, BdstT, i, also_f32=None):
        P = self.P
        psT, BpsT = self.psT[0], self.BpsT[0]
        ident = self.ident
        for c in range(8):
            P.pe(lambda e, c=c: e.transpose(out=psT[:, c * 128:(c + 1) * 128], in_=src[:, c * 128:(c + 1) * 128],
                                            identity=ident[:]),
                 reads=[Bsrc, self.Bc], writes=[BpsT] + self.psT_extra)
        if also_f32 is None:
            P.dve(lambda e: e.tensor_copy(out=dstT[:, :, i * 128:(i + 1) * 128],
                                          in_=psT[:].rearrange("p (c t) -> p c t", c=8)),
                  reads=[BpsT], writes=[BdstT])
        else:
            t, Bt = also_f32
            P.act(lambda e: e.copy(out=t[:], in_=psT[:]), reads=[BpsT], writes=[Bt])
            P.dve(lambda e: e.tensor_copy(out=dstT[:, :, i * 128:(i + 1) * 128],
                                          in_=t[:].rearrange("p (c t) -> p c t", c=8)),
                  reads=[Bt], writes=[BdstT])

    def router_tile(self, i, x1, Bx1):
        P = self.P
        r = self.rt[i % 2]
        B = r["B"]
        self.transpose_to_bf(x1, Bx1, self.x1T_bf, self.Bx1T[i], i, also_f32=(r["x1Tf"], r["BxTf"]))
        import os
        STOP = int(os.environ.get("RSTOP", "99"))
        if STOP < 1:
            return
        psR, BpsR = self.psR, self.BpsR
        x1Tf, wr = r["x1Tf"], self.wr
        for c in range(8):
            P.pe(lambda e, c=c: e.matmul(psR[:, 0:36], lhsT=x1Tf[:, c * 128:(c + 1) * 128], rhs=wr[:, c, :],
                                         start=(c == 0), stop=(c == 7)),
                 reads=[r["BxTf"], self.Bwr], writes=[BpsR])
        if STOP < 2:
            return
        lg, sm, me, top8, ex, comb, cT = r["lg"], r["sm"], r["me"], r["top8"], r["ex"], r["comb"], r["cT"]
        rb = self.rbias
        P.dve(lambda e: e.tensor_tensor(out=lg[:], in0=psR[:, 0:36], in1=rb[:], op=ALU.add),
              reads=[BpsR, self.Brb], writes=[B])
        P.dve(lambda e: e.reduce_max(out=sm[:, 0:1], in_=lg[:, 0:4], axis=AX.X), reads=[B], writes=[B])
        P.dve(lambda e: e.tensor_scalar(out=sm[:, 1:2], in0=sm[:, 0:1], scalar1=-1.0, scalar2=None, op0=ALU.mult),
              reads=[B], writes=[B])
        P.act(lambda e: e.activation(out=sm[:, 12:16], in_=lg[:, 0:4], func=AF.Exp, bias=sm[:, 1:2], scale=1.0),
              reads=[B], writes=[B])
        P.dve(lambda e: e.reduce_sum(out=sm[:, 2:3], in_=sm[:, 12:16], axis=AX.X), reads=[B], writes=[B])
        P.dve(lambda e: e.tensor_scalar(out=sm[:, 4:8], in0=lg[:, 0:4], scalar1=sm[:, 0:1], scalar2=None,
                                        op0=ALU.is_lt), reads=[B], writes=[B])
        P.dve(lambda e: e.tensor_scalar(out=sm[:, 4:8], in0=sm[:, 4:8], scalar1=-30000.0, scalar2=None,
                                        op0=ALU.mult), reads=[B], writes=[B])
        for g in range(4):
            P.dve(lambda e, g=g: e.tensor_scalar(out=me[:, 8 * g:8 * g + 8], in0=lg[:, 4 + 8 * g:12 + 8 * g],
                                                 scalar1=sm[:, 4 + g:5 + g], scalar2=None, op0=ALU.add),
                  reads=[B], writes=[B])
        P.dve(lambda e: e.max(out=top8[:], in_=me[:]), reads=[B], writes=[B])
        P.dve(lambda e: e.tensor_scalar(out=sm[:, 8:9], in0=top8[:, 0:1], scalar1=-1.0, scalar2=None, op0=ALU.mult),
              reads=[B], writes=[B])
        P.act(lambda e: e.activation(out=ex[:], in_=me[:], func=AF.Exp, bias=sm[:, 8:9], scale=1.0),
              reads=[B], writes=[B])
        P.act(lambda e: e.activation(out=sm[:, 9:10], in_=top8[:, 1:2], func=AF.Exp, bias=sm[:, 8:9], scale=1.0),
              reads=[B], writes=[B])
        P.dve(lambda e: e.scalar_tensor_tensor(out=sm[:, 11:12], in0=sm[:, 9:10], scalar=1.0, in1=sm[:, 2:3],
                                               op0=ALU.add, op1=ALU.mult), reads=[B], writes=[B])
        P.dve(lambda e: e.reciprocal(out=sm[:, 10:11], in_=sm[:, 11:12]), reads=[B], writes=[B])
        P.dve(lambda e: e.scalar_tensor_tensor(out=comb[:], in0=me[:], scalar=top8[:, 1:2], in1=ex[:],
                                               op0=ALU.is_ge, op1=ALU.mult), reads=[B], writes=[B])
        call = self.comb_all
        P.dve(lambda e: e.tensor_scalar(out=call[:, i, :], in0=comb[:], scalar1=sm[:, 10:11], scalar2=None,
                                        op0=ALU.mult), reads=[B], writes=[self.Bcomb[i]])

    def moe(self, li, b, last, after_experts=None):
        P, I = self.P, self.I
        with P.scope():
            yacc = P.sbuf("yacc", [128, NT, 1024], F32)
            Byacc = [[Buf() for _ in range(2)] for _ in range(NT)]
            w13b = [P.sbuf("w13b", [128, 8, 512], BF16) for _ in range(2)]
            w2b = [P.sbuf("w2b", [128, 2, 1024], BF16) for _ in range(2)]
            Bw13 = [Buf() for _ in range(2)]
            Bw2 = [Buf() for _ in range(2)]
            sa = [P.sbuf("sa", [128, 512], F32) for _ in range(2)]
            su = [P.sbuf("su", [128, 512], F32) for _ in range(2)]
            actT = [[P.sbuf("actT", [128, 512], BF16) for _ in range(2)] for _ in range(2)]
            Bsa = [Buf() for _ in range(2)]
            Bsu = [Buf() for _ in range(2)]
            Bact = [[Buf() for _ in range(2)] for _ in range(2)]
            hps = [P.psum("hps", [128, 512]) for _ in range(4)]
            Bh = [Buf() for _ in range(4)]
            yps_t = [P.psum("yps", [128, 512]) for _ in range(2)]
            psT0 = self.psT[0]
            yps = [yps_t[0][:, :], yps_t[1][:, :], psT0[:, 0:512], psT0[:, 512:1024]]
            By = [Buf() for _ in range(4)]
            x1T = self.x1T_bf
            call = self.comb_all
            MUL_ENG = MOE_MUL_ENG

            def load_w(e):
                s = e % 2
                g, ee = divmod(e, 8)
                P.dma("pool", w13b[s][:], I["moe_w13"][li, g, ee].rearrange("(c p) f -> p c f", p=128),
                      writes=[Bw13[s]])
                P.dma("pool", w2b[s][:], I["moe_w2"][li, g, ee].rearrange("(j p) d -> p j d", p=128),
                      writes=[Bw2[s]])

            units = [(e, tt) for e in range(32) for tt in range(4)]
            yrot = [0]

            def emit_h(k):
                e, tt = units[k]
                s, par = e % 2, k % 2
                tsl = slice(tt * 512, (tt + 1) * 512)
                for fc in range(4):
                    for c in range(8):
                        P.pe(lambda en: en.matmul(hps[fc][:], lhsT=w13b[s][:, c, fc * 128:(fc + 1) * 128],
                                                  rhs=x1T[:, c, tsl], start=(c == 0), stop=(c == 7)),
                             reads=[Bw13[s]] + self.Bx1T[tt * 4:tt * 4 + 4], writes=[Bh[fc]])
                for j in range(2):
                    P.act(lambda en: en.activation(out=sa[j][:], in_=hps[j][:], func=AF.Silu),
                          reads=[Bh[j]], writes=[Bsa[j]])
                    P.act(lambda en: en.copy(out=su[j][:], in_=hps[2 + j][:]), reads=[Bh[2 + j]], writes=[Bsu[j]])
                    P.op(MUL_ENG, lambda en: en.tensor_tensor(out=actT[par][j][:], in0=sa[j][:], in1=su[j][:],
                                                              op=ALU.mult),
                         reads=[Bsa[j], Bsu[j]], writes=[Bact[par][j]])

            def emit_y(k):
                e, tt = units[k]
                s, par = e % 2, k % 2
                for tch in range(4):
                    ti = tt * 4 + tch
                    for half in range(2):
                        r = yrot[0]
                        yrot[0] = (r + 1) % 4
                        for j in range(2):
                            P.pe(lambda en: en.matmul(yps[r], lhsT=actT[par][j][:, tch * 128:(tch + 1) * 128],
                                                      rhs=w2b[s][:, j, half * 512:(half + 1) * 512],
                                                      start=(j == 0), stop=(j == 1)),
                                 reads=[Bact[par][j], Bw2[s]], writes=[By[r]])
                        dst = yacc[:, ti, half * 512:(half + 1) * 512]
                        gate = call[:, ti, e:e + 1]
                        if e == 0:
                            P.dve(lambda en: en.tensor_scalar(out=dst, in0=yps[r], scalar1=gate, scalar2=None,
                                                              op0=ALU.mult),
                                  reads=[By[r], self.Bcomb[ti]], writes=[Byacc[ti][half]])
                        else:
                            P.dve(lambda en: en.scalar_tensor_tensor(out=dst, in0=yps[r], scalar=gate, in1=dst,
                                                                     op0=ALU.mult, op1=ALU.add),
                                  reads=[By[r], Byacc[ti][half], self.Bcomb[ti]], writes=[Byacc[ti][half]])

            load_w(0)
            for k in range(len(units)):
                emit_h(k)
                if k > 0:
                    emit_y(k - 1)
                e, tt = units[k]
                if tt == 0 and e + 1 < 32:
                    load_w(e + 1)
            emit_y(len(units) - 1)
            self.psT_extra = [By[2], By[3]]
            if after_experts is not None:
                after_experts()

            g2, Bg2 = self.bcast_row("g2", I["ln2_g"][li:li + 1, :], D)
            b2, Bb2 = self.bcast_row("b2", I["ln2_b"][li:li + 1, :], D)
            st2 = [self.ln_scratch("ln2a"), self.ln_scratch("ln2b")]
            xin = [P.sbuf("xin", [128, D], F32) for _ in range(2)]
            Bxin = [Buf() for _ in range(2)]
            xo = [P.sbuf("xo", [128, D], F32) for _ in range(2)]
            Bxo = [Buf() for _ in range(2)]
            for i in range(NT):
                p = i % 2
                P.dma("sp", xin[p][:], self.x1s[i * 128:(i + 1) * 128, :], reads=[self.Bx1s[i]], writes=[Bxin[p]])
                yv = yacc[:, i, :]
                P.dve(lambda en, p=p, yv=yv: en.scalar_tensor_tensor(out=xin[p][:], in0=xin[p][:], scalar=ALPHA,
                                                                      in1=yv, op0=ALU.mult, op1=ALU.add),
                      reads=[Bxin[p]] + Byacc[i], writes=[Bxin[p]])
                st, Bst = st2[p]
                self.layernorm_tile(xin[p], Bxin[p], xo[p], Bxo[p], g2, Bg2, b2, Bb2, st, Bst)
                if last:
                    P.dma("sp", self.out[b, i * 128:(i + 1) * 128, :], xo[p][:], reads=[Bxo[p]])
                else:
                    P.dma("sp", self.x2s[i * 128:(i + 1) * 128, :], xo[p][:], reads=[Bxo[p]], writes=[self.Bx2s[i]])
                    self.transpose_to_bf(xo[p], Bxo[p], self.xT_bf, self.BxT[i], i)
            self.psT_extra = []


def build_moe_test():
    k = K(nseq=1)
    P, I = k.P, k.I
    k.consts()
    with P.scope():
        k.router_setup(0)
        xt = [P.sbuf("xt", [128, D], F32) for _ in range(2)]
        Bxt = [Buf() for _ in range(2)]
        for i in range(NT):
            p = i % 2
            P.dma("sp", xt[p][:], I["x"][0, i * 128:(i + 1) * 128, :], writes=[Bxt[p]])
            P.dma("sp", k.x1s[i * 128:(i + 1) * 128, :], xt[p][:], reads=[Bxt[p]], writes=[k.Bx1s[i]])
            k.router_tile(i, xt[p], Bxt[p])
    k.moe(0, 0, True)
    P.finish()
    return k.nc


class Banks:
    def __init__(self, P, n):
        self.t = [P.psum("bank", [128, 512]) for _ in range(n)]
        self.B = [Buf() for _ in range(n)]
        self.i = 0

    def get(self):
        k = self.i
        self.i = (self.i + 1) % 4
        return self.t[k], self.B[k]

    def fixed(self, k):
        return self.t[4 + k], self.B[4 + k]


def _load_cast(P, dst, src, B):
    P.dma("pool", dst, src, writes=[B])


def k_load_xT(self, b):
    P, I = self.P, self.I
    P.dma("pool", self.xT_bf[:], I["xT"][b].rearrange("(c p) t -> p c t", p=128), writes=self.BxT)


def k_mem_attn(self, li, b, banks, w_in_ap, qm_off, catT, BcatT):
    P, I = self.P, self.I
    memT = P.sbuf("memT", [128, 8, 256], BF16)
    wkv = P.sbuf("wkv", [128, 8, 512], BF16)
    wqm = P.sbuf("wqm", [128, 8, 256], BF16)
    Bm, Bkv, Bqm = Buf(), Buf(), Buf()
    P.dma("pool", memT[:], I["memT"][b].rearrange("(c p) m -> p c m", p=128), writes=[Bm])
    P.dma("pool", wkv[:], I["w_mem_kv"][li].rearrange("(c p) f -> p c f", p=128), writes=[Bkv])
    P.dma("pool", wqm[:], w_in_ap[:, qm_off:qm_off + 256].rearrange("(c p) f -> p c f", p=128), writes=[Bqm])
    kmT = P.sbuf("kmT", [64, 4, 256], BF16)
    vma = P.sbuf("vma", [128, 2, 4, 128], BF16)
    qmT = P.sbuf("qmT", [64, 4, S], BF16)
    Bkm, Bvm, Bq = Buf(), Buf(), Buf()
    P.dve(lambda e: e.memset(vma[:], 1.0), writes=[Bvm])
    for h in range(4):
        ps, Bp = banks.get()
        for c in range(8):
            P.pe(lambda e, c=c, h=h, ps=ps: e.matmul(ps[0:64, 0:256], lhsT=wkv[:, c, h * 64:(h + 1) * 64],
                                                     rhs=memT[:, c, :], start=(c == 0), stop=(c == 7)),
                 reads=[Bkv, Bm], writes=[Bp])
        P.act(lambda e, h=h, ps=ps: e.copy(out=kmT[:, h, :], in_=ps[0:64, 0:256]), reads=[Bp], writes=[Bkm])
    for mc in range(2):
        ps, Bp = banks.get()
        for c in range(8):
            P.pe(lambda e, c=c, mc=mc, ps=ps: e.matmul(ps[:, 0:256], lhsT=memT[:, c, mc * 128:(mc + 1) * 128],
                                                       rhs=wkv[:, c, 256:512], start=(c == 0), stop=(c == 7)),
                 reads=[Bkv, Bm], writes=[Bp])
        P.act(lambda e, mc=mc, ps=ps: e.copy(out=vma[:, mc, :, 0:64],
                                             in_=ps[:, 0:256].rearrange("p (h d) -> p h d", h=4)),
              reads=[Bp], writes=[Bvm])
    for h in range(4):
        for tt in range(4):
            ps, Bp = banks.get()
            tsl = slice(tt * 512, (tt + 1) * 512)
            for c in range(8):
                P.pe(lambda e, c=c, h=h, ps=ps, tsl=tsl: e.matmul(ps[0:64, :], lhsT=wqm[:, c, h * 64:(h + 1) * 64],
                                                                  rhs=self.xT_bf[:, c, tsl], start=(c == 0),
                                                                  stop=(c == 7)),
                     reads=[Bqm] + self.BxT[tt * 4:tt * 4 + 4], writes=[Bp])
            P.act(lambda e, h=h, ps=ps, tsl=tsl: e.copy(out=qmT[:, h, tsl], in_=ps[0:64, :]), reads=[Bp], writes=[Bq])
    pT = [P.sbuf("pT", [128, 512], BF16) for _ in range(2)]
    BpT = [Buf(), Buf()]
    rec = P.sbuf("mrec", [64, 512], F32)
    Brec = Buf()
    for h in range(4):
        for tt in range(4):
            tsl = slice(tt * 512, (tt + 1) * 512)
            for mc in range(2):
                ps, Bp = banks.get()
                P.pe(lambda e, h=h, mc=mc, ps=ps, tsl=tsl: e.matmul(ps[:, :], lhsT=kmT[:, h, mc * 128:(mc + 1) * 128],
                                                                    rhs=qmT[:, h, tsl], start=True, stop=True),
                     reads=[Bkm, Bq], writes=[Bp])
                P.act(lambda e, mc=mc, ps=ps: e.activation(out=pT[mc][:], in_=ps[:, :], func=AF.Exp, scale=0.125),
                      reads=[Bp], writes=[BpT[mc]])
            po, Bpo = banks.get()
            for mc in range(2):
                P.pe(lambda e, h=h, mc=mc, po=po: e.matmul(po[:, :], lhsT=vma[:, mc, h, :], rhs=pT[mc][:],
                                                           start=(mc == 0), stop=(mc == 1)),
                     reads=[Bvm, BpT[mc]], writes=[Bpo])
            P.dve(lambda e, po=po: e.reciprocal(out=rec[:], in_=po[64:128, :]), reads=[Bpo], writes=[Brec])
            hp, ch = h % 2, 6 + h // 2
            P.dve(lambda e, po=po, hp=hp, ch=ch, tsl=tsl: e.tensor_tensor(
                out=catT[hp * 64:hp * 64 + 64, ch, tsl], in0=po[0:64, :], in1=rec[:], op=ALU.mult),
                reads=[Bpo, Brec], writes=[BcatT])


def k_xphase(self, li, b, banks, catT, BcatT, xsrc, Bxsrc):
    P, I = self.P, self.I
    wo = P.sbuf("wo", [128, 8, D], BF16)
    Bwo = Buf()
    P.dma("pool", wo[:], I["w_out"][li].rearrange("(c p) f -> p c f", p=128), writes=[Bwo])
    g1, Bg1 = self.bcast_row("g1", I["ln1_g"][li:li + 1, :], D)
    b1, Bb1 = self.bcast_row("b1", I["ln1_b"][li:li + 1, :], D)
    st2 = [self.ln_scratch("ln1a"), self.ln_scratch("ln1b")]
    self.router_setup(li)
    xin = [P.sbuf("xin1", [128, D], F32) for _ in range(2)]
    Bxin = [Buf(), Buf()]
    x1 = [P.sbuf("x1t", [128, D], F32) for _ in range(2)]
    Bx1 = [Buf(), Buf()]
    for i in range(NT):
        p = i % 2
        tsl = slice(i * 128, (i + 1) * 128)
        P.dma("sp", xin[p][:], xsrc[tsl, :], reads=[Bxsrc[i]], writes=[Bxin[p]])
        for half in range(2):
            ps, Bp = banks.get()
            for c in range(8):
                P.pe(lambda e, c=c, ps=ps, half=half, tsl=tsl: e.matmul(
                    ps[:, :], lhsT=catT[:, c, tsl], rhs=wo[:, c, half * 512:(half + 1) * 512],
                    start=(c == 0), stop=(c == 7)), reads=[BcatT, Bwo], writes=[Bp])
            hs = slice(half * 512, (half + 1) * 512)
            P.dve(lambda e, p=p, ps=ps, hs=hs: e.scalar_tensor_tensor(
                out=xin[p][:, hs], in0=xin[p][:, hs], scalar=ALPHA, in1=ps[:, :], op0=ALU.mult, op1=ALU.add),
                reads=[Bxin[p], Bp], writes=[Bxin[p]])
        st, Bst = st2[p]
        self.layernorm_tile(xin[p], Bxin[p], x1[p], Bx1[p], g1, Bg1, b1, Bb1, st, Bst)
        P.dma("sp", self.x1s[tsl, :], x1[p][:], reads=[Bx1[p]], writes=[self.Bx1s[i]])
        if self.dbg is not None:
            P.dma("sp", self.dbg[tsl, :], x1[p][:], reads=[Bx1[p]])
        self.router_tile(i, x1[p], Bx1[p])


def k_gla(self, b, banks, catT, BcatT):
    P, I = self.P, self.I
    W = I["gla_w_in"][0]
    w = P.sbuf("glaw", [128, 8, 2320], BF16)
    Bw = Buf()
    Bws = [Bw, Buf()]
    for hh in range(2):
        P.dma("pool", w[:, :, hh * 1160:(hh + 1) * 1160],
              W[:, hh * 1160:(hh + 1) * 1160].rearrange("(c p) f -> p c f", p=128), writes=[Bws[hh]])
    Bk = Buf()
    Lneg = P.sbuf("Lneg", [128, 128], F32)
    Uneg = P.sbuf("Uneg", [128, 128], F32)
    MG4 = P.sbuf("MG4", [128, 4, 128], F32)
    ones1 = P.sbuf("ones1", [1, 128], F32)
    P.pool(lambda e: e.memset(Lneg[:], -1.0 / 16.0), writes=[Bk])
    P.pool(lambda e: e.affine_select(out=Lneg[:], in_=Lneg[:], compare_op=ALU.is_ge, fill=0.0, base=0,
                                     pattern=[[1, 128]], channel_multiplier=-1), reads=[Bk], writes=[Bk])
    P.pool(lambda e: e.memset(Lneg[0:64, 64:128], 0.0), reads=[Bk], writes=[Bk])
    P.pool(lambda e: e.memset(Uneg[:], -1.0 / 16.0), reads=[Bk], writes=[Bk])
    P.pool(lambda e: e.affine_select(out=Uneg[:], in_=Uneg[:], compare_op=ALU.is_ge, fill=0.0, base=-1,
                                     pattern=[[-1, 128]], channel_multiplier=1), reads=[Bk], writes=[Bk])
    P.pool(lambda e: e.memset(Uneg[64:128, 0:64], 0.0), reads=[Bk], writes=[Bk])
    for h in range(4):
        P.pool(lambda e, h=h: e.memset(MG4[:, h, :], 1.0), reads=[Bk], writes=[Bk])
        P.pool(lambda e, h=h: e.affine_select(out=MG4[:, h, :], in_=MG4[:, h, :], compare_op=ALU.is_ge, fill=0.0,
                                              base=0, pattern=[[1, 128]], channel_multiplier=-1),
               reads=[Bk], writes=[Bk])
        P.pool(lambda e, h=h: e.memset(MG4[0:64, h, 64:128], 0.0), reads=[Bk], writes=[Bk])
    P.pool(lambda e: e.memset(ones1[:], 1.0), reads=[Bk], writes=[Bk])
    wg = P.sbuf("wg", [16, 384], F32)
    bg = P.sbuf("bg", [1, 384], F32)
    P.dma("sp", wg[:], I["gla_w_gate"][0], writes=[Bk])
    P.dma("sp", bg[:], I["gla_b_gate"][0:1, :], writes=[Bk])
    ng4 = P.sbuf("ng4", [128, 768], F32)
    for h in range(4):
        P.dma("sp", ng4[:, h * 192:(h + 1) * 192], I["gla_norm_g"][0:1, :].partition_broadcast(128), writes=[Bk])
    Sf = P.sbuf("Sf", [96, 4, 192], F32)
    Sb = P.sbuf("Sb", [96, 4, 192], BF16)
    Sb2 = P.sbuf("Sb2", [96, 4, 192], BF16)
    BS = [Buf() for _ in range(4)]
    BSb = [Buf() for _ in range(4)]
    BSb2 = [Buf() for _ in range(4)]
    P.dve(lambda e: e.memset(Sf[:], 0.0), writes=BS)
    P.dve(lambda e: e.memset(Sb[:], 0.0), writes=BSb)
    P.dve(lambda e: e.memset(Sb2[:], 0.0), writes=BSb2)
    def mkset():
        qeA = P.sbuf("qeA", [96, 4, 128], BF16)
        qeB = P.sbuf("qeB", [96, 4, 128], BF16)
        BqA, BqB = Buf(), Buf()
        P.dve(lambda e: e.memset(qeA[:], 0.0), writes=[BqA])
        P.dve(lambda e: e.memset(qeB[:], 0.0), writes=[BqB])
        tiles = (qeA, qeB, BqA, BqB,
                 P.sbuf("a1T", [16, 128], F32), P.sbuf("spl", [128, 384], F32), P.sbuf("eb", [96, 512], F32),
                 P.sbuf("enb", [96, 512], F32), P.sbuf("erb", [128, 384], F32), P.sbuf("qe", [96, 512], BF16),
                 P.sbuf("ke", [96, 512], BF16), P.sbuf("kdec", [128, 384], BF16), P.sbuf("vb", [128, 768], BF16),
                 P.sbuf("sr", [128, 768], F32), P.sbuf("at", [128, 4, 128], BF16), P.sbuf("sq", [128, 192], F32),
                 P.sbuf("ss", [128, 8], F32), P.sbuf("on", [128, 768], F32), P.sbuf("onb", [128, 768], BF16))
        return tiles + tuple(Buf() for _ in range(14))

    sets = [mkset(), mkset()]
    xT = self.xT_bf
    for i in range(NT):
        tsl = slice(i * 128, (i + 1) * 128)
        Bx = [self.BxT[i]]
        (qeA, qeB, BqA, BqB, a1T, sp_, eb, enb, erb, qe, ke, kdec, vb, sr, at, sq, ss, on, onb,
         Ba1, Bsp, Beb, Benb, Berb, Bqe, Bke, Bkd, Bvb, Bsr, Bat, Bsq, Bss, Bon) = sets[i % 2]

        def proj_tok(ps, n0, n1, width):
            for c in range(8):
                P.pe(lambda e, c=c: e.matmul(ps[:, 0:width], lhsT=xT[:, c, tsl], rhs=w[:, c, n0:n1],
                                             start=(c == 0), stop=(c == 7)), reads=Bws + Bx, writes=[None])

        pa, Bpa = banks.get()
        for c in range(8):
            P.pe(lambda e, c=c, pa=pa: e.matmul(pa[0:16, 0:128], lhsT=w[:, c, 2304:2320], rhs=xT[:, c, tsl],
                                                start=(c == 0), stop=(c == 7)), reads=Bws + Bx, writes=[Bpa])
        P.act(lambda e, pa=pa: e.copy(out=a1T[:], in_=pa[0:16, 0:128]), reads=[Bpa], writes=[Ba1])
        pz, Bpz = banks.get()
        P.pe(lambda e, pz=pz: e.matmul(pz[:, 0:384], lhsT=a1T[:], rhs=wg[:], start=True, stop=False),
             reads=[Ba1, Bk], writes=[Bpz])
        P.pe(lambda e, pz=pz: e.matmul(pz[:, 0:384], lhsT=ones1[:], rhs=bg[:], start=False, stop=True),
             reads=[Bk], writes=[Bpz])
        P.act(lambda e, pz=pz: e.activation(out=sp_[:], in_=pz[:, 0:384], func=AF.Exp, scale=-1.0),
              reads=[Bpz], writes=[Bsp])
        P.act(lambda e: e.activation(out=sp_[:], in_=sp_[:], func=AF.Ln, bias=1.0, scale=1.0),
              reads=[Bsp], writes=[Bsp])
        pb_, Bpb = banks.get()
        for h in range(4):
            P.pe(lambda e, h=h, pb_=pb_: e.matmul(pb_[0:96, h * 128:(h + 1) * 128], lhsT=sp_[:, h * 96:(h + 1) * 96],
                                                  rhs=Lneg[:], start=True, stop=True), reads=[Bsp, Bk], writes=[Bpb])
        P.act(lambda e, pb_=pb_: e.activation(out=eb[:], in_=pb_[0:96, :], func=AF.Exp), reads=[Bpb], writes=[Beb])
        P.act(lambda e, pb_=pb_: e.activation(out=enb[:], in_=pb_[0:96, :], func=AF.Exp, scale=-1.0),
              reads=[Bpb], writes=[Benb])
        prb, Bprb = banks.get()
        P.pe(lambda e, prb=prb: e.matmul(prb[:, 0:384], lhsT=Uneg[:], rhs=sp_[:], start=True, stop=True),
             reads=[Bsp, Bk], writes=[Bprb])
        P.act(lambda e, prb=prb: e.activation(out=erb[:], in_=prb[:, 0:384], func=AF.Exp), reads=[Bprb], writes=[Berb])
        pq, Bpq = banks.get()
        for h in range(4):
            for c in range(8):
                P.pe(lambda e, c=c, h=h, pq=pq: e.matmul(pq[0:96, h * 128:(h + 1) * 128], lhsT=w[:, c, h * 96:(h + 1) * 96],
                                                         rhs=xT[:, c, tsl], start=(c == 0), stop=(c == 7)),
                     reads=Bws + Bx, writes=[Bpq])
        P.dve(lambda e, pq=pq: e.scalar_tensor_tensor(out=qe[:], in0=pq[0:96, :], scalar=96.0 ** -0.5, in1=eb[:],
                                                      op0=ALU.mult, op1=ALU.mult), reads=[Bpq, Beb], writes=[Bqe])
        P.dve(lambda e: e.tensor_copy(out=qeA[:, :, 0:64], in_=qe[:].rearrange("p (h t) -> p h t", h=4)[:, :, 0:64]),
              reads=[Bqe], writes=[BqA])
        P.dve(lambda e: e.tensor_copy(out=qeB[:, :, 64:128], in_=qe[:].rearrange("p (h t) -> p h t", h=4)[:, :, 64:128]),
              reads=[Bqe], writes=[BqB])
        pk, Bpk = banks.get()
        for h in range(4):
            for c in range(8):
                P.pe(lambda e, c=c, h=h, pk=pk: e.matmul(pk[0:96, h * 128:(h + 1) * 128],
                                                         lhsT=w[:, c, 384 + h * 96:384 + (h + 1) * 96],
                                                         rhs=xT[:, c, tsl], start=(c == 0), stop=(c == 7)),
                     reads=Bws + Bx, writes=[Bpk])
        P.dve(lambda e, pk=pk: e.tensor_tensor(out=ke[:], in0=pk[0:96, :], in1=enb[:], op=ALU.mult),
              reads=[Bpk, Benb], writes=[Bke])
        pkt, Bpkt = banks.get()
        for c in range(8):
            P.pe(lambda e, c=c, pkt=pkt: e.matmul(pkt[:, 0:384], lhsT=xT[:, c, tsl], rhs=w[:, c, 384:768],
                                                  start=(c == 0), stop=(c == 7)), reads=Bws + Bx, writes=[Bpkt])
        P.dve(lambda e, pkt=pkt: e.tensor_tensor(out=kdec[:], in0=pkt[:, 0:384], in1=erb[:], op=ALU.mult),
              reads=[Bpkt, Berb], writes=[Bkd])
        for part in range(2):
            pv, Bpv = banks.get()
            for c in range(8):
                P.pe(lambda e, c=c, pv=pv, part=part: e.matmul(pv[:, 0:384], lhsT=xT[:, c, tsl],
                                                               rhs=w[:, c, 768 + part * 384:768 + (part + 1) * 384],
                                                               start=(c == 0), stop=(c == 7)),
                     reads=Bws + Bx, writes=[Bpv])
            P.act(lambda e, pv=pv, part=part: e.copy(out=vb[:, part * 384:(part + 1) * 384], in_=pv[:, 0:384]),
                  reads=[Bpv], writes=[Bvb])
        for part in range(2):
            pr, Bpr = banks.get()
            for c in range(8):
                P.pe(lambda e, c=c, pr=pr, part=part: e.matmul(pr[:, 0:384], lhsT=xT[:, c, tsl],
                                                               rhs=w[:, c, 1536 + part * 384:1536 + (part + 1) * 384],
                                                               start=(c == 0), stop=(c == 7)),
                     reads=Bws + Bx, writes=[Bpr])
            P.act(lambda e, pr=pr, part=part: e.activation(out=sr[:, part * 384:(part + 1) * 384], in_=pr[:, 0:384],
                                                           func=AF.Silu), reads=[Bpr], writes=[Bsr])
        pat, Bpat = banks.get()
        for h in range(4):
            P.pe(lambda e, h=h, pat=pat: e.matmul(pat[:, h * 128:(h + 1) * 128], lhsT=ke[:, h * 128:(h + 1) * 128],
                                                  rhs=qe[:, h * 128:(h + 1) * 128], start=True, stop=True),
                 reads=[Bke, Bqe], writes=[Bpat])
        P.dve(lambda e, pat=pat: e.tensor_tensor(out=at[:].rearrange("p h t -> p (h t)"), in0=pat[:, :],
                                                 in1=MG4[:].rearrange("p h t -> p (h t)"), op=ALU.mult),
              reads=[Bpat, Bk], writes=[Bat])
        po2 = [banks.fixed(0), banks.fixed(1)]
        for h in range(4):
            po, Bpo = po2[h // 2]
            osl = slice((h % 2) * 192, (h % 2) * 192 + 192)
            vsl = slice(h * 192, (h + 1) * 192)
            ksl = slice(h * 96, (h + 1) * 96)
            P.pe(lambda e, h=h, po=po, osl=osl, vsl=vsl: e.matmul(po[:, osl], lhsT=at[:, h, :], rhs=vb[:, vsl],
                                                                   start=True, stop=False),
                 reads=[Bat, Bvb], writes=[Bpo])
            P.pe(lambda e, h=h, po=po, osl=osl: e.matmul(po[:, osl], lhsT=qeA[:, h, :], rhs=Sb[:, h, :],
                                                          start=False, stop=False), reads=[BqA, BSb[h]], writes=[Bpo])
            pi1, Bpi1 = banks.get()
            P.pe(lambda e, pi1=pi1, ksl=ksl, vsl=vsl: e.matmul(pi1[0:96, 0:192], lhsT=kdec[0:64, ksl], rhs=vb[0:64, vsl],
                                                               start=True, stop=True), reads=[Bkd, Bvb], writes=[Bpi1])
            P.dve(lambda e, h=h, pi1=pi1: e.scalar_tensor_tensor(out=Sf[:, h, :], in0=Sf[:, h, :],
                                                                 scalar=eb[:, h * 128 + 63:h * 128 + 64],
                                                                 in1=pi1[0:96, 0:192], op0=ALU.mult, op1=ALU.add),
                  reads=[BS[h], Beb, Bpi1], writes=[BS[h]])
            P.act(lambda e, h=h: e.copy(out=Sb2[:, h, :], in_=Sf[:, h, :]), reads=[BS[h]], writes=[BSb2[h]])
            P.pe(lambda e, h=h, po=po, osl=osl: e.matmul(po[:, osl], lhsT=qeB[:, h, :], rhs=Sb2[:, h, :],
                                                          start=False, stop=True), reads=[BqB, BSb2[h]], writes=[Bpo])
            pi2, Bpi2 = banks.get()
            P.pe(lambda e, pi2=pi2, ksl=ksl, vsl=vsl: e.matmul(pi2[0:96, 0:192], lhsT=kdec[64:128, ksl],
                                                               rhs=vb[64:128, vsl], start=True, stop=True),
                 reads=[Bkd, Bvb], writes=[Bpi2])
            P.dve(lambda e, h=h, pi2=pi2: e.scalar_tensor_tensor(out=Sf[:, h, :], in0=Sf[:, h, :],
                                                                 scalar=eb[:, h * 128 + 127:h * 128 + 128],
                                                                 in1=pi2[0:96, 0:192], op0=ALU.mult, op1=ALU.add),
                  reads=[BS[h], Beb, Bpi2], writes=[BS[h]])
            P.act(lambda e, h=h: e.copy(out=Sb[:, h, :], in_=Sf[:, h, :]), reads=[BS[h]], writes=[BSb[h]])
            P.act(lambda e, po=po, osl=osl: e.activation(out=sq[:], in_=po[:, osl], func=AF.Square),
                  reads=[Bpo], writes=[Bsq])
            P.dve(lambda e, h=h: e.reduce_sum(out=ss[:, h:h + 1], in_=sq[:], axis=AX.X), reads=[Bsq], writes=[Bss])
        P.act(lambda e: e.activation(out=ss[:, 4:8], in_=ss[:, 0:4], func=AF.Ln, bias=RMS_EPS, scale=1.0 / 192.0),
              reads=[Bss], writes=[Bss], cost=0.2)
        P.act(lambda e: e.activation(out=ss[:, 4:8], in_=ss[:, 4:8], func=AF.Exp, scale=-0.5),
              reads=[Bss], writes=[Bss], cost=0.2)
        for h in range(4):
            po, Bpo = po2[h // 2]
            osl = slice((h % 2) * 192, (h % 2) * 192 + 192)
            vsl = slice(h * 192, (h + 1) * 192)
            P.dve(lambda e, h=h, po=po, osl=osl, vsl=vsl: e.scalar_tensor_tensor(
                out=on[:, vsl], in0=po[:, osl], scalar=ss[:, 4 + h:5 + h], in1=ng4[:, vsl],
                op0=ALU.mult, op1=ALU.mult), reads=[Bpo, Bss, Bk], writes=[Bon])
        Bonb = Bsq
        P.dve(lambda e: e.tensor_tensor(out=onb[:], in0=on[:], in1=sr[:], op=ALU.mult), reads=[Bon, Bsr], writes=[Bonb])
        psT, BpsT = self.psT[0], self.BpsT[0]
        psTb = psT[:].bitcast(BF16)
        for c in range(6):
            P.pe(lambda e, c=c: e.transpose(out=psTb[:, c * 128:(c + 1) * 128], in_=onb[:, c * 128:(c + 1) * 128],
                                            identity=self.identb[:]), reads=[Bonb, self.Bc], writes=[BpsT])
        P.dve(lambda e: e.tensor_copy(out=catT[:, 0:6, tsl], in_=psTb[:, 0:768].rearrange("p (c t) -> p c t", c=6)),
              reads=[BpsT], writes=[BcatT])


K.load_xT = k_load_xT
K.mem_attn = k_mem_attn
K.xphase = k_xphase
K.gla = k_gla


def k_rope_tables(self, b):
    P, I = self.P, self.I
    B = Buf()
    cosT = P.sbuf("cosT", [128, NT, 32], F32)
    sinT = P.sbuf("sinT", [128, NT, 32], F32)
    with P.scope():
        self._rope_tables_body(b, cosT, sinT, B)
    return cosT, sinT, B


def k_rope_tables_body(self, b, cosT, sinT, B):
    P, I = self.P, self.I
    pi = P.sbuf("posi", [128, NT], I32)
    pf = P.sbuf("posf", [128, NT], F32)
    io = P.sbuf("iot", [128, 32], I32)
    inv = P.sbuf("inv", [128, 32], F32)
    ang = P.sbuf("ang", [128, NT, 32], F32)
    t1 = P.sbuf("rt1", [128, NT, 32], F32)
    P.dma("sp", pi[:], I["pos"][b], writes=[B])
    P.dve(lambda e: e.tensor_copy(out=pf[:], in_=pi[:]), reads=[B], writes=[B])
    P.pool(lambda e: e.iota(io[:], pattern=[[1, 32]], base=0, channel_multiplier=0), reads=[B], writes=[B])
    P.dve(lambda e: e.tensor_copy(out=inv[:], in_=io[:]), reads=[B], writes=[B])
    P.act(lambda e: e.activation(out=inv[:], in_=inv[:], func=AF.Exp, scale=-math.log(10000.0) / 32.0),
          reads=[B], writes=[B])
    for c in range(NT):
        P.dve(lambda e, c=c: e.tensor_scalar(out=ang[:, c, :], in0=inv[:], scalar1=pf[:, c:c + 1], scalar2=None,
                                             op0=ALU.mult), reads=[B], writes=[B])
    MAGIC = 12582912.0
    TWO_PI = 2.0 * math.pi
    for which, dst in ((0, sinT), (1, cosT)):
        src = ang
        if which == 1:
            P.dve(lambda e: e.tensor_scalar(out=ang[:], in0=ang[:], scalar1=math.pi / 2.0, scalar2=None, op0=ALU.add),
                  reads=[B], writes=[B])
        P.dve(lambda e: e.tensor_scalar(out=t1[:], in0=ang[:], scalar1=1.0 / TWO_PI, scalar2=MAGIC,
                                        op0=ALU.mult, op1=ALU.add), reads=[B], writes=[B])
        P.dve(lambda e: e.tensor_scalar(out=t1[:], in0=t1[:], scalar1=-MAGIC, scalar2=None, op0=ALU.add),
              reads=[B], writes=[B])
        P.dve(lambda e: e.scalar_tensor_tensor(out=t1[:], in0=t1[:], scalar=-TWO_PI, in1=ang[:],
                                               op0=ALU.mult, op1=ALU.add), reads=[B], writes=[B])
        P.dve(lambda e: e.tensor_scalar(out=t1[:], in0=t1[:], scalar1=3.1415925, scalar2=-3.1415925,
                                        op0=ALU.min, op1=ALU.max), reads=[B], writes=[B])
        P.act(lambda e, dst=dst: e.activation(out=dst[:], in_=t1[:], func=AF.Sin, scale=0.999999),
              reads=[B], writes=[B])


def k_rope(self, src, Bsrc, dst, Bdst, nh, cosT, sinT, Btab, i, tmp, Btmp):
    P = self.P
    s3 = src.rearrange("p (h d) -> p h d", h=nh)
    d3 = dst.rearrange("p (h d) -> p h d", h=nh)
    cb = cosT[:, i, :].unsqueeze(1).to_broadcast([128, nh, 32])
    sb = sinT[:, i, :].unsqueeze(1).to_broadcast([128, nh, 32])
    ta = tmp[:, 0:nh * 32].rearrange("p (h d) -> p h d", h=nh)
    tb = tmp[:, 512:512 + nh * 32].rearrange("p (h d) -> p h d", h=nh)
    x1, x2 = s3[:, :, 0:32], s3[:, :, 32:64]
    P.dve(lambda e: e.tensor_tensor(out=ta, in0=x1, in1=cb, op=ALU.mult), reads=[Bsrc, Btab], writes=[Btmp])
    P.dve(lambda e: e.tensor_tensor(out=tb, in0=x2, in1=sb, op=ALU.mult), reads=[Bsrc, Btab], writes=[Btmp])
    P.dve(lambda e: e.tensor_tensor(out=d3[:, :, 0:32], in0=ta, in1=tb, op=ALU.subtract), reads=[Btmp], writes=[Bdst])
    P.dve(lambda e: e.tensor_tensor(out=ta, in0=x2, in1=cb, op=ALU.mult), reads=[Bsrc, Btab, Bdst], writes=[Btmp])
    P.dve(lambda e: e.tensor_tensor(out=tb, in0=x1, in1=sb, op=ALU.mult), reads=[Bsrc, Btab], writes=[Btmp])
    P.dve(lambda e: e.tensor_tensor(out=d3[:, :, 32:64], in0=ta, in1=tb, op=ALU.add), reads=[Btmp], writes=[Bdst])


def k_dsa(self, b, banks, catT, BcatT):
    P, I = self.P, self.I
    W = I["dsa_w_in"][0]
    xT = self.xT_bf
    psT, BpsT = self.psT[0], self.BpsT[0]
    ident = self.ident
    cosT, sinT, Btab = self.rope_tables(b)
    tmp = P.sbuf("ropetmp", [128, 1024], F32)
    Btmp = Buf()
    maskT = [P.sbuf("maskT", [128, (NT - j) * 128], BF16) for j in range(NT)]
    BmT = [Buf() for _ in range(NT)]
    negm = P.sbuf("negm", [128, 128], F32)
    Bk = Buf()
    P.pool(lambda e: e.memset(negm[:], 0.0), writes=[Bk])
    P.pool(lambda e: e.affine_select(out=negm[:], in_=negm[:], compare_op=ALU.is_ge, fill=NEG, base=0,
                                     pattern=[[-1, 128]], channel_multiplier=1), reads=[Bk], writes=[Bk])
    with P.scope():
        wi_ = P.sbuf("dsw1", [128, 8, 584], BF16)
        Bw = Buf()
        P.dma("pool", wi_[:], W[:, 2304:2888].rearrange("(c p) f -> p c f", p=128), writes=[Bw])
        kg, Bkg = self.bcast_row("kg", I["dsa_idx_k_g"][0:1, :], 64)
        kb, Bkb = self.bcast_row("kb", I["dsa_idx_k_b"][0:1, :], 64)
        qiT = P.sbuf("qiT", [128, 4, S], BF16)
        kiT = P.sbuf("kiT", [128, S], BF16)
        wis = P.sbuf("wis", [128, NT, 8], F32)
        BqiT, BkiT, Bwis = Buf(), Buf(), Buf()
        qiR = P.sbuf("qiR", [128, 512], F32)
        kiN = P.sbuf("kiN", [128, 64], F32)
        kiR = P.sbuf("kiR", [128, 128], F32)
        BqiR, BkiN, BkiR = Buf(), Buf(), Buf()
        st, Bst = self.ln_scratch("kln")
        stats, mv, rstd, nmr = st
        for i in range(NT):
            tsl = slice(i * 128, (i + 1) * 128)
            Bx = [self.BxT[i]]
            pq, Bpq = banks.get()
            for c in range(8):
                P.pe(lambda e, c=c, pq=pq: e.matmul(pq[:, 0:512], lhsT=xT[:, c, tsl], rhs=wi_[:, c, 0:512],
                                                    start=(c == 0), stop=(c == 7)), reads=[Bw] + Bx, writes=[Bpq])
            pk, Bpk = banks.get()
            for c in range(8):
                P.pe(lambda e, c=c, pk=pk: e.matmul(pk[:, 0:72], lhsT=xT[:, c, tsl], rhs=wi_[:, c, 512:584],
                                                    start=(c == 0), stop=(c == 7)), reads=[Bw] + Bx, writes=[Bpk])
            self.rope(pq[:, 0:512], Bpq, qiR[:, :], BqiR, 8, cosT, sinT, Btab, i, tmp, Btmp)
            P.dve(lambda e, pk=pk, i=i: e.tensor_scalar(out=wis[:, i, :], in0=pk[:, 64:72],
                                                        scalar1=(8.0 ** -0.5) * (64.0 ** -0.5), scalar2=None, op0=ALU.mult),
                  reads=[Bpk], writes=[Bwis])
            P.dve(lambda e, pk=pk: e.bn_stats(out=stats[:, 0:6], in_=pk[:, 0:64]), reads=[Bpk], writes=[Bst])
            P.dve(lambda e: e.bn_aggr(out=mv[:], in_=stats[:, 0:6]), reads=[Bst], writes=[Bst])
            P.act(lambda e: e.activation(out=rstd[:], in_=mv[:, 1:2], func=AF.Ln, bias=LN_EPS, scale=1.0),
                  reads=[Bst], writes=[Bst], cost=0.2)
            P.act(lambda e: e.activation(out=rstd[:], in_=rstd[:], func=AF.Exp, scale=-0.5),
                  reads=[Bst], writes=[Bst], cost=0.2)
            P.dve(lambda e, pk=pk: e.tensor_scalar(out=kiN[:], in0=pk[:, 0:64], scalar1=mv[:, 0:1], scalar2=rstd[:],
                                                   op0=ALU.subtract, op1=ALU.mult), reads=[Bpk, Bst], writes=[BkiN])
            P.dve(lambda e: e.tensor_tensor(out=kiN[:], in0=kiN[:], in1=kg[:], op=ALU.mult), reads=[BkiN, Bkg], writes=[BkiN])
            P.dve(lambda e: e.tensor_tensor(out=kiN[:], in0=kiN[:], in1=kb[:], op=ALU.add), reads=[BkiN, Bkb], writes=[BkiN])
            self.rope(kiN[:, :], BkiN, kiR[:, 0:64], BkiR, 1, cosT, sinT, Btab, i, tmp, Btmp)
            P.dve(lambda e: e.tensor_copy(out=kiR[:, 64:128], in_=kiR[:, 0:64]), reads=[BkiR], writes=[BkiR])
            for c in range(4):
                P.pe(lambda e, c=c: e.transpose(out=psT[:, c * 128:(c + 1) * 128], in_=qiR[:, c * 128:(c + 1) * 128],
                                                identity=ident[:]), reads=[BqiR, self.Bc], writes=[BpsT])
            P.pe(lambda e: e.transpose(out=psT[:, 512:640], in_=kiR[:, :], identity=ident[:]),
                 reads=[BkiR, self.Bc], writes=[BpsT])
            P.act(lambda e, tsl=tsl: e.copy(out=qiT[:, :, tsl], in_=psT[:, 0:512].rearrange("p (c t) -> p c t", c=4)),
                  reads=[BpsT], writes=[BqiT])
            P.act(lambda e, tsl=tsl: e.copy(out=kiT[:, tsl], in_=psT[:, 512:640]), reads=[BpsT], writes=[BkiT])
        acc2 = [P.sbuf("acc", [128, S], F32) for _ in range(4)]
        Bacc2 = [Buf() for _ in range(4)]
        junk = P.sbuf("junk", [128, S], BF16)
        Bjunk = Buf()
        mk = P.sbuf("mk", [128, S], F32)
        Bmk = Buf()
        bs2 = [P.sbuf("bs", [128, 8], F32) for _ in range(2)]
        Bbs2 = [Buf(), Buf()]
        rrc = [0]
        NIT = 24

        dg2 = [P.sbuf("dg", [128, 8, 128], F32) for _ in range(2)]
        Bdg2 = [Buf(), Buf()]
        rl = [P.sbuf("rl", [128, 512], F32) for _ in range(4)]
        Brl = [Buf() for _ in range(4)]

        def scores(i, slot):
            acc, Bacc = acc2[slot], Bacc2[slot]
            dg, Bdg = dg2[slot % 2], Bdg2[slot % 2]
            Wd = (i + 1) * 128
            tsl = slice(i * 128, (i + 1) * 128)
            npc = (Wd + 511) // 512
            for h in range(8):
                P.dve(lambda e: e.tensor_scalar(out=dg[:, h, :], in0=ident[:], scalar1=wis[:, i, h:h + 1], scalar2=None,
                                                op0=ALU.mult), reads=[self.Bc, Bwis], writes=[Bdg], cost=0.15)
            for n in range(npc):
                c0, c1 = n * 512, min(Wd, (n + 1) * 512)
                pacc, Bpacc = banks.fixed(n % 2)
                for h in range(8):
                    hp, hc = (h % 2) * 64, h // 2
                    ps, Bp = banks.get()
                    P.pe(lambda e: e.matmul(ps[:, 0:c1 - c0], lhsT=qiT[hp:hp + 64, hc, tsl], rhs=kiT[hp:hp + 64, c0:c1],
                                            start=True, stop=True), reads=[BqiT, BkiT], writes=[Bp])
                    r_ = rrc[0]
                    rrc[0] = (r_ + 1) % 4
                    P.act(lambda e: e.activation(out=rl[r_][:, 0:c1 - c0], in_=ps[:, 0:c1 - c0], func=AF.Relu),
                          reads=[Bp], writes=[Brl[r_]])
                    last = (h == 7) and (n != npc - 1)
                    P.pe(lambda e: e.matmul(pacc[:, 0:c1 - c0], lhsT=dg[:, h, :], rhs=rl[r_][:, 0:c1 - c0],
                                            start=(h == 0), stop=last), reads=[Bdg, Brl[r_]], writes=[Bpacc], cost=0.9)
                if n == npc - 1:
                    off = i * 128 - c0
                    P.pe(lambda e: e.matmul(pacc[:, off:off + 128], lhsT=ident[:], rhs=negm[:], start=False, stop=True),
                         reads=[self.Bc, Bk], writes=[Bpacc], cost=0.4)
                P.act(lambda e: e.copy(out=acc[:, c0:c1], in_=pacc[:, 0:c1 - c0]), reads=[Bpacc], writes=[Bacc])

        def bis_init(i, slot):
            acc, Bacc, bs, Bbs = acc2[slot], Bacc2[slot], bs2[slot % 2], Bbs2[slot % 2]
            Wd = (i + 1) * 128
            P.dve(lambda e: e.reduce_max(out=bs[:, 5:6], in_=acc[:, 0:Wd], axis=AX.X), reads=[Bacc], writes=[Bbs])
            P.dve(lambda e: e.tensor_reduce(out=bs[:, 0:1], in_=acc[:, 0:i * 128], axis=AX.X, op=ALU.min),
                  reads=[Bacc], writes=[Bbs])
            P.dve(lambda e: e.tensor_tensor(out=bs[:, 1:2], in0=bs[:, 5:6], in1=bs[:, 0:1], op=ALU.subtract),
                  reads=[Bbs], writes=[Bbs])

        def bis_iter(i, slot, k):
            acc, Bacc, bs, Bbs = acc2[slot], Bacc2[slot], bs2[slot % 2], Bbs2[slot % 2]
            Wd = (i + 1) * 128
            ck = 2.0 ** -(k + 1)
            P.dve(lambda e: e.scalar_tensor_tensor(out=bs[:, 2:3], in0=bs[:, 1:2], scalar=ck, in1=bs[:, 0:1],
                                                   op0=ALU.mult, op1=ALU.add), reads=[Bbs], writes=[Bbs], cost=0.12)
            P.dve(lambda e: e.tensor_scalar(out=junk[:, 0:Wd], in0=acc[:, 0:Wd], scalar1=bs[:, 2:3], scalar2=0.0,
                                            op0=ALU.is_ge, op1=ALU.add, accum_out=bs[:, 3:4]),
                  reads=[Bacc, Bbs], writes=[Bbs, Bjunk], cost=Wd / 960.0 + 0.15)
            P.dve(lambda e: e.tensor_scalar(out=bs[:, 4:5], in0=bs[:, 3:4], scalar1=255.5, scalar2=ck,
                                            op0=ALU.is_ge, op1=ALU.mult), reads=[Bbs], writes=[Bbs], cost=0.12)
            P.dve(lambda e: e.scalar_tensor_tensor(out=bs[:, 0:1], in0=bs[:, 4:5], scalar=bs[:, 1:2], in1=bs[:, 0:1],
                                                   op0=ALU.mult, op1=ALU.add), reads=[Bbs], writes=[Bbs], cost=0.12)

        def finish_tile(i, slot, use_thr):
            acc, Bacc, bs, Bbs = acc2[slot], Bacc2[slot], bs2[slot % 2], Bbs2[slot % 2]
            Wd = (i + 1) * 128
            if use_thr:
                P.dve(lambda e: e.tensor_scalar(out=mk[:, 0:Wd], in0=acc[:, 0:Wd], scalar1=bs[:, 0:1], scalar2=None,
                                                op0=ALU.is_ge), reads=[Bacc, Bbs], writes=[Bmk])
            else:
                P.dve(lambda e: e.tensor_scalar(out=mk[:, 0:Wd], in0=acc[:, 0:Wd], scalar1=-1.0e29, scalar2=None,
                                                op0=ALU.is_ge), reads=[Bacc], writes=[Bmk])
            for j0 in range(0, i + 1, 8):
                js = list(range(j0, min(i + 1, j0 + 8)))
                for j in js:
                    P.pe(lambda e: e.transpose(out=psT[:, (j - j0) * 128:(j - j0 + 1) * 128],
                                               in_=mk[:, j * 128:(j + 1) * 128], identity=ident[:]),
                         reads=[Bmk, self.Bc], writes=[BpsT])
                for j in js:
                    P.act(lambda e: e.copy(out=maskT[j][:, (i - j) * 128:(i - j + 1) * 128],
                                           in_=psT[:, (j - j0) * 128:(j - j0 + 1) * 128]),
                          reads=[BpsT], writes=[BmT[j]])

        pairs = [(ia, ia + 1) for ia in range(2, NT, 2)]
        for i in range(2):
            scores(i, i)
        scores(pairs[0][0], 2)
        scores(pairs[0][1], 3)
        for i in range(2):
            finish_tile(i, i, False)
        for pi_, (ia, ib) in enumerate(pairs):
            sa, sb = (2, 3) if pi_ % 2 == 0 else (0, 1)
            if pi_ + 1 < len(pairs):
                na, nb = (0, 1) if pi_ % 2 == 0 else (2, 3)
                scores(pairs[pi_ + 1][0], na)
                scores(pairs[pi_ + 1][1], nb)
            bis_init(ia, sa)
            bis_init(ib, sb)
            for k in range(NIT):
                bis_iter(ia, sa, k)
                bis_iter(ib, sb, k)
            finish_tile(ia, sa, True)
            finish_tile(ib, sb, True)
    with P.scope():
        sets = []
        w_shared = P.sbuf("dsw2", [128, 8, 768], BF16)
        Bw_shared = Buf()
        for k_ in range(2):
            d = dict(w=w_shared, Bw=Bw_shared,
                     qT=P.sbuf("qT", [128, 2, S], BF16), kT=P.sbuf("kT", [128, 2, S], BF16),
                     va=P.sbuf("va", [128, NT, 4, 128], BF16),
                     BqT=[Buf() for _ in range(NT)], BkT=[Buf() for _ in range(NT)], Bva=[Buf() for _ in range(NT)],
                     qR=P.sbuf("qR", [128, 512], BF16), BqR=Buf())
            va_ = d["va"]
            P.dve(lambda e: e.memset(va_[:], 1.0), writes=d["Bva"])
            sets.append(d)
        tmp2 = [tmp, tmp]
        Btmp2 = [Btmp, Btmp]
        NR = 4
        LOOK = 3
        pe_ = [P.sbuf("pe_", [128, 512], BF16) for _ in range(NR)]
        pm_ = [P.sbuf("pm_", [128, 512], BF16) for _ in range(NR)]
        Bpe = [Buf() for _ in range(NR)]
        Bpm = [Buf() for _ in range(NR)]
        rec = [P.sbuf("arec", [64, 512], F32) for _ in range(2)]
        Brec = [Buf(), Buf()]
        blocks = [(h, tt, j) for h in range(4) for tt in range(4) for j in range(4 * tt + 4)]
        ropecnt = [0]

        def load_group(hg):
            d = sets[hg % 2]
            w2_ = d["w"]
            for part in range(3):
                P.dma("pool", w2_[:, :, part * 256:(part + 1) * 256],
                      W[:, part * 768 + hg * 256:part * 768 + (hg + 1) * 256].rearrange("(c p) f -> p c f", p=128),
                      writes=[d["Bw"]])

        def proj_tile(hg, i):
            d = sets[hg % 2]
            w2_, qT, kT, va, qR, BqR, Bw = d["w"], d["qT"], d["kT"], d["va"], d["qR"], d["BqR"], d["Bw"]
            tsl = slice(i * 128, (i + 1) * 128)
            Bx = [self.BxT[i]]
            for part in range(2):
                pq, Bpq = banks.get()
                for c in range(8):
                    P.pe(lambda e: e.matmul(pq[:, 0:256], lhsT=xT[:, c, tsl], rhs=w2_[:, c, part * 256:(part + 1) * 256],
                                            start=(c == 0), stop=(c == 7)), reads=[Bw] + Bx, writes=[Bpq])
                tk = ropecnt[0] % 2
                ropecnt[0] += 1
                self.rope(pq[:, 0:256], Bpq, qR[:, part * 256:(part + 1) * 256], BqR, 4, cosT, sinT, Btab, i,
                          tmp2[tk], Btmp2[tk])
            pv, Bpv = banks.get()
            for c in range(8):
                P.pe(lambda e: e.matmul(pv[:, 0:256], lhsT=xT[:, c, tsl], rhs=w2_[:, c, 512:768],
                                        start=(c == 0), stop=(c == 7)), reads=[Bw] + Bx, writes=[Bpv])
            P.act(lambda e: e.copy(out=va[:, i, :, 0:64], in_=pv[:, 0:256].rearrange("p (h d) -> p h d", h=4)),
                  reads=[Bpv], writes=[d["Bva"][i]])
            psTb = psT[:].bitcast(BF16)
            for c in range(4):
                P.pe(lambda e: e.transpose(out=psTb[:, c * 128:(c + 1) * 128], in_=qR[:, c * 128:(c + 1) * 128],
                                           identity=self.identb[:]), reads=[BqR, self.Bc], writes=[BpsT])
            P.act(lambda e: e.copy(out=qT[:, :, tsl], in_=psTb[:, 0:256].rearrange("p (c t) -> p c t", c=2)),
                  reads=[BpsT], writes=[d["BqT"][i]])
            P.act(lambda e: e.copy(out=kT[:, :, tsl], in_=psTb[:, 256:512].rearrange("p (c t) -> p c t", c=2)),
                  reads=[BpsT], writes=[d["BkT"][i]])

        def stage_a(hg, idx):
            d = sets[hg % 2]
            qT, kT = d["qT"], d["kT"]
            h, tt, j = blocks[idx]
            hp, hc = (h % 2) * 64, h // 2
            t0 = max(tt * 512, j * 128)
            c0 = t0 - tt * 512
            t1 = (tt + 1) * 512
            r_ = idx % NR
            ps, Bp = banks.get()
            P.pe(lambda e: e.matmul(ps[:, c0:512], lhsT=kT[hp:hp + 64, hc, j * 128:(j + 1) * 128],
                                    rhs=qT[hp:hp + 64, hc, t0:t1], start=True, stop=True),
                 reads=[d["BkT"][j]] + d["BqT"][tt * 4:tt * 4 + 4], writes=[Bp])
            P.act(lambda e: e.activation(out=pe_[r_][:, c0:512], in_=ps[:, c0:512], func=AF.Exp, scale=0.125),
                  reads=[Bp], writes=[Bpe[r_]])
            P.dve(lambda e: e.tensor_tensor(out=pm_[r_][:, c0:512], in0=pe_[r_][:, c0:512],
                                            in1=maskT[j][:, t0 - j * 128:t1 - j * 128], op=ALU.mult),
                  reads=[Bpe[r_], BmT[j]], writes=[Bpm[r_]])

        def stage_b(hg, idx):
            d = sets[hg % 2]
            va = d["va"]
            h, tt, j = blocks[idx]
            g = h * 4 + tt
            nblk = 4 * tt + 4
            t0 = max(tt * 512, j * 128)
            c0 = t0 - tt * 512
            r_ = idx % NR
            po, Bpo = banks.fixed(g % 2)
            P.pe(lambda e: e.matmul(po[:, c0:512], lhsT=va[:, j, h, :], rhs=pm_[r_][:, c0:512],
                                    start=(j == 0), stop=(j == nblk - 1)), reads=[d["Bva"][j], Bpm[r_]], writes=[Bpo])
            if j == nblk - 1:
                gh = hg * 4 + h
                ghp, gch = (gh % 2) * 64, gh // 2
                rc, Brc = rec[g % 2], Brec[g % 2]
                P.act(lambda e: e.activation(out=rc[:], in_=po[64:128, :], func=AF.Ln), reads=[Bpo], writes=[Brc])
                P.act(lambda e: e.activation(out=rc[:], in_=rc[:], func=AF.Exp, scale=-1.0), reads=[Brc], writes=[Brc])
                P.dve(lambda e: e.tensor_tensor(out=catT[ghp:ghp + 64, gch, tt * 512:(tt + 1) * 512],
                                                in0=po[0:64, :], in1=rc[:], op=ALU.mult),
                      reads=[Bpo, Brc], writes=[BcatT])

        load_group(0)
        for i in range(NT):
            proj_tile(0, i)
        for hg in range(3):
            nxt = hg + 1 if hg + 1 < 3 else None
            if nxt is not None:
                load_group(nxt)
            nsteps = len(blocks) + LOOK
            every = nsteps // NT
            pi_ = 0
            for idx in range(nsteps):
                if idx < len(blocks):
                    stage_a(hg, idx)
                if idx >= LOOK:
                    stage_b(hg, idx - LOOK)
                if nxt is not None and idx % every == every - 1 and pi_ < NT:
                    proj_tile(nxt, pi_)
                    pi_ += 1
            if nxt is not None:
                while pi_ < NT:
                    proj_tile(nxt, pi_)
                    pi_ += 1


def k_layer(self, li, b, first, last, after_experts=None):
    P, I = self.P, self.I
    with P.scope():
        catT = P.sbuf("catT", [128, 8, S], BF16)
        BcatT = Buf()
        banks = Banks(P, 6)
        with P.scope():
            if li % 2 == 0:
                self.dsa(b, banks, catT, BcatT)
                w_in, qm_off = I["dsa_w_in"][0], 2888
            else:
                self.gla(b, banks, catT, BcatT)
                w_in, qm_off = I["gla_w_in"][0], 2320
        with P.scope():
          self.mem_attn(li, b, banks, w_in, qm_off, catT, BcatT)
          if self.dbgc is not None:
            if True:
                stg = [P.sbuf("stg", [128, 512], F32) for _ in range(2)]
                Bstg = [Buf(), Buf()]
                kk = 0
                for c in range(8):
                    for tt in range(4):
                        p = kk % 2
                        kk += 1
                        P.dve(lambda e, p=p, c=c, tt=tt: e.tensor_copy(out=stg[p][:], in_=catT[:, c, tt * 512:(tt + 1) * 512]),
                              reads=[BcatT], writes=[Bstg[p]])
                        P.dma("sp", self.dbgc[c * 128:(c + 1) * 128, tt * 512:(tt + 1) * 512], stg[p][:], reads=[Bstg[p]])
          if first:
              xsrc, Bxsrc = I["x"][b], [Buf() for _ in range(NT)]
          else:
              xsrc, Bxsrc = self.x2s, self.Bx2s
          self.xphase(li, b, banks, catT, BcatT, xsrc, Bxsrc)
    self.moe(li, b, last, after_experts)


K.rope_tables = k_rope_tables
K._rope_tables_body = k_rope_tables_body
K.rope = k_rope
K.dsa = k_dsa
K.layer = k_layer


def build_full(nseq=NB_PER_CORE, layers=(0, 1)):
    k = K(nseq=nseq)
    k.consts()
    k.load_xT(0)
    for b in range(nseq):
        for li in layers:
            nxt = None
            if li == layers[-1] and b + 1 < nseq:
                nxt = (lambda bb=b + 1: k.load_xT(bb))
            k.layer(li, b, li == layers[0], li == layers[-1], nxt)
        k.P.barrier()
    k.P.finish()
    return k.nc


_NC_CACHE = {}


def kernel(**inputs):
    n = 8
    if "nc" not in _NC_CACHE:
        _NC_CACHE["nc"] = build_full()
    nc = _NC_CACHE["nc"]
    x = np.ascontiguousarray(inputs["x"], dtype=np.float32)
    mem = np.asarray(inputs["mem"], dtype=np.float32)
    pos = np.asarray(inputs["positions"], dtype=np.int32)
    shared = {k_: np.ascontiguousarray(v) for k_, v in inputs.items() if k_ not in ("x", "mem", "positions")}
    in_maps = []
    for c in range(n):
        sl = slice(c * NB_PER_CORE, (c + 1) * NB_PER_CORE)
        m = dict(shared)
        m["x"] = x[sl]
        m["xT"] = np.ascontiguousarray(x[sl].transpose(0, 2, 1))
        m["memT"] = np.ascontiguousarray(mem[sl].transpose(0, 2, 1))
        m["pos"] = np.ascontiguousarray(pos[sl].reshape(NB_PER_CORE, NT, 128).transpose(0, 2, 1))
        in_maps.append(m)
    res = run_bass_kernel_spmd(nc, in_maps, core_ids=list(range(n)))
    return np.concatenate([r["out"] for r in res.results], axis=0).astype(np.float32)
```
